# Optimizing a Trainium2 kernel written in Bass

```python
import jax, jax.numpy as jnp
from jax import lax
import numpy as np

D_MODEL = 1024
BATCH = 16
SEQ = 2048
DEPTH = 2

CHUNK = 64
MIX_WIDTH = D_MODEL
RWKV_HEADS = 8
RWKV_HEAD_DIM = 64
RWKV_WIDTH = RWKV_HEADS * RWKV_HEAD_DIM
DECAY_LORA = 64
ICL_LORA = 64
GATE_LORA = 128
RWKV_IN = 3 * RWKV_WIDTH + DECAY_LORA + ICL_LORA + GATE_LORA
RET_HEADS = 4
RET_QK_DIM = 64
RET_V_DIM = 128
RET_QK_WIDTH = RET_HEADS * RET_QK_DIM
RET_V_WIDTH = RET_HEADS * RET_V_DIM
RET_IN = 2 * RET_QK_WIDTH + 2 * RET_V_WIDTH
PROJ_WIDTH = RWKV_IN + RET_IN
D_FF = 2816
ROPE_BASE = 10000.0
NORM_EPS = 1e-6
LN_X_EPS = 64e-5

kernel_name = "hybrid_rwkv7_retnet_macaron_trunk"


def rms_norm(x, gain, eps=NORM_EPS):
    xf = x.astype(jnp.float32)
    y = xf * lax.rsqrt(jnp.mean(xf * xf, axis=-1, keepdims=True) + eps)
    return (y * gain.astype(jnp.float32)).astype(x.dtype)


def swiglu(h, w_gate, w_up, w_down):
    return (jax.nn.silu(h @ w_gate) * (h @ w_up)) @ w_down


def token_shift(p, mu):
    prev = jnp.pad(p, ((0, 0), (1, 0), (0, 0)))[:, :-1]
    return p + mu * (prev - p)


def rwkv7_scan(r, w, k, v, a, b):
    B, S, H, N = r.shape

    def step(state, inp):
        r_t, w_t, k_t, v_t, a_t, b_t = inp
        sa = jnp.einsum('bhvk,bhk->bhv', state, a_t)
        state = (state * w_t[:, :, None, :] + sa[..., None] * b_t[:, :, None, :]
                 + v_t[..., None] * k_t[:, :, None, :])
        return state, jnp.einsum('bhvk,bhk->bhv', state, r_t)

    xs = (jnp.moveaxis(r, 1, 0), jnp.moveaxis(w, 1, 0), jnp.moveaxis(k, 1, 0),
          jnp.moveaxis(v, 1, 0), jnp.moveaxis(a, 1, 0), jnp.moveaxis(b, 1, 0))
    state0 = jnp.zeros((B, H, N, N), jnp.float32)
    _, y = lax.scan(step, state0, xs)
    return jnp.moveaxis(y, 0, 1)


def rwkv7_group(p, mu, w0, w_lora_up, a0, a_lora_up, g_lora_up, k_k, k_a, r_k,
                ln_w, ln_b):
    B, S, _ = p.shape
    p = token_shift(p, mu)
    o1 = RWKV_WIDTH
    o2 = 2 * RWKV_WIDTH
    o3 = 3 * RWKV_WIDTH
    o4 = o3 + DECAY_LORA
    o5 = o4 + ICL_LORA
    r, k, v, w_d, a_d, g_d = jnp.split(p, [o1, o2, o3, o4, o5], axis=-1)
    f32 = jnp.float32
    log_w = -jax.nn.softplus(-(w0 + jnp.tanh(w_d) @ w_lora_up).astype(f32)) - 0.5
    decay = jnp.exp(-jnp.exp(log_w))
    a = jax.nn.sigmoid((a0 + a_d @ a_lora_up).astype(f32))
    g = (jax.nn.sigmoid(g_d) @ g_lora_up).astype(f32)

    def heads(t):
        return t.astype(f32).reshape(B, S, RWKV_HEADS, RWKV_HEAD_DIM)

    kk = heads(k * k_k)
    kk = kk * lax.rsqrt(jnp.maximum(jnp.sum(kk * kk, axis=-1, keepdims=True), 1e-24))
    k_h = heads(k.astype(f32) * (1.0 + (a - 1.0) * k_a.astype(f32)))
    r_h = heads(r)
    v_h = heads(v)
    a_h = heads(a)
    y = rwkv7_scan(r_h, heads(decay), k_h, v_h, -kk, kk * a_h)
    mean = jnp.mean(y, axis=-1, keepdims=True)
    var = jnp.mean(jnp.square(y - mean), axis=-1, keepdims=True)
    y = ((y - mean) * lax.rsqrt(var + LN_X_EPS)
         * ln_w.astype(f32).reshape(RWKV_HEADS, RWKV_HEAD_DIM)
         + ln_b.astype(f32).reshape(RWKV_HEADS, RWKV_HEAD_DIM))
    y = y + jnp.sum(r_h * k_h * r_k.astype(f32), axis=-1, keepdims=True) * v_h
    return (y.reshape(B, S, RWKV_WIDTH) * g).astype(p.dtype)


def rotary(x, pos):
    half = x.shape[-1] // 2
    inv_freq = 1.0 / (ROPE_BASE ** jnp.linspace(0.0, 1.0, half, dtype=jnp.float32))
    ang = pos[:, None] * inv_freq[None, :]
    cos = jnp.cos(ang)[None, :, None, :]
    sin = jnp.sin(ang)[None, :, None, :]
    x1, x2 = x[..., :half], x[..., half:]
    return jnp.concatenate([x1 * cos - x2 * sin, x1 * sin + x2 * cos], axis=-1)


def retention_chunkwise(q, k, v):
    B, S, H, dk = q.shape
    dv = v.shape[-1]
    nc = S // CHUNK
    log_gamma = jnp.log(1.0 - jnp.power(2.0, -5.0 - jnp.arange(H, dtype=jnp.float32)))
    qc = q.reshape(B, nc, CHUNK, H, dk)
    kc = k.reshape(B, nc, CHUNK, H, dk)
    vc = v.reshape(B, nc, CHUNK, H, dv)
    pos = jnp.arange(CHUNK, dtype=jnp.float32)
    dist = jnp.abs(pos[:, None] - pos[None, :])
    intra_decay = jnp.exp(log_gamma[:, None, None] * dist)
    scores = jnp.einsum('bnihd,bnjhd->bnhij', qc, kc) * intra_decay
    intra = jnp.einsum('bnhij,bnjhe->bnihe', scores, vc)
    key_w = jnp.exp(log_gamma[:, None] * (CHUNK - 1.0 - pos)[None, :])
    chunk_kv = jnp.einsum('bnjhd,hj,bnjhe->bnhde', kc, key_w, vc)
    chunk_decay = jnp.exp(log_gamma * CHUNK)[None, :, None, None]

    def step(state, kv_n):
        return state * chunk_decay + kv_n, state

    _, prev = lax.scan(step, jnp.zeros((B, H, dk, dv), jnp.float32),
                       jnp.moveaxis(chunk_kv, 1, 0))
    prev = jnp.moveaxis(prev, 0, 1)
    query_w = jnp.exp(log_gamma[:, None] * (pos + 1.0)[None, :])
    cross = jnp.einsum('bnihd,hi,bnhde->bnihe', qc, query_w, prev)
    return (intra + cross).reshape(B, S, H, dv)


def retnet_group(p):
    B, S, _ = p.shape
    f32 = jnp.float32
    q, k, v, g = jnp.split(p, [RET_QK_WIDTH, 2 * RET_QK_WIDTH,
                               2 * RET_QK_WIDTH + RET_V_WIDTH], axis=-1)
    pos = jnp.arange(S, dtype=f32)
    q = rotary(q.astype(f32).reshape(B, S, RET_HEADS, RET_QK_DIM), pos)
    k = rotary(k.astype(f32).reshape(B, S, RET_HEADS, RET_QK_DIM), pos) * (RET_QK_DIM ** -0.5)
    v = v.astype(f32).reshape(B, S, RET_HEADS, RET_V_DIM)
    o = retention_chunkwise(q, k, v)
    o = o * lax.rsqrt(jnp.mean(o * o, axis=-1, keepdims=True) + NORM_EPS)
    return (o.reshape(B, S, RET_V_WIDTH) * jax.nn.silu(g.astype(f32))).astype(p.dtype)


def setup_inputs(seed: int = 0) -> dict:
    key = jax.random.key(seed)
    ks = jax.random.split(key, 32)
    f32 = jnp.float32

    def nrm(k, shape, scale):
        return jax.random.normal(k, shape, f32) * scale

    decay_speed = -7.0 + 5.0 * jnp.linspace(0.0, 1.0, RWKV_WIDTH, dtype=f32) ** 0.85 + 0.5
    return {
        "x": nrm(ks[0], (BATCH, SEQ, D_MODEL), 1.0),
        "ffn1_norm": 1.0 + nrm(ks[1], (DEPTH, D_MODEL), 0.1),
        "ffn1_w_gate": nrm(ks[2], (DEPTH, D_MODEL, D_FF), D_MODEL ** -0.5),
        "ffn1_w_up": nrm(ks[3], (DEPTH, D_MODEL, D_FF), D_MODEL ** -0.5),
        "ffn1_w_down": nrm(ks[4], (DEPTH, D_FF, D_MODEL), D_FF ** -0.5),
        "mix_norm": 1.0 + nrm(ks[5], (DEPTH, D_MODEL), 0.1),
        "w_in": nrm(ks[6], (DEPTH, D_MODEL, PROJ_WIDTH), D_MODEL ** -0.5),
        "shift_mu": jax.random.uniform(ks[7], (DEPTH, RWKV_IN), f32),
        "w0": decay_speed[None, :] + nrm(ks[8], (DEPTH, RWKV_WIDTH), 0.1),
        "w_lora_up": nrm(ks[9], (DEPTH, DECAY_LORA, RWKV_WIDTH), 0.1),
        "a0": nrm(ks[10], (DEPTH, RWKV_WIDTH), 0.1),
        "a_lora_up": nrm(ks[11], (DEPTH, ICL_LORA, RWKV_WIDTH), 0.5 * ICL_LORA ** -0.5),
        "g_lora_up": nrm(ks[12], (DEPTH, GATE_LORA, RWKV_WIDTH), GATE_LORA ** -0.5),
        "k_k": 0.85 + nrm(ks[13], (DEPTH, RWKV_WIDTH), 0.05),
        "k_a": 1.0 + nrm(ks[14], (DEPTH, RWKV_WIDTH), 0.05),
        "r_k": nrm(ks[15], (DEPTH, RWKV_HEADS, RWKV_HEAD_DIM), 0.1),
        "ln_x_w": 1.0 + nrm(ks[16], (DEPTH, RWKV_WIDTH), 0.1),
        "ln_x_b": nrm(ks[17], (DEPTH, RWKV_WIDTH), 0.01),
        "w_out": nrm(ks[18], (DEPTH, MIX_WIDTH, D_MODEL), MIX_WIDTH ** -0.5),
        "ffn2_norm": 1.0 + nrm(ks[19], (DEPTH, D_MODEL), 0.1),
        "ffn2_w_gate": nrm(ks[20], (DEPTH, D_MODEL, D_FF), D_MODEL ** -0.5),
        "ffn2_w_up": nrm(ks[21], (DEPTH, D_MODEL, D_FF), D_MODEL ** -0.5),
        "ffn2_w_down": nrm(ks[22], (DEPTH, D_FF, D_MODEL), D_FF ** -0.5),
        "final_norm": 1.0 + nrm(ks[23], (D_MODEL,), 0.1),
    }


def reference(x, ffn1_norm, ffn1_w_gate, ffn1_w_up, ffn1_w_down, mix_norm, w_in,
              shift_mu, w0, w_lora_up, a0, a_lora_up, g_lora_up, k_k, k_a, r_k,
              ln_x_w, ln_x_b, w_out, ffn2_norm, ffn2_w_gate, ffn2_w_up, ffn2_w_down,
              final_norm):
    for l in range(DEPTH):
        x = x + 0.5 * swiglu(rms_norm(x, ffn1_norm[l]), ffn1_w_gate[l], ffn1_w_up[l],
                             ffn1_w_down[l])
        h = rms_norm(x, mix_norm[l])
        proj = h @ w_in[l]
        y_rwkv = rwkv7_group(proj[..., :RWKV_IN], shift_mu[l], w0[l], w_lora_up[l], a0[l],
                             a_lora_up[l], g_lora_up[l], k_k[l], k_a[l], r_k[l],
                             ln_x_w[l], ln_x_b[l])
        y_ret = retnet_group(proj[..., RWKV_IN:])
        mixed = jnp.concatenate([y_rwkv, y_ret], axis=-1).astype(x.dtype)
        x = x + mixed @ w_out[l]
        x = x + 0.5 * swiglu(rms_norm(x, ffn2_norm[l]), ffn2_w_gate[l], ffn2_w_up[l],
                             ffn2_w_down[l])
    return rms_norm(x, final_norm)
```

```python
import contextlib
import numpy as np
import concourse.bass as bass
import concourse.mybir as mybir
from concourse.bass_utils import run_bass_kernel_spmd

F32 = mybir.dt.float32
BF16 = mybir.dt.bfloat16
AF = mybir.ActivationFunctionType
ALU = mybir.AluOpType
AX = mybir.AxisListType

D = 1024
DFF = 2816
NF = DFF // 128
PROJ = 3328
RW_IN = 1792
NCORES = 8
ENGS = ("pe", "act", "dve", "pool", "sp")
C_DEC = float(np.exp(-0.5))


class _StopTile(Exception):
    pass


class _Op:
    __slots__ = ("eng", "fn", "deps", "dma_deps", "idx", "needs_inc", "count",
                 "dma_sem", "epoch")


class Sched:
    def __init__(self, nc):
        self.nc = nc
        self.streams = {e: [] for e in ENGS}
        self.last_w = {}
        self.readers = {}
        self.seen = {e: {} for e in ENGS}
        self.seen_dma = {e: {} for e in ENGS}
        self.dma_counts = {}
        self.epoch = 0
        self.alias = {}

    def _new(self, eng, fn):
        o = _Op()
        o.eng = eng
        o.fn = fn
        o.deps = []
        o.dma_deps = []
        o.idx = len(self.streams[eng])
        o.needs_inc = False
        o.count = None
        o.dma_sem = None
        o.epoch = self.epoch
        return o

    def _collect(self, op, reads, writes):
        deps = []
        for k in reads:
            w = self.last_w.get(k)
            if w is not None:
                deps.append((w, "raw"))
        for k in writes:
            w = self.last_w.get(k)
            if w is not None:
                deps.append((w, "waw"))
            for r in self.readers.get(k, ()):
                deps.append((r, "war"))
        e = op.eng
        for d, kind in deps:
            if d is op:
                continue
            if d.dma_sem is not None:
                cnt = self.dma_counts[d.dma_sem]
                if self.seen_dma[e].get(d.dma_sem, 0) < cnt:
                    self.seen_dma[e][d.dma_sem] = cnt
                    op.dma_deps.append((d.dma_sem, cnt))
                continue
            if d.epoch != self.epoch:
                continue
            if d.eng == e and e == "pe":
                continue
            if self.seen[e].get(d.eng, -1) >= d.idx:
                continue
            self.seen[e][d.eng] = d.idx
            d.needs_inc = True
            op.deps.append(d)
        for k in reads:
            self.readers.setdefault(k, []).append(op)
        for k in writes:
            self.last_w[k] = op
            self.readers[k] = []

    def op(self, eng, fn, reads=(), writes=()):
        reads = [self.alias.get(k, k) for k in reads]
        writes = [self.alias.get(k, k) for k in writes]
        o = self._new(eng, fn)
        self._collect(o, reads, writes)
        self.streams[eng].append(o)
        return o

    def dma(self, queue, out, in_, reads=(), writes=(), sem="dma0"):
        def fn(eng, out=out, in_=in_):
            return eng.dma_start(out=out, in_=in_)
        sem = f"{sem}_{queue}"
        o = self.op(queue, fn, reads, writes)
        o.dma_sem = sem
        self.dma_counts[sem] = self.dma_counts.get(sem, 0) + 1
        return o

    def barrier(self):
        lasts = {}
        for e in ENGS:
            for o in reversed(self.streams[e]):
                if o.epoch != self.epoch:
                    break
                if o.dma_sem is None and o.fn is not None:
                    lasts[e] = o
                    break
        for e in ENGS:
            o = self._new(e, None)
            for e2, l in lasts.items():
                if e2 == e and e == "pe":
                    continue
                if self.seen[e].get(e2, -1) < l.idx:
                    l.needs_inc = True
                    o.deps.append(l)
            for s, c in self.dma_counts.items():
                if self.seen_dma[e].get(s, 0) < c:
                    self.seen_dma[e][s] = c
                    o.dma_deps.append((s, c))
            self.streams[e].append(o)
        self.epoch += 1
        self.seen = {e: {} for e in ENGS}

    def emit(self, final_waits=()):
        nc = self.nc
        n_epochs = self.epoch + 1
        for e in ENGS:
            c = 0
            ep = 0
            for o in self.streams[e]:
                if o.epoch != ep:
                    ep = o.epoch
                    c = 0
                if o.needs_inc:
                    c += 1
                    o.count = c
        with contextlib.ExitStack() as st:
            esem = {}
            for e in ENGS:
                used = set(o.epoch for o in self.streams[e] if o.needs_inc)
                for ep in sorted(used):
                    esem[(e, ep)] = st.enter_context(nc.semaphore(f"s_{e}_{ep}"))
            dsem = {s: st.enter_context(nc.semaphore(f"d_{s}")) for s in self.dma_counts}
            block = st.enter_context(nc.Block())

            def replay(e, eng):
                for o in self.streams[e]:
                    for d in o.deps:
                        eng.wait_ge(esem[(d.eng, d.epoch)], d.count)
                    for s, c in o.dma_deps:
                        eng.wait_ge(dsem[s], 16 * c)
                    if o.fn is None:
                        continue
                    ins = o.fn(eng)
                    if o.dma_sem is not None:
                        ins.then_inc(dsem[o.dma_sem], 16)
                    elif o.needs_inc:
                        ins.then_inc(esem[(o.eng, o.epoch)], 1)
                if e == "sp":
                    for s in final_waits:
                        eng.wait_ge(dsem[s], 16 * self.dma_counts[s])

            @block.tensor
            def _(eng):
                replay("pe", eng)

            @block.scalar
            def _(eng):
                replay("act", eng)

            @block.vector
            def _(eng):
                replay("dve", eng)

            @block.gpsimd
            def _(eng):
                replay("pool", eng)

            @block.sync
            def _(eng):
                replay("sp", eng)


def make_consts(seq):
    nt = seq // 64
    p = np.arange(128)
    s_of = p // 64
    t_of = p % 64
    same = (s_of[:, None] == s_of[None, :])
    c = {}
    c["ident"] = np.eye(128, dtype=np.float32)
    c["tri_i"] = (1.0 * (same & (t_of[:, None] <= t_of[None, :]))).astype(np.float32)
    c["tri_r"] = (1.0 * (same & (t_of[:, None] > t_of[None, :]))).astype(np.float32)
    seqind = np.zeros((128, 2), np.float32)
    seqind[p, s_of] = 1.0
    c["seqind"] = seqind
    su = (same & (t_of[:, None] < t_of[None, :])).astype(np.float32)
    ui = (same & (t_of[:, None] <= t_of[None, :])).astype(np.float32)
    sl = (same & (t_of[:, None] > t_of[None, :])).astype(np.float32)
    c["m1"] = np.concatenate([su, ui], axis=1)
    c["msl"] = sl
    H = 4
    log_g = np.log(1.0 - np.power(2.0, -5.0 - np.arange(H, dtype=np.float64)))
    j = t_of[:, None].astype(np.float64)
    i = t_of[None, :].astype(np.float64)
    dm = np.zeros((128, H, 128), np.float64)
    for h in range(H):
        dm[:, h, :] = same * np.exp(log_g[h] * (np.abs(i - j) - (i + 1.0))) * 0.125
    c["dmask"] = dm.reshape(128, H * 128).astype(np.float32)
    qw = np.exp(log_g[None, :] * (t_of[:, None] + 1.0))
    kw = np.exp(log_g[None, :] * (63.0 - t_of[:, None])) * 0.125
    cd = np.broadcast_to(np.exp(log_g * 64.0)[None, :], (128, H))
    c["qkw"] = np.concatenate([qw, kw, cd], axis=1).astype(np.float32)
    half = 32
    inv_freq = (1.0 / (np.float32(10000.0) ** np.linspace(0.0, 1.0, half, dtype=np.float32))).astype(np.float32)
    pos = np.arange(seq, dtype=np.float32)
    ang = (pos[:, None] * inv_freq[None, :]).astype(np.float32).astype(np.float64)
    cs = np.concatenate([np.cos(ang), np.sin(ang)], axis=1).astype(np.float32)
    cs = cs.reshape(nt, 64, 64).transpose(1, 0, 2)
    c["rope"] = np.ascontiguousarray(np.concatenate([cs, cs], axis=0).reshape(128, nt * 64))
    return c


CONST_ORDER = ["ident", "tri_i", "tri_r", "seqind", "m1", "msl", "dmask", "qkw", "rope"]


def build_program(seq, depth, do_ffn=True, do_mix=True, final_norm=True):
    nc = bass.Bass("TRN2", target_bir_lowering=False)
    ntok = 2 * seq
    nt = seq // 64

    def din(name, shape):
        return nc.dram_tensor(name, list(shape), F32, kind="ExternalInput").ap()

    x_in = din("x", [ntok, D])
    w = {}
    for nm, shp in [("ffn1_norm", [depth, D]), ("ffn1_w_gate", [depth, D, DFF]), ("ffn1_w_up", [depth, D, DFF]),
                    ("ffn1_w_down", [depth, DFF, D]), ("mix_norm", [depth, D]), ("w_in", [depth, D, PROJ]),
                    ("shift_mu", [depth, RW_IN]), ("w0", [depth, 512]), ("w_lora_up", [depth, 64, 512]),
                    ("a0", [depth, 512]), ("a_lora_up", [depth, 64, 512]), ("g_lora_up", [depth, 128, 512]),
                    ("k_k", [depth, 512]), ("k_a", [depth, 512]), ("r_k", [depth, 512]),
                    ("ln_x_w", [depth, 512]), ("ln_x_b", [depth, 512]), ("w_out", [depth, D, D]),
                    ("ffn2_norm", [depth, D]), ("ffn2_w_gate", [depth, D, DFF]), ("ffn2_w_up", [depth, D, DFF]),
                    ("ffn2_w_down", [depth, DFF, D]), ("final_norm", [1, D])]:
        w[nm] = din(nm, shp)
    cshape = {"ident": 128, "tri_i": 128, "tri_r": 128, "seqind": 2, "m1": 256, "msl": 128,
              "dmask": 512, "qkw": 12, "rope": nt * 64}
    cd = {k: din("c_" + k, [128, v]) for k, v in cshape.items()}
    out = nc.dram_tensor("out", [ntok, D], F32, kind="ExternalOutput").ap()

    S = Sched(nc)
    st = contextlib.ExitStack()
    with st:
        uid = [0]

        def sb(name, shape, dt=F32, stack=st):
            uid[0] += 1
            return stack.enter_context(nc.sbuf_tensor(f"{name}_{uid[0]}", list(shape), dt))

        banks = [st.enter_context(nc.psum_tensor(f"ps{i}", [128, 512], F32)) for i in range(8)]
        pctr = [0]

        def getps():
            i = pctr[0] % 8
            pctr[0] += 1
            return banks[i], f"ps{i}"

        ident_b = sb("ident_b", [128, 128], BF16)
        tri_i = sb("tri_i", [128, 128], BF16)
        tri_r = sb("tri_r", [128, 128], BF16)
        seqind = sb("seqind", [128, 2], BF16)
        m1 = sb("m1", [128, 256])
        msl = sb("msl", [128, 128])
        ident_f = sb("ident_f", [128, 128])
        dmask = sb("dmask", [128, 512])
        qkw = sb("qkw", [128, 12])
        S.dma("pool", ident_b[:], cd["ident"], writes=["ident_b"], sem="c")
        S.dma("sp", ident_f[:], cd["ident"], writes=["ident_f"], sem="c")
        S.dma("pool", tri_i[:], cd["tri_i"], writes=["tri_i"], sem="c")
        S.dma("pool", tri_r[:], cd["tri_r"], writes=["tri_r"], sem="c")
        S.dma("pool", seqind[:], cd["seqind"], writes=["seqind"], sem="c")
        S.dma("sp", m1[:], cd["m1"], writes=["m1"], sem="c")
        S.dma("sp", msl[:], cd["msl"], writes=["msl"], sem="c")
        S.dma("sp", dmask[:], cd["dmask"], writes=["dmask"], sem="c")
        S.dma("sp", qkw[:], cd["qkw"], writes=["qkw"], sem="c")

        src_x = [x_in]

        def rstd_ops(ss, n, tag):
            S.op("dve", lambda e: e.tensor_scalar(ss[:, 0:n], ss[:, 0:n], 1.0 / D, 1e-6, ALU.mult, ALU.add),
                 reads=[tag], writes=[tag])
            S.op("act", lambda e: e.activation(ss[:, 0:n], ss[:, 0:n], AF.Sqrt), reads=[tag], writes=[tag])
            S.op("dve", lambda e: e.reciprocal(ss[:, 0:n], ss[:, 0:n]), reads=[tag], writes=[tag])

        def ffn_phase(l, which, last):
            TB = 256
            nblk = ntok // TB
            with contextlib.ExitStack() as fs:
                wg = sb("wg", [128, 8, DFF], BF16, fs)
                wu = sb("wu", [128, 8, DFF], BF16, fs)
                wd = sb("wd", [128, NF, D], BF16, fs)
                gain = sb("gain", [128, D], F32, fs)
                xt = [sb(f"fx{i}", [128, 2, D], F32, fs) for i in range(2)]
                hb = sb("fh", [128, 2, D], BF16, fs)
                hT = sb("fhT", [128, 8, TB], BF16, fs)
                aT = sb("faT", [128, NF, TB], BF16, fs)
                sg = [sb(f"fsg{i}", [128, TB], F32, fs) for i in range(2)]
                junk = sb("fjunk", [128, D], BF16, fs)
                ss = [sb(f"fss{i}", [128, 4], F32, fs) for i in range(2)]
                if last:
                    fin_bc = sb("fin_bc", [128, D], F32, fs)
                    S.dma("sp", fin_bc[:], w["final_norm"][0:1, :].broadcast_to([128, D]), writes=["fin_bc"], sem="w")
                pre = "ffn1" if which == 1 else "ffn2"
                S.dma("sp", gain[:], w[pre + "_norm"][l:l + 1, :].broadcast_to([128, D]), writes=["gain"], sem="w")
                for c in range(8):
                    S.dma("pool", wg[:, c, :], w[pre + "_w_gate"][l, c * 128:(c + 1) * 128, :], writes=["wg"], sem="w")
                    S.dma("pool", wu[:, c, :], w[pre + "_w_up"][l, c * 128:(c + 1) * 128, :], writes=["wu"], sem="w")
                for f in range(NF):
                    S.dma("pool", wd[:, f, :], w[pre + "_w_down"][l, f * 128:(f + 1) * 128, :], writes=["wd"], sem="w")

                def load(b):
                    i = b % 2
                    src = src_x[0]
                    for j in range(2):
                        r0 = b * TB + j * 128
                        S.dma("sp", xt[i][:, j, :], src[r0:r0 + 128, :], writes=[f"fx{i}"], sem=f"fl{i}")

                load(0)
                for b in range(nblk):
                    i = b % 2
                    X = xt[i]
                    xk = f"fx{i}"
                    if b + 1 < nblk:
                        load(b + 1)
                    ssb = ss[i]
                    sk = f"fss{i}"
                    for j in range(2):
                        S.op("act", lambda e, j=j, X=X, ssb=ssb: e.activation(junk[:], X[:, j, :], AF.Square,
                                                                              accum_out=ssb[:, j:j + 1]),
                             reads=[xk], writes=["fjunk", sk])
                    rstd_ops(ssb, 2, sk)
                    for j in range(2):
                        S.op("dve", lambda e, j=j, X=X, ssb=ssb: e.scalar_tensor_tensor(
                            hb[:, j, :], X[:, j, :], ssb[:, j:j + 1], gain[:], ALU.mult, ALU.mult),
                            reads=[xk, sk, "gain"], writes=["fh"])
                    for half in range(2):
                        ps, pk = getps()
                        psb = ps[:].bitcast(BF16)
                        for cc in range(4):
                            c = half * 4 + cc
                            for j in range(2):
                                S.op("pe", lambda e, c=c, cc=cc, j=j, psb=psb: e.transpose(
                                    psb[:, cc * 256 + j * 128: cc * 256 + (j + 1) * 128],
                                    hb[:, j, c * 128:(c + 1) * 128], ident_b[:]),
                                    reads=["fh", "ident_b"], writes=[pk])
                        eng = "act" if half == 0 else "dve"
                        if eng == "act":
                            S.op("act", lambda e, half=half, psb=psb: e.activation(
                                hT[:, half * 4:(half + 1) * 4, :], psb.rearrange("p (c t) -> p c t", c=4), AF.Copy),
                                reads=[pk], writes=["fhT"])
                        else:
                            S.op("dve", lambda e, half=half, psb=psb: e.tensor_copy(
                                hT[:, half * 4:(half + 1) * 4, :], psb.rearrange("p (c t) -> p c t", c=4)),
                                reads=[pk], writes=["fhT"])
                    for f in range(NF):
                        ps, pk = getps()
                        for c in range(8):
                            S.op("pe", lambda e, c=c, f=f, ps=ps: e.matmul(
                                ps[:, 0:TB], wg[:, c, f * 128:(f + 1) * 128], hT[:, c, :],
                                start=(c == 0), stop=(c == 7)), reads=["wg", "fhT"], writes=[pk])
                        for c in range(8):
                            S.op("pe", lambda e, c=c, f=f, ps=ps: e.matmul(
                                ps[:, TB:2 * TB], wu[:, c, f * 128:(f + 1) * 128], hT[:, c, :],
                                start=(c == 0), stop=(c == 7)), reads=["wu", "fhT"], writes=[pk])
                        sgb = sg[f % 2]
                        sgk = f"fsg{f % 2}"
                        S.op("act", lambda e, ps=ps, sgb=sgb: e.activation(sgb[:], ps[:, 0:TB], AF.Silu),
                             reads=[pk], writes=[sgk])
                        S.op("dve", lambda e, ps=ps, sgb=sgb, f=f: e.tensor_tensor(
                            aT[:, f, :], sgb[:], ps[:, TB:2 * TB], ALU.mult),
                            reads=[pk, sgk], writes=["faT"])
                    for j in range(2):
                        for n in range(2):
                            ps, pk = getps()
                            for f in range(NF):
                                S.op("pe", lambda e, f=f, j=j, n=n, ps=ps: e.matmul(
                                    ps[:], aT[:, f, j * 128:(j + 1) * 128], wd[:, f, n * 512:(n + 1) * 512],
                                    start=(f == 0), stop=(f == NF - 1)), reads=["faT", "wd"], writes=[pk])
                            S.op("dve", lambda e, j=j, n=n, ps=ps, X=X: e.scalar_tensor_tensor(
                                X[:, j, n * 512:(n + 1) * 512], ps[:], 0.5, X[:, j, n * 512:(n + 1) * 512],
                                ALU.mult, ALU.add), reads=[pk, xk], writes=[xk])
                    if last:
                        for j in range(2):
                            S.op("act", lambda e, j=j, X=X, ssb=ssb: e.activation(
                                junk[:], X[:, j, :], AF.Square, accum_out=ssb[:, 2 + j:3 + j]),
                                reads=[xk], writes=["fjunk", sk])
                        S.op("dve", lambda e, ssb=ssb: e.tensor_scalar(ssb[:, 2:4], ssb[:, 2:4], 1.0 / D, 1e-6,
                                                                       ALU.mult, ALU.add), reads=[sk], writes=[sk])
                        S.op("act", lambda e, ssb=ssb: e.activation(ssb[:, 2:4], ssb[:, 2:4], AF.Sqrt),
                             reads=[sk], writes=[sk])
                        S.op("dve", lambda e, ssb=ssb: e.reciprocal(ssb[:, 2:4], ssb[:, 2:4]), reads=[sk], writes=[sk])
                        for j in range(2):
                            S.op("dve", lambda e, j=j, X=X, ssb=ssb: e.scalar_tensor_tensor(
                                X[:, j, :], X[:, j, :], ssb[:, 2 + j:3 + j], fin_bc[:], ALU.mult, ALU.mult),
                                reads=[xk, sk, "fin_bc"], writes=[xk])
                    for j in range(2):
                        r0 = b * TB + j * 128
                        S.dma("sp", out[r0:r0 + 128, :], X[:, j, :], reads=[xk], sem=f"fs{i}")
                S.barrier()
            src_x[0] = out

        def mix_phase(l):
            with contextlib.ExitStack() as ms:
                wm = sb("wm", [128, 8, 5120], BF16, ms)
                wo = sb("wo", [128, 8, D], BF16, ms)
                wal = sb("wal", [128, 1024], BF16, ms)
                glu = sb("glu", [128, 512], BF16, ms)
                gain = sb("mgain", [128, D], F32, ms)
                bcn = {}
                for nm in ["w0", "a0", "k_k", "k_a", "r_k", "ln_x_w", "ln_x_b"]:
                    bcn[nm] = sb("bc_" + nm, [128, 512], F32, ms)
                    S.dma("sp", bcn[nm][:], w[nm][l:l + 1, :].broadcast_to([128, 512]), writes=["bc_" + nm], sem="w")
                S.dma("sp", gain[:], w["mix_norm"][l:l + 1, :].broadcast_to([128, D]), writes=["mgain"], sem="w")
                S.op("pool", lambda e: e.memset(wal[:], 0.0), writes=["wal"])
                S.dma("pool", wal[0:64, 0:512], w["w_lora_up"][l], writes=["wal"], sem="w")
                S.dma("pool", wal[64:128, 512:1024], w["a_lora_up"][l], writes=["wal"], sem="w")
                S.dma("pool", glu[:], w["g_lora_up"][l], writes=["glu"], sem="w")
                for c in range(8):
                    S.dma("pool", wo[:, c, :], w["w_out"][l, c * 128:(c + 1) * 128, :], writes=["wo"], sem="w")
                    S.dma("pool", wm[:, c, 3584:5120], w["w_in"][l, c * 128:(c + 1) * 128, RW_IN:PROJ],
                          writes=["wm"], sem="w")
                with contextlib.ExitStack() as ps_:
                    mu = sb("mu", [128, RW_IN], F32, ps_)
                    omm = sb("omm", [128, RW_IN], F32, ps_)
                    stg = [sb(f"stg{i}", [128, RW_IN], F32, ps_) for i in range(2)]
                    S.dma("sp", mu[:], w["shift_mu"][l:l + 1, :].broadcast_to([128, RW_IN]), writes=["mu"], sem="w")
                    S.op("dve", lambda e: e.tensor_scalar(omm[:], mu[:], -1.0, 1.0, ALU.mult, ALU.add),
                         reads=["mu"], writes=["omm"])
                    for c in range(8):
                        sg_ = stg[c % 2]
                        sk_ = f"stg{c % 2}"
                        S.dma("sp", sg_[:], w["w_in"][l, c * 128:(c + 1) * 128, 0:RW_IN], writes=[sk_], sem=f"wp{c % 2}")
                        S.op("dve", lambda e, c=c, sg_=sg_: e.tensor_tensor(wm[:, c, 0:RW_IN], sg_[:], omm[:], ALU.mult),
                             reads=[sk_, "omm"], writes=["wm"])
                        S.op("pool", lambda e, c=c, sg_=sg_: e.tensor_tensor(wm[:, c, RW_IN:2 * RW_IN], sg_[:], mu[:], ALU.mult),
                             reads=[sk_, "mu"], writes=["wm"])
                    S.barrier()

                xts = [sb(f"mx{i}", [128, D], F32, ms) for i in range(2)]
                hb = sb("mh", [128, D], BF16, ms)
                junk = hb
                ss = sb("mss", [128, 2], F32, ms)
                hT = sb("mhT", [128, 8, 128], BF16, ms)
                hTp = sb("mhTp", [128, 8, 128], BF16, ms)
                carry = sb("mcarry", [128, 8, 2], BF16, ms)
                ropet = [sb(f"ropet{i}", [128, 64], F32, ms) for i in range(2)]
                G = {i: sb(f"G{i}", [128, 512], F32, ms) for i in (1, 2, 3, 4, 6, 7, 8, 9, 10)}
                G5 = sb("G5", [128, 1024], F32, ms)

                def bf3(t, h):
                    return t[:].bitcast(BF16).rearrange("p (h t) -> p h t", h=h)
                qk_f = Wi = G[1][:]
                rg_s = Wn = G[2][:]
                rot = We = G[3][:]
                o_f = Wh = G[4][:]
                Pm = [bf3(G[1], 8), bf3(G[2], 8)]
                Qm = [bf3(G[3], 8), bf3(G[4], 8)]
                ra = G5[:, 0:256]
                rb = G5[:, 256:512]
                kk = G5[:, 0:512]
                bp = G5[:, 512:1024]
                TTf = G5[:].rearrange("p (h t) -> p h t", h=8)
                sigw = G[6][:]
                TTb = bf3(G[6], 8)
                r_f = y_f = G[7][:]
                k_f = G[8][:]
                mT = bf3(G[8], 8)
                g9 = G[9][:].bitcast(BF16)
                g10 = G[10][:].bitcast(BF16)
                tm4 = [g9[:, 0:512], g9[:, 512:1024], g10[:, 0:512], g10[:, 512:1024]]
                Xb = g10[:, 0:512].rearrange("p (h v) -> p h v", h=8)
                Ub = g10[:, 512:1024].rearrange("p (h v) -> p h v", h=8)
                S.alias.update({"qk_f": "G1", "Wi": "G1", "Pm0": "G1", "rg_s": "G2", "Wn": "G2", "Pm1": "G2",
                                "rot": "G3", "We": "G3", "Qm0": "G3", "o_f": "G4", "Wh": "G4", "Qm1": "G4",
                                "ra": "G5", "rb": "G5", "kk": "G5", "bp": "G5", "TTf": "G5", "sigw": "G6", "TTb": "G6",
                                "r_f": "G7", "y_f": "G7", "k_f": "G8", "mT": "G8", "tm0": "G9", "tm1": "G9",
                                "tm2": "G10", "tm3": "G10", "Xb": "G10", "Ub": "G10", "mjunk": "mh"})
                v_b = sb("v_b", [128, 512], BF16, ms)
                lwT = sb("lwT", [128, 128], BF16, ms)
                lgT = sb("lgT", [128, 128], BF16, ms)
                rv_b = sb("rv_b", [128, 512], BF16, ms)
                a_f = sb("a_f", [128, 512], F32, ms)
                g_f = sb("g_f", [128, 512], F32, ms)
                WC = sb("WC", [128, 8], F32, ms)
                sighl = sb("sighl", [128, 1024], BF16, ms)
                t0 = sb("t0", [128, 512], F32, ms)
                t1 = sb("t1", [128, 512], F32, ms)
                k_h = sb("k_h", [128, 512], F32, ms)
                s8 = sb("s8", [128, 8], F32, ms)
                bon8 = sb("bon8", [128, 8], F32, ms)
                arT = sb("arT", [128, 8, 2, 128], BF16, ms)
                bT = sb("bT", [128, 8, 128], BF16, ms)
                kT = sb("kT", [128, 8, 128], BF16, ms)
                Bh = sb("Bh", [128, 8, 128], BF16, ms)
                Kh = sb("Kh", [128, 8, 128], BF16, ms)
                QA = sb("QA", [128, 8, 2, 128], BF16, ms)
                KA = sb("KA", [128, 8, 2, 128], BF16, ms)
                Sf = sb("Sf", [128, 8, 64], F32, ms)
                Sb = sb("Sb", [128, 8, 64], BF16, ms)
                mixed = sb("mixed", [128, D], BF16, ms)
                qt_b = sb("qt_b", [128, 256], BF16, ms)
                kp_b = sb("kp_b", [128, 256], BF16, ms)
                rKh = sb("rKh", [128, 4, 128], BF16, ms)
                rqT = sb("rqT", [128, 4, 128], BF16, ms)
                rkT = sb("rkT", [128, 4, 128], BF16, ms)
                Sc = sb("Sc", [128, 4, 128], BF16, ms)
                RSf = sb("RSf", [128, 4, 128], F32, ms)
                RSb = sb("RSb", [128, 4, 128], BF16, ms)
                s4 = sb("s4", [128, 4], F32, ms)

                for tname, tt in [("arT", arT), ("bT", bT), ("kT", kT), ("Bh", Bh), ("Kh", Kh), ("rKh", rKh),
                                  ("rqT", rqT), ("rkT", rkT), ("Sb", Sb), ("RSb", RSb)]:
                    S.op("pool", lambda e, tt=tt: e.memset(tt[:], 0.0), writes=[tname])
                S.op("dve", lambda e: e.memset(Sf[:], 0.0), writes=["Sf"])
                S.op("dve", lambda e: e.memset(RSf[:], 0.0), writes=["RSf"])
                S.op("dve", lambda e: e.memset(carry[:], 0.0), writes=["mcarry"])

                def bc3(ap2, n_in, n_out):
                    return ap2.unsqueeze(2).to_broadcast([ap2.shape[0], n_in, n_out])

                def bch(ap2, nh, ncol):
                    return ap2.unsqueeze(1).to_broadcast([ap2.shape[0], nh, ncol])

                def v3(ap2, a, b_):
                    return ap2.rearrange("p (a b) -> p a b", a=a)

                src = src_x[0]
                import os as _os
                _stop = _os.environ.get("MIX_STOP", "")

                def chk(tag):
                    if tag == _stop:
                        raise _StopTile()

                for n in range(nt):
                  try:
                    xt = xts[n % 2]
                    mxk = f"mx{n % 2}"
                    for s in range(2):
                        r0 = s * seq + n * 64
                        S.dma("sp", xt[s * 64:(s + 1) * 64, :], src[r0:r0 + 64, :], writes=[mxk], sem=f"ml{n % 2}")
                    S.op("act", lambda e, xt=xt: e.activation(junk[:], xt[:], AF.Square, accum_out=ss[:, 0:1]),
                         reads=[mxk], writes=["mjunk", "mss"])
                    rstd_ops(ss, 1, "mss")
                    S.op("dve", lambda e, xt=xt: e.scalar_tensor_tensor(hb[:], xt[:], ss[:, 0:1], gain[:], ALU.mult, ALU.mult),
                         reads=[mxk, "mss", "mgain"], writes=["mh"])
                    chk("C")
                    ps, pk = getps()
                    psb = ps[:].bitcast(BF16)
                    for c in range(8):
                        S.op("pe", lambda e, c=c, psb=psb: e.transpose(psb[:, c * 128:(c + 1) * 128],
                                                                       hb[:, c * 128:(c + 1) * 128], ident_b[:]),
                             reads=["mh", "ident_b"], writes=[pk])
                    S.op("act", lambda e, psb=psb: e.activation(
                        hT[:], psb.rearrange("p (c t) -> p c t", c=8), AF.Copy),
                        reads=[pk], writes=["mhT"])
                    hT4 = hT[:].rearrange("p c (s t) -> p c s t", s=2)
                    hTp4 = hTp[:].rearrange("p c (s t) -> p c s t", s=2)
                    S.op("pool", lambda e: e.tensor_copy(hTp4[:, :, :, 1:64], hT4[:, :, :, 0:63]),
                         reads=["mhT"], writes=["mhTp"])
                    S.op("pool", lambda e: e.tensor_copy(hTp4[:, :, :, 0], carry[:]),
                         reads=["mcarry"], writes=["mhTp"])
                    S.op("pool", lambda e: e.tensor_copy(carry[:], hT4[:, :, :, 63]),
                         reads=["mhT"], writes=["mcarry"])

                    def cur(c):
                        return hT[:, c, :]

                    def prev(c):
                        return hTp[:, c, :]

                    chk("D")
                    def proj_rw(ps_ap, col0, ncol, pk):
                        for c in range(8):
                            S.op("pe", lambda e, c=c: e.matmul(ps_ap, cur(c), wm[:, c, col0:col0 + ncol],
                                                               start=(c == 0), stop=False),
                                 reads=["mhT", "wm"], writes=[pk])
                        for c in range(8):
                            S.op("pe", lambda e, c=c: e.matmul(ps_ap, prev(c), wm[:, c, RW_IN + col0:RW_IN + col0 + ncol],
                                                               start=False, stop=(c == 7)),
                                 reads=["mhTp", "wm"], writes=[pk])

                    ps, pk = getps()
                    proj_rw(ps[:], 0, 512, pk)
                    S.op("act", lambda e, ps=ps: e.activation(r_f[:], ps[:], AF.Copy), reads=[pk], writes=["r_f"])
                    ps, pk = getps()
                    proj_rw(ps[:], 512, 512, pk)
                    S.op("dve", lambda e, ps=ps: e.tensor_copy(k_f[:], ps[:]), reads=[pk], writes=["k_f"])
                    ps, pk = getps()
                    proj_rw(ps[:], 1024, 512, pk)
                    S.op("dve", lambda e, ps=ps: e.tensor_copy(v_b[:], ps[:]), reads=[pk], writes=["v_b"])
                    ps, pk = getps()
                    for gi in range(2):
                        col0 = 1536 + gi * 128
                        for c in range(8):
                            S.op("pe", lambda e, c=c, gi=gi, col0=col0, ps=ps: e.matmul(
                                ps[:, gi * 128:(gi + 1) * 128], wm[:, c, col0:col0 + 128], cur(c),
                                start=(c == 0), stop=False), reads=["mhT", "wm"], writes=[pk])
                        for c in range(8):
                            S.op("pe", lambda e, c=c, gi=gi, col0=col0, ps=ps: e.matmul(
                                ps[:, gi * 128:(gi + 1) * 128], wm[:, c, RW_IN + col0:RW_IN + col0 + 128], prev(c),
                                start=False, stop=(c == 7)), reads=["mhTp", "wm"], writes=[pk])
                    S.op("act", lambda e, ps=ps: e.activation(lwT[0:64, :], ps[0:64, 0:128], AF.Tanh), reads=[pk], writes=["lwT"])
                    S.op("dve", lambda e, ps=ps: e.tensor_copy(lwT[64:128, :], ps[64:128, 0:128]), reads=[pk], writes=["lwT"])
                    S.op("act", lambda e, ps=ps: e.activation(lgT[:], ps[:, 128:256], AF.Sigmoid), reads=[pk], writes=["lgT"])
                    ps, pk = getps()
                    for c in range(8):
                        S.op("pe", lambda e, c=c, ps=ps: e.matmul(ps[:], cur(c), wm[:, c, 3584:4096],
                                                                  start=(c == 0), stop=(c == 7)),
                             reads=["mhT", "wm"], writes=[pk])
                    S.op("dve", lambda e, ps=ps: e.tensor_copy(qk_f[:], ps[:]), reads=[pk], writes=["qk_f"])
                    ps, pk = getps()
                    for c in range(8):
                        S.op("pe", lambda e, c=c, ps=ps: e.matmul(ps[:], cur(c), wm[:, c, 4096:4608],
                                                                  start=(c == 0), stop=(c == 7)),
                             reads=["mhT", "wm"], writes=[pk])
                    S.op("act", lambda e, ps=ps: e.activation(rv_b[:], ps[:], AF.Copy), reads=[pk], writes=["rv_b"])
                    ps, pk = getps()
                    for c in range(8):
                        S.op("pe", lambda e, c=c, ps=ps: e.matmul(ps[:], cur(c), wm[:, c, 4608:5120],
                                                                  start=(c == 0), stop=(c == 7)),
                             reads=["mhT", "wm"], writes=[pk])
                    S.op("act", lambda e, ps=ps: e.activation(rg_s[:], ps[:], AF.Silu), reads=[pk], writes=["rg_s"])

                    chk("K")
                    cs_ = ropet[n % 2]
                    ropek = f"ropet{n % 2}"
                    S.dma("sp", cs_[:], cd["rope"][:, n * 64:(n + 1) * 64], writes=[ropek], sem=f"rp{n % 2}")
                    cosb = cs_[:, 0:32].unsqueeze(1).to_broadcast([128, 8, 32])
                    sinb = cs_[:, 32:64].unsqueeze(1).to_broadcast([128, 8, 32])
                    qk4 = qk_f[:].rearrange("p (g a d) -> p g a d", g=8, a=2)
                    rot4 = rot[:].rearrange("p (g a d) -> p g a d", g=8, a=2)
                    ra3 = v3(ra[:], 8, 32)
                    rb3 = v3(rb[:], 8, 32)
                    S.op("dve", lambda e, cosb=cosb, sinb=sinb: e.tensor_tensor(ra3, qk4[:, :, 0, :], cosb, ALU.mult), reads=["qk_f", ropek], writes=["ra"])
                    S.op("pool", lambda e, cosb=cosb, sinb=sinb: e.tensor_tensor(rb3, qk4[:, :, 1, :], sinb, ALU.mult), reads=["qk_f", ropek], writes=["rb"])
                    S.op("dve", lambda e: e.tensor_tensor(rot4[:, :, 0, :], ra3, rb3, ALU.subtract), reads=["ra", "rb"], writes=["rot"])
                    S.op("dve", lambda e, cosb=cosb, sinb=sinb: e.tensor_tensor(ra3, qk4[:, :, 0, :], sinb, ALU.mult), reads=["qk_f", ropek], writes=["ra"])
                    S.op("pool", lambda e, cosb=cosb, sinb=sinb: e.tensor_tensor(rb3, qk4[:, :, 1, :], cosb, ALU.mult), reads=["qk_f", ropek], writes=["rb"])
                    S.op("dve", lambda e: e.tensor_tensor(rot4[:, :, 1, :], ra3, rb3, ALU.add), reads=["ra", "rb"], writes=["rot"])
                    S.op("dve", lambda e: e.tensor_tensor(v3(qt_b[:], 4, 64), v3(rot[:, 0:256], 4, 64), bc3(qkw[:, 0:4], 4, 64), ALU.mult),
                         reads=["rot", "qkw"], writes=["qt_b"])
                    S.op("pool", lambda e: e.tensor_copy(kp_b[:], rot[:, 256:512]), reads=["rot"], writes=["kp_b"])
                    for s in range(2):
                        sl_ = slice(s * 64, (s + 1) * 64)
                        S.op("dve" if s == 0 else "pool", lambda e, s=s, sl_=sl_: e.tensor_tensor(
                            rKh[sl_, :, s * 64:(s + 1) * 64], v3(rot[sl_, 256:512], 4, 64), bc3(qkw[sl_, 4:8], 4, 64), ALU.mult),
                            reads=["rot", "qkw"], writes=["rKh"])
                    chk("K1")
                    ps, pk = getps()
                    psb = ps[:].bitcast(BF16)
                    for pr in range(2):
                        S.op("pe", lambda e, pr=pr, psb=psb: e.transpose(psb[:, pr * 128:(pr + 1) * 128],
                                                                         qt_b[:, pr * 128:(pr + 1) * 128], ident_b[:]),
                             reads=["qt_b", "ident_b"], writes=[pk])
                        S.op("pe", lambda e, pr=pr, psb=psb: e.transpose(psb[:, (2 + pr) * 128:(3 + pr) * 128],
                                                                         kp_b[:, pr * 128:(pr + 1) * 128], ident_b[:]),
                             reads=["kp_b", "ident_b"], writes=[pk])
                    pv = psb[:, 0:512].rearrange("p (g t) -> p g t", g=4)
                    k_ = 0
                    for gi, (dst, dk) in enumerate([(rqT, "rqT"), (rkT, "rkT")]):
                        dst5 = dst[:].rearrange("p (pr par) t -> p pr par t", par=2)
                        for par in range(2):
                            for s in range(2):
                                d_ = dst5[s * 64:(s + 1) * 64, :, par, s * 64:(s + 1) * 64]
                                i_ = pv[par * 64:(par + 1) * 64, gi * 2:gi * 2 + 2, s * 64:(s + 1) * 64]
                                if k_ % 2 == 0:
                                    S.op("act", lambda e, d_=d_, i_=i_: e.activation(d_, i_, AF.Copy), reads=[pk], writes=[dk])
                                else:
                                    S.op("dve", lambda e, d_=d_, i_=i_: e.tensor_copy(d_, i_), reads=[pk], writes=[dk])
                                k_ += 1
                    chk("K2")
                    ps, pk = getps()
                    for h in range(4):
                        S.op("pe", lambda e, h=h, ps=ps: e.matmul(ps[:, h * 128:(h + 1) * 128], rkT[:, h, :], rqT[:, h, :],
                                                                  start=True, stop=True), reads=["rkT", "rqT"], writes=[pk])
                    S.op("dve", lambda e, ps=ps: e.tensor_tensor(Sc[:], v3(ps[:], 4, 128), v3(dmask[:], 4, 128), ALU.mult),
                         reads=[pk, "dmask"], writes=["Sc"])
                    ps, pk = getps()
                    for h in range(4):
                        S.op("pe", lambda e, h=h, ps=ps: e.matmul(ps[:, h * 128:(h + 1) * 128], Sc[:, h, :],
                                                                  rv_b[:, h * 128:(h + 1) * 128], start=True, stop=False),
                             reads=["Sc", "rv_b"], writes=[pk])
                        S.op("pe", lambda e, h=h, ps=ps: e.matmul(ps[:, h * 128:(h + 1) * 128], rqT[:, h, :], RSb[:, h, :],
                                                                  start=False, stop=True), reads=["rqT", "RSb"], writes=[pk])
                    S.op("act", lambda e, ps=ps: e.activation(o_f[:], ps[:], AF.Copy), reads=[pk], writes=["o_f"])
                    ps, pk = getps()
                    for h in range(4):
                        S.op("pe", lambda e, h=h, ps=ps: e.matmul(ps[:, h * 128:(h + 1) * 128], rKh[:, h, :],
                                                                  rv_b[:, h * 128:(h + 1) * 128], start=True, stop=True),
                             reads=["rKh", "rv_b"], writes=[pk])
                    S.op("dve", lambda e: e.tensor_tensor(RSf[:], RSf[:], bc3(qkw[:, 8:12], 4, 128), ALU.mult),
                         reads=["RSf", "qkw"], writes=["RSf"])
                    S.op("dve", lambda e, ps=ps: e.tensor_tensor(RSf[:], RSf[:], v3(ps[:], 4, 128), ALU.add),
                         reads=[pk, "RSf"], writes=["RSf"])
                    S.op("pool", lambda e: e.tensor_copy(RSb[:], RSf[:]), reads=["RSf"], writes=["RSb"])
                    S.op("pool", lambda e: e.tensor_tensor(t1[:], o_f[:], o_f[:], ALU.mult), reads=["o_f"], writes=["t1"])
                    S.op("dve", lambda e: e.tensor_reduce(s4[:], v3(t1[:], 4, 128), AX.X, ALU.add), reads=["t1"], writes=["s4"])
                    S.op("dve", lambda e: e.tensor_scalar(s4[:], s4[:], 1.0 / 128, 1e-6, ALU.mult, ALU.add), reads=["s4"], writes=["s4"])
                    S.op("act", lambda e: e.activation(s4[:], s4[:], AF.Sqrt), reads=["s4"], writes=["s4"])
                    S.op("dve", lambda e: e.reciprocal(s4[:], s4[:]), reads=["s4"], writes=["s4"])
                    S.op("dve", lambda e: e.tensor_tensor(v3(o_f[:], 4, 128), v3(o_f[:], 4, 128), bc3(s4[:], 4, 128), ALU.mult),
                         reads=["o_f", "s4"], writes=["o_f"])
                    S.op("pool", lambda e: e.tensor_tensor(mixed[:, 512:1024], o_f[:], rg_s[:], ALU.mult),
                         reads=["o_f", "rg_s"], writes=["mixed"])

                    chk("E")
                    ps, pk = getps()
                    S.op("pe", lambda e, ps=ps: e.matmul(ps[:], lwT[:], wal[:, 0:512], start=True, stop=True),
                         reads=["lwT", "wal"], writes=[pk])
                    S.op("dve", lambda e, ps=ps: e.tensor_tensor(t0[:], ps[:], bcn["w0"][:], ALU.add),
                         reads=[pk, "bc_w0"], writes=["t0"])
                    S.op("act", lambda e: e.activation(sigw[:], t0[:], AF.Sigmoid), reads=["t0"], writes=["sigw"])
                    ps, pk = getps()
                    S.op("pe", lambda e, ps=ps: e.matmul(ps[:], lwT[:], wal[:, 512:1024], start=True, stop=True),
                         reads=["lwT", "wal"], writes=[pk])
                    S.op("dve", lambda e, ps=ps: e.tensor_tensor(t1[:], ps[:], bcn["a0"][:], ALU.add),
                         reads=[pk, "bc_a0"], writes=["t1"])
                    S.op("act", lambda e: e.activation(a_f[:], t1[:], AF.Sigmoid), reads=["t1"], writes=["a_f"])
                    ps, pk = getps()
                    S.op("pe", lambda e, ps=ps: e.matmul(ps[:], lgT[:], glu[:], start=True, stop=True),
                         reads=["lgT", "glu"], writes=[pk])
                    S.op("act", lambda e, ps=ps: e.activation(g_f[:], ps[:], AF.Copy), reads=[pk], writes=["g_f"])
                    S.op("pool", lambda e: e.tensor_copy(sighl[:, 0:512], sigw[:]), reads=["sigw"], writes=["sighl"])
                    S.op("dve", lambda e: e.tensor_tensor(sighl[:, 512:1024], sigw[:], sighl[:, 0:512], ALU.subtract),
                         reads=["sigw", "sighl"], writes=["sighl"])
                    ps, pk = getps()
                    for hl in range(2):
                        S.op("pe", lambda e, ps=ps, hl=hl: e.matmul(ps[:], tri_i[:], sighl[:, hl * 512:(hl + 1) * 512],
                                                                    start=(hl == 0), stop=(hl == 1)),
                             reads=["tri_i", "sighl"], writes=[pk])
                    S.op("act", lambda e, ps=ps: e.activation(Wi[:], ps[:], AF.Exp, scale=-C_DEC), reads=[pk], writes=["Wi"])
                    S.op("act", lambda e, ps=ps: e.activation(Wn[:], ps[:], AF.Exp, scale=C_DEC), reads=[pk], writes=["Wn"])
                    S.op("dve", lambda e, ps=ps: e.tensor_tensor(t0[:], sigw[:], ps[:], ALU.subtract),
                         reads=[pk, "sigw"], writes=["t0"])
                    S.op("act", lambda e: e.activation(We[:], t0[:], AF.Exp, scale=C_DEC), reads=["t0"], writes=["We"])
                    ps, pk = getps()
                    for hl in range(2):
                        S.op("pe", lambda e, ps=ps, hl=hl: e.matmul(ps[:], tri_r[:], sighl[:, hl * 512:(hl + 1) * 512],
                                                                    start=(hl == 0), stop=(hl == 1)),
                             reads=["tri_r", "sighl"], writes=[pk])
                    S.op("act", lambda e, ps=ps: e.activation(Wh[:], ps[:], AF.Exp, scale=-C_DEC), reads=[pk], writes=["Wh"])
                    ps, pk = getps()
                    for pr in range(4):
                        for hl in range(2):
                            S.op("pe", lambda e, pr=pr, ps=ps, hl=hl: e.matmul(
                                ps[:, pr * 2:pr * 2 + 2], sighl[:, hl * 512 + pr * 128:hl * 512 + (pr + 1) * 128],
                                seqind[:], start=(hl == 0), stop=(hl == 1)),
                                reads=["sighl", "seqind"], writes=[pk])
                    WC4 = WC[:].rearrange("p (pr par) -> p pr par", par=2)
                    for s in range(2):
                        for par in range(2):
                            S.op("act", lambda e, s=s, par=par, ps=ps: e.activation(
                                WC4[s * 64:(s + 1) * 64, :, par],
                                ps[par * 64:(par + 1) * 64, 0:8].rearrange("p (pr s) -> p pr s", s=2)[:, :, s], AF.Exp, scale=-C_DEC),
                                reads=[pk], writes=["WC"])

                    chk("F")
                    S.op("pool", lambda e: e.tensor_tensor(t1[:], k_f[:], bcn["k_k"][:], ALU.mult),
                         reads=["k_f", "bc_k_k"], writes=["t1"])
                    S.op("pool", lambda e: e.tensor_tensor(t0[:], t1[:], t1[:], ALU.mult), reads=["t1"], writes=["t0"])
                    S.op("dve", lambda e: e.tensor_reduce(s8[:], v3(t0[:], 8, 64), AX.X, ALU.add),
                         reads=["t0"], writes=["s8"])
                    S.op("dve", lambda e: e.tensor_scalar_max(s8[:], s8[:], 1e-24), reads=["s8"], writes=["s8"])
                    S.op("act", lambda e: e.activation(s8[:], s8[:], AF.Sqrt), reads=["s8"], writes=["s8"])
                    S.op("dve", lambda e: e.reciprocal(s8[:], s8[:]), reads=["s8"], writes=["s8"])
                    S.op("dve", lambda e: e.tensor_tensor(v3(kk[:], 8, 64), v3(t1[:], 8, 64), bc3(s8[:], 8, 64), ALU.mult),
                         reads=["t1", "s8"], writes=["kk"])
                    S.op("dve", lambda e: e.scalar_tensor_tensor(t0[:], a_f[:], -1.0, bcn["k_a"][:], ALU.add, ALU.mult),
                         reads=["a_f", "bc_k_a"], writes=["t0"])
                    S.op("dve", lambda e: e.scalar_tensor_tensor(k_h[:], t0[:], 1.0, k_f[:], ALU.add, ALU.mult),
                         reads=["t0", "k_f"], writes=["k_h"])
                    S.op("dve", lambda e: e.tensor_tensor(bp[:], kk[:], a_f[:], ALU.mult), reads=["kk", "a_f"], writes=["bp"])
                    S.op("dve", lambda e: e.scalar_tensor_tensor(tm4[0][:], kk[:], -1.0, We[:], ALU.mult, ALU.mult),
                         reads=["kk", "We"], writes=["tm0"])
                    S.op("pool", lambda e: e.tensor_tensor(tm4[1][:], r_f[:], Wi[:], ALU.mult), reads=["r_f", "Wi"], writes=["tm1"])
                    S.op("dve", lambda e: e.tensor_tensor(tm4[2][:], bp[:], Wn[:], ALU.mult), reads=["bp", "Wn"], writes=["tm2"])
                    S.op("pool", lambda e: e.tensor_tensor(tm4[3][:], k_h[:], Wn[:], ALU.mult), reads=["k_h", "Wn"], writes=["tm3"])
                    chk("F1")
                    for s in range(2):
                        sl_ = slice(s * 64, (s + 1) * 64)
                        S.op("dve" if s == 0 else "pool", lambda e, s=s, sl_=sl_: e.tensor_tensor(
                            Bh[sl_, :, s * 64:(s + 1) * 64], v3(bp[sl_, :], 8, 64), v3(Wh[sl_, :], 8, 64), ALU.mult),
                            reads=["bp", "Wh"], writes=["Bh"])
                        S.op("pool" if s == 0 else "dve", lambda e, s=s, sl_=sl_: e.tensor_tensor(
                            Kh[sl_, :, s * 64:(s + 1) * 64], v3(k_h[sl_, :], 8, 64), v3(Wh[sl_, :], 8, 64), ALU.mult),
                            reads=["k_h", "Wh"], writes=["Kh"])
                    chk("F2")
                    S.op("pool", lambda e: e.tensor_tensor(t0[:], r_f[:], k_h[:], ALU.mult), reads=["r_f", "k_h"], writes=["t0"])
                    S.op("pool", lambda e: e.tensor_tensor(t0[:], t0[:], bcn["r_k"][:], ALU.mult),
                         reads=["t0", "bc_r_k"], writes=["t0"])
                    S.op("dve", lambda e: e.tensor_reduce(bon8[:], v3(t0[:], 8, 64), AX.X, ALU.add),
                         reads=["t0"], writes=["bon8"])
                    chk("F3")
                    dkeys = ["arT", "arT", "bT", "kT"]
                    for qi in range(4):
                        ps, pk = getps()
                        psb = ps[:].bitcast(BF16)
                        for pr in range(4):
                            S.op("pe", lambda e, qi=qi, pr=pr, psb=psb: e.transpose(
                                psb[:, pr * 128:(pr + 1) * 128], tm4[qi][:, pr * 128:(pr + 1) * 128], ident_b[:]),
                                reads=[f"tm{qi}", "ident_b"], writes=[pk])
                        pv = psb[:, 0:512].rearrange("p (pr t) -> p pr t", pr=4)
                        if qi < 2:
                            dst = arT[:, :, qi, :]
                        elif qi == 2:
                            dst = bT[:]
                        else:
                            dst = kT[:]
                        dst5 = dst.rearrange("p (pr par) t -> p pr par t", par=2)
                        k_ = 0
                        for par in range(2 if (not _os.environ.get("MIX_NOEVAC") or str(qi) in _os.environ.get("MIX_EVACQ", "")) else 0):
                            for s in range(2):
                                d_ = dst5[s * 64:(s + 1) * 64, :, par, s * 64:(s + 1) * 64]
                                i_ = pv[par * 64:(par + 1) * 64, :, s * 64:(s + 1) * 64]
                                if k_ % 2 == 0:
                                    S.op("act", lambda e, d_=d_, i_=i_: e.activation(d_, i_, AF.Copy),
                                         reads=[pk], writes=[dkeys[qi]])
                                else:
                                    S.op("dve", lambda e, d_=d_, i_=i_: e.tensor_copy(d_, i_),
                                         reads=[pk], writes=[dkeys[qi]])
                                k_ += 1

                    chk("G")
                    for hp in range(4):
                        ps, pk = getps()
                        for hh in range(2):
                            h = hp * 2 + hh
                            S.op("pe", lambda e, h=h, hh=hh, ps=ps: e.matmul(
                                ps[:, hh * 256:(hh + 1) * 256], bT[:, h, :], arT[:, h, :, :].rearrange("p a t -> p (a t)"), start=True, stop=True),
                                reads=["bT", "arT"], writes=[pk])
                        S.op("dve", lambda e, hp=hp, ps=ps: e.tensor_tensor(
                            QA[:, hp * 2:hp * 2 + 2, :, :].rearrange("p h a t -> p h (a t)"),
                            v3(ps[:], 2, 256), bch(m1[:], 2, 256), ALU.mult),
                            reads=[pk, "m1"], writes=["QA"])
                        ps, pk = getps()
                        for hh in range(2):
                            h = hp * 2 + hh
                            S.op("pe", lambda e, h=h, hh=hh, ps=ps: e.matmul(
                                ps[:, hh * 256:(hh + 1) * 256], kT[:, h, :], arT[:, h, :, :].rearrange("p a t -> p (a t)"), start=True, stop=True),
                                reads=["kT", "arT"], writes=[pk])
                        S.op("dve", lambda e, hp=hp, ps=ps: e.tensor_tensor(
                            KA[:, hp * 2:hp * 2 + 2, :, :].rearrange("p h a t -> p h (a t)"),
                            v3(ps[:], 2, 256), bch(m1[:], 2, 256), ALU.mult),
                            reads=[pk, "m1"], writes=["KA"])
                    for hq in range(2):
                        ps, pk = getps()
                        for hh in range(4):
                            h = hq * 4 + hh
                            S.op("pe", lambda e, h=h, hh=hh, ps=ps: e.matmul(
                                ps[:, hh * 128:(hh + 1) * 128], arT[:, h, 0, :], bT[:, h, :], start=True, stop=True),
                                reads=["bT", "arT"], writes=[pk])
                        S.op("dve", lambda e, hq=hq, ps=ps: e.tensor_tensor(
                            Pm[0][:, hq * 4:hq * 4 + 4, :], v3(ps[:], 4, 128), bch(msl[:], 4, 128), ALU.mult),
                            reads=[pk, "msl"], writes=["Pm0"])
                    chk("H")
                    S.op("pool", lambda e: e.tensor_tensor(TTf[:], QA[:, :, 0, :], bch(ident_f[:], 8, 128), ALU.add),
                         reads=["QA", "ident_f"], writes=["TTf"])
                    S.op("pool", lambda e: e.tensor_copy(TTb[:], TTf[:]), reads=["TTf"], writes=["TTb"])
                    for lvl in range(1, 6):
                        pi_, po_ = (lvl - 1) % 2, lvl % 2

                        def Qprev(h, lvl=lvl, pi_=pi_):
                            return QA[:, h, 0, :] if lvl == 1 else Qm[pi_][:, h, :]
                        qprev_key = "QA" if lvl == 1 else f"Qm{pi_}"
                        for hq in range(2):
                            ps, pk = getps()
                            for hh in range(4):
                                h = hq * 4 + hh
                                S.op("pe", lambda e, h=h, hh=hh, ps=ps, Qprev=Qprev, pi_=pi_: e.matmul(
                                    ps[:, hh * 128:(hh + 1) * 128], Qprev(h), Pm[pi_][:, h, :], start=True, stop=True),
                                    reads=[qprev_key, f"Pm{pi_}"], writes=[pk])
                            S.op("act", lambda e, hq=hq, ps=ps, po_=po_: e.activation(
                                Pm[po_][:, hq * 4:hq * 4 + 4, :], v3(ps[:], 4, 128), AF.Copy),
                                reads=[pk], writes=[f"Pm{po_}"])
                        if lvl < 5:
                            for hq in range(2):
                                ps, pk = getps()
                                for hh in range(4):
                                    h = hq * 4 + hh
                                    S.op("pe", lambda e, h=h, hh=hh, ps=ps, Qprev=Qprev, pi_=pi_: e.matmul(
                                        ps[:, hh * 128:(hh + 1) * 128], Pm[pi_][:, h, :], Qprev(h), start=True, stop=True),
                                        reads=[qprev_key, f"Pm{pi_}"], writes=[pk])
                                S.op("dve", lambda e, hq=hq, ps=ps, po_=po_: e.tensor_copy(
                                    Qm[po_][:, hq * 4:hq * 4 + 4, :], v3(ps[:], 4, 128)),
                                    reads=[pk], writes=[f"Qm{po_}"])
                        pss = []
                        for hq in range(2):
                            ps, pk = getps()
                            pss.append((ps, pk))
                            for hh in range(4):
                                h = hq * 4 + hh
                                S.op("pe", lambda e, h=h, hh=hh, ps=ps, po_=po_: e.matmul(
                                    ps[:, hh * 128:(hh + 1) * 128], Pm[po_][:, h, :], TTb[:, h, :], start=True, stop=True),
                                    reads=[f"Pm{po_}", "TTb"], writes=[pk])
                        for hq in range(2):
                            ps, pk = pss[hq]
                            S.op("dve", lambda e, hq=hq, ps=ps: e.tensor_tensor(
                                TTf[:, hq * 4:hq * 4 + 4, :], TTf[:, hq * 4:hq * 4 + 4, :], v3(ps[:], 4, 128), ALU.add),
                                reads=[pk, "TTf"], writes=["TTf"])
                        S.op("pool", lambda e: e.tensor_copy(TTb[:], TTf[:]), reads=["TTf"], writes=["TTb"])

                    chk("I")
                    ps, pk = getps()
                    for h in range(8):
                        S.op("pe", lambda e, h=h, ps=ps: e.matmul(ps[:, h * 64:(h + 1) * 64], arT[:, h, 0, :], Sb[:, h, :],
                                                                  start=True, stop=False), reads=["arT", "Sb"], writes=[pk])
                        S.op("pe", lambda e, h=h, ps=ps: e.matmul(ps[:, h * 64:(h + 1) * 64], KA[:, h, 0, :],
                                                                  v_b[:, h * 64:(h + 1) * 64], start=False, stop=True),
                             reads=["KA", "v_b"], writes=[pk])
                    S.op("act", lambda e, ps=ps: e.activation(Xb[:], v3(ps[:], 8, 64), AF.Copy), reads=[pk], writes=["Xb"])
                    ps, pk = getps()
                    for h in range(8):
                        S.op("pe", lambda e, h=h, ps=ps: e.matmul(ps[:, h * 64:(h + 1) * 64], TTb[:, h, :], Xb[:, h, :],
                                                                  start=True, stop=True), reads=["TTb", "Xb"], writes=[pk])
                    S.op("dve", lambda e, ps=ps: e.tensor_copy(Ub[:], v3(ps[:], 8, 64)), reads=[pk], writes=["Ub"])
                    ps, pk = getps()
                    for h in range(8):
                        S.op("pe", lambda e, h=h, ps=ps: e.matmul(ps[:, h * 64:(h + 1) * 64], arT[:, h, 1, :], Sb[:, h, :],
                                                                  start=True, stop=False), reads=["arT", "Sb"], writes=[pk])
                        S.op("pe", lambda e, h=h, ps=ps: e.matmul(ps[:, h * 64:(h + 1) * 64], QA[:, h, 1, :], Ub[:, h, :],
                                                                  start=False, stop=False), reads=["QA", "Ub"], writes=[pk])
                        S.op("pe", lambda e, h=h, ps=ps: e.matmul(ps[:, h * 64:(h + 1) * 64], KA[:, h, 1, :],
                                                                  v_b[:, h * 64:(h + 1) * 64], start=False, stop=True),
                             reads=["KA", "v_b"], writes=[pk])
                    S.op("act", lambda e, ps=ps: e.activation(y_f[:], ps[:], AF.Copy), reads=[pk], writes=["y_f"])
                    ps, pk = getps()
                    for h in range(8):
                        S.op("pe", lambda e, h=h, ps=ps: e.matmul(ps[:, h * 64:(h + 1) * 64], Bh[:, h, :], Ub[:, h, :],
                                                                  start=True, stop=False), reads=["Bh", "Ub"], writes=[pk])
                        S.op("pe", lambda e, h=h, ps=ps: e.matmul(ps[:, h * 64:(h + 1) * 64], Kh[:, h, :],
                                                                  v_b[:, h * 64:(h + 1) * 64], start=False, stop=True),
                             reads=["Kh", "v_b"], writes=[pk])
                    S.op("dve", lambda e: e.tensor_tensor(Sf[:], Sf[:], bc3(WC[:], 8, 64), ALU.mult),
                         reads=["Sf", "WC"], writes=["Sf"])
                    S.op("dve", lambda e, ps=ps: e.tensor_tensor(Sf[:], Sf[:], v3(ps[:], 8, 64), ALU.add),
                         reads=[pk, "Sf"], writes=["Sf"])
                    S.op("pool", lambda e: e.tensor_copy(Sb[:], Sf[:]), reads=["Sf"], writes=["Sb"])

                    chk("J")
                    y3 = v3(y_f[:], 8, 64)
                    S.op("dve", lambda e: e.tensor_reduce(s8[:], y3, AX.X, ALU.add), reads=["y_f"], writes=["s8"])
                    S.op("dve", lambda e: e.tensor_scalar(s8[:], s8[:], 1.0 / 64, None, ALU.mult), reads=["s8"], writes=["s8"])
                    S.op("dve", lambda e: e.tensor_tensor(y3, y3, bc3(s8[:], 8, 64), ALU.subtract),
                         reads=["y_f", "s8"], writes=["y_f"])
                    S.op("pool", lambda e: e.tensor_tensor(t0[:], y_f[:], y_f[:], ALU.mult), reads=["y_f"], writes=["t0"])
                    S.op("dve", lambda e: e.tensor_reduce(s8[:], v3(t0[:], 8, 64), AX.X, ALU.add), reads=["t0"], writes=["s8"])
                    S.op("dve", lambda e: e.tensor_scalar(s8[:], s8[:], 1.0 / 64, 64e-5, ALU.mult, ALU.add),
                         reads=["s8"], writes=["s8"])
                    S.op("act", lambda e: e.activation(s8[:], s8[:], AF.Sqrt), reads=["s8"], writes=["s8"])
                    S.op("dve", lambda e: e.reciprocal(s8[:], s8[:]), reads=["s8"], writes=["s8"])
                    S.op("dve", lambda e: e.tensor_tensor(y3, y3, bc3(s8[:], 8, 64), ALU.mult), reads=["y_f", "s8"], writes=["y_f"])
                    S.op("pool", lambda e: e.tensor_tensor(y_f[:], y_f[:], bcn["ln_x_w"][:], ALU.mult),
                         reads=["y_f", "bc_ln_x_w"], writes=["y_f"])
                    S.op("pool", lambda e: e.tensor_tensor(y_f[:], y_f[:], bcn["ln_x_b"][:], ALU.add),
                         reads=["y_f", "bc_ln_x_b"], writes=["y_f"])
                    S.op("dve", lambda e: e.tensor_tensor(v3(t0[:], 8, 64), v3(v_b[:], 8, 64), bc3(bon8[:], 8, 64), ALU.mult),
                         reads=["v_b", "bon8"], writes=["t0"])
                    S.op("pool", lambda e: e.tensor_tensor(y_f[:], y_f[:], t0[:], ALU.add), reads=["y_f", "t0"], writes=["y_f"])
                    S.op("dve", lambda e: e.tensor_tensor(mixed[:, 0:512], y_f[:], g_f[:], ALU.mult),
                         reads=["y_f", "g_f"], writes=["mixed"])

                    chk("L")
                    ps, pk = getps()
                    psb = ps[:].bitcast(BF16)
                    for c in range(8):
                        S.op("pe", lambda e, c=c, psb=psb: e.transpose(psb[:, c * 128:(c + 1) * 128],
                                                                       mixed[:, c * 128:(c + 1) * 128], ident_b[:]),
                             reads=["mixed", "ident_b"], writes=[pk])
                    S.op("act", lambda e, psb=psb: e.activation(mT[:], psb.rearrange("p (c t) -> p c t", c=8), AF.Copy),
                         reads=[pk], writes=["mT"])
                    for nh in range(2):
                        ps, pk = getps()
                        for c in range(8):
                            S.op("pe", lambda e, c=c, nh=nh, ps=ps: e.matmul(ps[:], mT[:, c, :], wo[:, c, nh * 512:(nh + 1) * 512],
                                                                             start=(c == 0), stop=(c == 7)),
                                 reads=["mT", "wo"], writes=[pk])
                        S.op("dve", lambda e, nh=nh, ps=ps, xt=xt: e.tensor_tensor(xt[:, nh * 512:(nh + 1) * 512],
                                                                            xt[:, nh * 512:(nh + 1) * 512], ps[:], ALU.add),
                             reads=[pk, mxk], writes=[mxk])
                  except _StopTile:
                    pass
                  for s in range(2):
                        r0 = s * seq + n * 64
                        S.dma("sp", out[r0:r0 + 64, :], xt[s * 64:(s + 1) * 64, :], reads=[mxk], sem=f"ms{n % 2}")
                S.barrier()
            src_x[0] = out

        for l in range(depth):
            if do_ffn:
                ffn_phase(l, 1, False)
            if do_mix:
                mix_phase(l)
            if do_ffn:
                ffn_phase(l, 2, final_norm and l == depth - 1)
        S.emit(final_waits=[k for k in S.dma_counts if k.startswith("fs") or k.startswith("ms")])
    return nc


_CACHE = {}


def kernel(**inputs):
    x = np.ascontiguousarray(inputs["x"], dtype=np.float32)
    B, seq, d = x.shape
    depth = inputs["w_in"].shape[0]
    key = (seq, depth)
    if key not in _CACHE:
        _CACHE[key] = build_program(seq, depth)
    nc = _CACHE[key]
    consts = make_consts(seq)
    shared = {}
    for k, v in inputs.items():
        if k == "x":
            continue
        a = np.ascontiguousarray(v, dtype=np.float32)
        if k == "r_k":
            a = a.reshape(depth, 512)
        if k == "final_norm":
            a = a.reshape(1, D)
        shared[k] = a
    for k in CONST_ORDER:
        shared["c_" + k] = consts[k]
    ncores = B // 2
    in_maps = []
    for c in range(ncores):
        m = dict(shared)
        m["x"] = x[2 * c:2 * c + 2].reshape(2 * seq, d)
        in_maps.append(m)
    res = run_bass_kernel_spmd(nc, in_maps, core_ids=list(range(ncores)))
    outs = [r["out"].reshape(2, seq, d) for r in res.results]
    return np.concatenate(outs, axis=0).astype(np.float32)
```

```python
import contextlib
import numpy as np
import concourse.bass as bass
import concourse.mybir as mybir
from concourse.bass_utils import run_bass_kernel_spmd

F32 = mybir.dt.float32
BF16 = mybir.dt.bfloat16
AF = mybir.ActivationFunctionType
ALU = mybir.AluOpType
AX = mybir.AxisListType

D = 1024
DFF = 2816
NF = DFF // 128
PROJ = 3328
RW_IN = 1792
NCORES = 8
ENGS = ("pe", "act", "dve", "pool", "sp")
C_DEC = float(np.exp(-0.5))


class _StopTile(Exception):
    pass


class _Op:
    __slots__ = ("eng", "fn", "deps", "dma_deps", "idx", "needs_inc", "count",
                 "dma_sem", "epoch")


class Sched:
    def __init__(self, nc):
        self.nc = nc
        self.streams = {e: [] for e in ENGS}
        self.last_w = {}
        self.readers = {}
        self.seen = {e: {} for e in ENGS}
        self.seen_dma = {e: {} for e in ENGS}
        self.dma_counts = {}
        self.epoch = 0
        self.alias = {}

    def _new(self, eng, fn):
        o = _Op()
        o.eng = eng
        o.fn = fn
        o.deps = []
        o.dma_deps = []
        o.idx = len(self.streams[eng])
        o.needs_inc = False
        o.count = None
        o.dma_sem = None
        o.epoch = self.epoch
        return o

    def _collect(self, op, reads, writes):
        deps = []
        for k in reads:
            w = self.last_w.get(k)
            if w is not None:
                deps.append((w, "raw"))
        for k in writes:
            w = self.last_w.get(k)
            if w is not None:
                deps.append((w, "waw"))
            for r in self.readers.get(k, ()):
                deps.append((r, "war"))
        e = op.eng
        for d, kind in deps:
            if d is op:
                continue
            if d.dma_sem is not None:
                cnt = self.dma_counts[d.dma_sem]
                if self.seen_dma[e].get(d.dma_sem, 0) < cnt:
                    self.seen_dma[e][d.dma_sem] = cnt
                    op.dma_deps.append((d.dma_sem, cnt))
                continue
            if d.epoch != self.epoch:
                continue
            if d.eng == e and e == "pe":
                continue
            if self.seen[e].get(d.eng, -1) >= d.idx:
                continue
            self.seen[e][d.eng] = d.idx
            d.needs_inc = True
            op.deps.append(d)
        for k in reads:
            self.readers.setdefault(k, []).append(op)
        for k in writes:
            self.last_w[k] = op
            self.readers[k] = []

    def op(self, eng, fn, reads=(), writes=()):
        reads = [self.alias.get(k, k) for k in reads]
        writes = [self.alias.get(k, k) for k in writes]
        o = self._new(eng, fn)
        self._collect(o, reads, writes)
        self.streams[eng].append(o)
        return o

    def dma(self, queue, out, in_, reads=(), writes=(), sem="dma0"):
        def fn(eng, out=out, in_=in_):
            return eng.dma_start(out=out, in_=in_)
        sem = f"{sem}_{queue}"
        o = self.op(queue, fn, reads, writes)
        o.dma_sem = sem
        self.dma_counts[sem] = self.dma_counts.get(sem, 0) + 1
        return o

    def barrier(self):
        lasts = {}
        for e in ENGS:
            for o in reversed(self.streams[e]):
                if o.epoch != self.epoch:
                    break
                if o.dma_sem is None and o.fn is not None:
                    lasts[e] = o
                    break
        for e in ENGS:
            o = self._new(e, None)
            for e2, l in lasts.items():
                if e2 == e and e == "pe":
                    continue
                if self.seen[e].get(e2, -1) < l.idx:
                    l.needs_inc = True
                    o.deps.append(l)
            for s, c in self.dma_counts.items():
                if self.seen_dma[e].get(s, 0) < c:
                    self.seen_dma[e][s] = c
                    o.dma_deps.append((s, c))
            self.streams[e].append(o)
        self.epoch += 1
        self.seen = {e: {} for e in ENGS}

    def emit(self, final_waits=()):
        nc = self.nc
        n_epochs = self.epoch + 1
        for e in ENGS:
            c = 0
            ep = 0
            for o in self.streams[e]:
                if o.epoch != ep:
                    ep = o.epoch
                    c = 0
                if o.needs_inc:
                    c += 1
                    o.count = c
        with contextlib.ExitStack() as st:
            esem = {}
            for e in ENGS:
                used = set(o.epoch for o in self.streams[e] if o.needs_inc)
                for ep in sorted(used):
                    esem[(e, ep)] = st.enter_context(nc.semaphore(f"s_{e}_{ep}"))
            dsem = {s: st.enter_context(nc.semaphore(f"d_{s}")) for s in self.dma_counts}
            block = st.enter_context(nc.Block())

            def replay(e, eng):
                for o in self.streams[e]:
                    for d in o.deps:
                        eng.wait_ge(esem[(d.eng, d.epoch)], d.count)
                    for s, c in o.dma_deps:
                        eng.wait_ge(dsem[s], 16 * c)
                    if o.fn is None:
                        continue
                    ins = o.fn(eng)
                    if o.dma_sem is not None:
                        ins.then_inc(dsem[o.dma_sem], 16)
                    elif o.needs_inc:
                        ins.then_inc(esem[(o.eng, o.epoch)], 1)
                if e == "sp":
                    for s in final_waits:
                        eng.wait_ge(dsem[s], 16 * self.dma_counts[s])

            @block.tensor
            def _(eng):
                replay("pe", eng)

            @block.scalar
            def _(eng):
                replay("act", eng)

            @block.vector
            def _(eng):
                replay("dve", eng)

            @block.gpsimd
            def _(eng):
                replay("pool", eng)

            @block.sync
            def _(eng):
                replay("sp", eng)


def make_consts(seq):
    nt = seq // 64
    p = np.arange(128)
    s_of = p // 64
    t_of = p % 64
    same = (s_of[:, None] == s_of[None, :])
    c = {}
    c["ident"] = np.eye(128, dtype=np.float32)
    c["tri_i"] = (1.0 * (same & (t_of[:, None] <= t_of[None, :]))).astype(np.float32)
    c["tri_r"] = (1.0 * (same & (t_of[:, None] > t_of[None, :]))).astype(np.float32)
    seqind = np.zeros((128, 2), np.float32)
    seqind[p, s_of] = 1.0
    c["seqind"] = seqind
    su = (same & (t_of[:, None] < t_of[None, :])).astype(np.float32)
    ui = (same & (t_of[:, None] <= t_of[None, :])).astype(np.float32)
    sl = (same & (t_of[:, None] > t_of[None, :])).astype(np.float32)
    c["m1"] = np.concatenate([su, ui], axis=1)
    c["msl"] = sl
    H = 4
    log_g = np.log(1.0 - np.power(2.0, -5.0 - np.arange(H, dtype=np.float64)))
    j = t_of[:, None].astype(np.float64)
    i = t_of[None, :].astype(np.float64)
    dm = np.zeros((128, H, 128), np.float64)
    for h in range(H):
        dm[:, h, :] = same * np.exp(log_g[h] * (np.abs(i - j) - (i + 1.0))) * 0.125
    c["dmask"] = dm.reshape(128, H * 128).astype(np.float32)
    qw = np.exp(log_g[None, :] * (t_of[:, None] + 1.0))
    kw = np.exp(log_g[None, :] * (63.0 - t_of[:, None])) * 0.125
    cd = np.broadcast_to(np.exp(log_g * 64.0)[None, :], (128, H))
    c["qkw"] = np.concatenate([qw, kw, cd], axis=1).astype(np.float32)
    half = 32
    inv_freq = (1.0 / (np.float32(10000.0) ** np.linspace(0.0, 1.0, half, dtype=np.float32))).astype(np.float32)
    pos = np.arange(seq, dtype=np.float32)
    ang = (pos[:, None] * inv_freq[None, :]).astype(np.float32).astype(np.float64)
    cs = np.concatenate([np.cos(ang), np.sin(ang)], axis=1).astype(np.float32)
    cs = cs.reshape(nt, 64, 64).transpose(1, 0, 2)
    c["rope"] = np.ascontiguousarray(np.concatenate([cs, cs], axis=0).reshape(128, nt * 64))
    return c


CONST_ORDER = ["ident", "tri_i", "tri_r", "seqind", "m1", "msl", "dmask", "qkw", "rope"]


def build_program(seq, depth, do_ffn=True, do_mix=True, final_norm=True):
    nc = bass.Bass("TRN2", target_bir_lowering=False)
    ntok = 2 * seq
    nt = seq // 64

    def din(name, shape):
        return nc.dram_tensor(name, list(shape), F32, kind="ExternalInput").ap()

    x_in = din("x", [ntok, D])
    w = {}
    for nm, shp in [("ffn1_norm", [depth, D]), ("ffn1_w_gate", [depth, D, DFF]), ("ffn1_w_up", [depth, D, DFF]),
                    ("ffn1_w_down", [depth, DFF, D]), ("mix_norm", [depth, D]), ("w_in", [depth, D, PROJ]),
                    ("shift_mu", [depth, RW_IN]), ("w0", [depth, 512]), ("w_lora_up", [depth, 64, 512]),
                    ("a0", [depth, 512]), ("a_lora_up", [depth, 64, 512]), ("g_lora_up", [depth, 128, 512]),
                    ("k_k", [depth, 512]), ("k_a", [depth, 512]), ("r_k", [depth, 512]),
                    ("ln_x_w", [depth, 512]), ("ln_x_b", [depth, 512]), ("w_out", [depth, D, D]),
                    ("ffn2_norm", [depth, D]), ("ffn2_w_gate", [depth, D, DFF]), ("ffn2_w_up", [depth, D, DFF]),
                    ("ffn2_w_down", [depth, DFF, D]), ("final_norm", [1, D])]:
        w[nm] = din(nm, shp)
    cshape = {"ident": 128, "tri_i": 128, "tri_r": 128, "seqind": 2, "m1": 256, "msl": 128,
              "dmask": 512, "qkw": 12, "rope": nt * 64}
    cd = {k: din("c_" + k, [128, v]) for k, v in cshape.items()}
    out = nc.dram_tensor("out", [ntok, D], F32, kind="ExternalOutput").ap()

    S = Sched(nc)
    st = contextlib.ExitStack()
    with st:
        uid = [0]

        def sb(name, shape, dt=F32, stack=st):
            uid[0] += 1
            return stack.enter_context(nc.sbuf_tensor(f"{name}_{uid[0]}", list(shape), dt))

        banks = [st.enter_context(nc.psum_tensor(f"ps{i}", [128, 512], F32)) for i in range(8)]
        pctr = [0]

        def getps():
            i = pctr[0] % 8
            pctr[0] += 1
            return banks[i], f"ps{i}"

        ident_b = sb("ident_b", [128, 128], BF16)
        tri_i = sb("tri_i", [128, 128], BF16)
        tri_r = sb("tri_r", [128, 128], BF16)
        seqind = sb("seqind", [128, 2], BF16)
        m1 = sb("m1", [128, 256])
        msl = sb("msl", [128, 128])
        ident_f = sb("ident_f", [128, 128])
        dmask = sb("dmask", [128, 512])
        qkw = sb("qkw", [128, 12])
        S.dma("pool", ident_b[:], cd["ident"], writes=["ident_b"], sem="c")
        S.dma("sp", ident_f[:], cd["ident"], writes=["ident_f"], sem="c")
        S.dma("pool", tri_i[:], cd["tri_i"], writes=["tri_i"], sem="c")
        S.dma("pool", tri_r[:], cd["tri_r"], writes=["tri_r"], sem="c")
        S.dma("pool", seqind[:], cd["seqind"], writes=["seqind"], sem="c")
        S.dma("sp", m1[:], cd["m1"], writes=["m1"], sem="c")
        S.dma("sp", msl[:], cd["msl"], writes=["msl"], sem="c")
        S.dma("sp", dmask[:], cd["dmask"], writes=["dmask"], sem="c")
        S.dma("sp", qkw[:], cd["qkw"], writes=["qkw"], sem="c")

        src_x = [x_in]

        def rstd_ops(ss, n, tag):
            S.op("dve", lambda e: e.tensor_scalar(ss[:, 0:n], ss[:, 0:n], 1.0 / D, 1e-6, ALU.mult, ALU.add),
                 reads=[tag], writes=[tag])
            S.op("act", lambda e: e.activation(ss[:, 0:n], ss[:, 0:n], AF.Sqrt), reads=[tag], writes=[tag])
            S.op("dve", lambda e: e.reciprocal(ss[:, 0:n], ss[:, 0:n]), reads=[tag], writes=[tag])

        def ffn_phase(l, which, last):
            TB = 256
            nblk = ntok // TB
            with contextlib.ExitStack() as fs:
                wg = sb("wg", [128, 8, DFF], BF16, fs)
                wu = sb("wu", [128, 8, DFF], BF16, fs)
                wd = sb("wd", [128, NF, D], BF16, fs)
                gain = sb("gain", [128, D], F32, fs)
                xt = [sb(f"fx{i}", [128, 2, D], F32, fs) for i in range(2)]
                hb = sb("fh", [128, 2, D], BF16, fs)
                hT = sb("fhT", [128, 8, TB], BF16, fs)
                aT = sb("faT", [128, NF, TB], BF16, fs)
                sg = [sb(f"fsg{i}", [128, TB], F32, fs) for i in range(2)]
                junk = sb("fjunk", [128, D], BF16, fs)
                ss = [sb(f"fss{i}", [128, 4], F32, fs) for i in range(2)]
                if last:
                    fin_bc = sb("fin_bc", [128, D], F32, fs)
                    S.dma("sp", fin_bc[:], w["final_norm"][0:1, :].broadcast_to([128, D]), writes=["fin_bc"], sem="w")
                pre = "ffn1" if which == 1 else "ffn2"
                S.dma("sp", gain[:], w[pre + "_norm"][l:l + 1, :].broadcast_to([128, D]), writes=["gain"], sem="w")
                for c in range(8):
                    S.dma("pool", wg[:, c, :], w[pre + "_w_gate"][l, c * 128:(c + 1) * 128, :], writes=["wg"], sem="wgu")
                    S.dma("pool", wu[:, c, :], w[pre + "_w_up"][l, c * 128:(c + 1) * 128, :], writes=["wu"], sem="wgu")
                for f in range(NF):
                    S.dma("pool", wd[:, f, :], w[pre + "_w_down"][l, f * 128:(f + 1) * 128, :], writes=["wd"], sem="wd")

                def load(b):
                    i = b % 2
                    src = src_x[0]
                    for j in range(2):
                        r0 = b * TB + j * 128
                        S.dma("sp", xt[i][:, j, :], src[r0:r0 + 128, :], writes=[f"fx{i}"], sem=f"fl{i}")

                def norm(b):
                    i = b % 2
                    X = xt[i]
                    xk = f"fx{i}"
                    ssb = ss[i]
                    sk = f"fss{i}"
                    for j in range(2):
                        S.op("act", lambda e, j=j, X=X, ssb=ssb: e.activation(junk[:], X[:, j, :], AF.Square,
                                                                              accum_out=ssb[:, j:j + 1]),
                             reads=[xk], writes=["fjunk", sk])
                    rstd_ops(ssb, 2, sk)
                    for j in range(2):
                        S.op("dve", lambda e, j=j, X=X, ssb=ssb: e.scalar_tensor_tensor(
                            hb[:, j, :], X[:, j, :], ssb[:, j:j + 1], gain[:], ALU.mult, ALU.mult),
                            reads=[xk, sk, "gain"], writes=["fh"])

                load(0)
                norm(0)
                for b in range(nblk):
                    i = b % 2
                    X = xt[i]
                    xk = f"fx{i}"
                    if b + 1 < nblk:
                        load(b + 1)
                    ssb = ss[i]
                    sk = f"fss{i}"
                    for half in range(2):
                        ps, pk = getps()
                        psb = ps[:].bitcast(BF16)
                        for cc in range(4):
                            c = half * 4 + cc
                            for j in range(2):
                                S.op("pe", lambda e, c=c, cc=cc, j=j, psb=psb: e.transpose(
                                    psb[:, cc * 256 + j * 128: cc * 256 + (j + 1) * 128],
                                    hb[:, j, c * 128:(c + 1) * 128], ident_b[:]),
                                    reads=["fh", "ident_b"], writes=[pk])
                        eng = "act" if half == 0 else "dve"
                        if eng == "act":
                            S.op("act", lambda e, half=half, psb=psb: e.activation(
                                hT[:, half * 4:(half + 1) * 4, :], psb.rearrange("p (c t) -> p c t", c=4), AF.Copy),
                                reads=[pk], writes=["fhT"])
                        else:
                            S.op("dve", lambda e, half=half, psb=psb: e.tensor_copy(
                                hT[:, half * 4:(half + 1) * 4, :], psb.rearrange("p (c t) -> p c t", c=4)),
                                reads=[pk], writes=["fhT"])
                    for f in range(NF):
                        ps, pk = getps()
                        for c in range(8):
                            S.op("pe", lambda e, c=c, f=f, ps=ps: e.matmul(
                                ps[:, 0:TB], wg[:, c, f * 128:(f + 1) * 128], hT[:, c, :],
                                start=(c == 0), stop=(c == 7)), reads=["wg", "fhT"], writes=[pk])
                        for c in range(8):
                            S.op("pe", lambda e, c=c, f=f, ps=ps: e.matmul(
                                ps[:, TB:2 * TB], wu[:, c, f * 128:(f + 1) * 128], hT[:, c, :],
                                start=(c == 0), stop=(c == 7)), reads=["wu", "fhT"], writes=[pk])
                        sgb = sg[f % 2]
                        sgk = f"fsg{f % 2}"
                        S.op("act", lambda e, ps=ps, sgb=sgb: e.activation(sgb[:], ps[:, 0:TB], AF.Silu),
                             reads=[pk], writes=[sgk])
                        S.op("dve", lambda e, ps=ps, sgb=sgb, f=f: e.tensor_tensor(
                            aT[:, f, :], sgb[:], ps[:, TB:2 * TB], ALU.mult),
                            reads=[pk, sgk], writes=["faT"])
                    if b + 1 < nblk:
                        norm(b + 1)
                    for j in range(2):
                        for n in range(2):
                            ps, pk = getps()
                            for f in range(NF):
                                S.op("pe", lambda e, f=f, j=j, n=n, ps=ps: e.matmul(
                                    ps[:], aT[:, f, j * 128:(j + 1) * 128], wd[:, f, n * 512:(n + 1) * 512],
                                    start=(f == 0), stop=(f == NF - 1)), reads=["faT", "wd"], writes=[pk])
                            S.op("dve", lambda e, j=j, n=n, ps=ps, X=X: e.scalar_tensor_tensor(
                                X[:, j, n * 512:(n + 1) * 512], ps[:], 0.5, X[:, j, n * 512:(n + 1) * 512],
                                ALU.mult, ALU.add), reads=[pk, xk], writes=[xk])
                    if last:
                        for j in range(2):
                            S.op("act", lambda e, j=j, X=X, ssb=ssb: e.activation(
                                junk[:], X[:, j, :], AF.Square, accum_out=ssb[:, 2 + j:3 + j]),
                                reads=[xk], writes=["fjunk", sk])
                        S.op("dve", lambda e, ssb=ssb: e.tensor_scalar(ssb[:, 2:4], ssb[:, 2:4], 1.0 / D, 1e-6,
                                                                       ALU.mult, ALU.add), reads=[sk], writes=[sk])
                        S.op("act", lambda e, ssb=ssb: e.activation(ssb[:, 2:4], ssb[:, 2:4], AF.Sqrt),
                             reads=[sk], writes=[sk])
                        S.op("dve", lambda e, ssb=ssb: e.reciprocal(ssb[:, 2:4], ssb[:, 2:4]), reads=[sk], writes=[sk])
                        for j in range(2):
                            S.op("dve", lambda e, j=j, X=X, ssb=ssb: e.scalar_tensor_tensor(
                                X[:, j, :], X[:, j, :], ssb[:, 2 + j:3 + j], fin_bc[:], ALU.mult, ALU.mult),
                                reads=[xk, sk, "fin_bc"], writes=[xk])
                    for j in range(2):
                        r0 = b * TB + j * 128
                        S.dma("sp", out[r0:r0 + 128, :], X[:, j, :], reads=[xk], sem=f"fs{i}")
                S.barrier()
            src_x[0] = out

        def mix_phase(l):
            with contextlib.ExitStack() as ms:
                wm = sb("wm", [128, 8, 5120], BF16, ms)
                wo = sb("wo", [128, 8, D], BF16, ms)
                wal = sb("wal", [128, 1024], BF16, ms)
                glu = sb("glu", [128, 512], BF16, ms)
                gain = sb("mgain", [128, D], F32, ms)
                bcn = {}
                for nm in ["w0", "a0", "k_k", "k_a", "r_k", "ln_x_w", "ln_x_b"]:
                    bcn[nm] = sb("bc_" + nm, [128, 512], F32, ms)
                    S.dma("sp", bcn[nm][:], w[nm][l:l + 1, :].broadcast_to([128, 512]), writes=["bc_" + nm], sem="w")
                S.dma("sp", gain[:], w["mix_norm"][l:l + 1, :].broadcast_to([128, D]), writes=["mgain"], sem="w")
                S.op("pool", lambda e: e.memset(wal[:], 0.0), writes=["wal"])
                S.dma("pool", wal[0:64, 0:512], w["w_lora_up"][l], writes=["wal"], sem="w")
                S.dma("pool", wal[64:128, 512:1024], w["a_lora_up"][l], writes=["wal"], sem="w")
                S.dma("pool", glu[:], w["g_lora_up"][l], writes=["glu"], sem="w")
                for c in range(8):
                    S.dma("pool", wo[:, c, :], w["w_out"][l, c * 128:(c + 1) * 128, :], writes=["wo"], sem="w")
                    S.dma("pool", wm[:, c, 3584:5120], w["w_in"][l, c * 128:(c + 1) * 128, RW_IN:PROJ],
                          writes=["wm"], sem="w")
                with contextlib.ExitStack() as ps_:
                    mu = sb("mu", [128, RW_IN], F32, ps_)
                    omm = sb("omm", [128, RW_IN], F32, ps_)
                    stg = [sb(f"stg{i}", [128, RW_IN], F32, ps_) for i in range(2)]
                    S.dma("sp", mu[:], w["shift_mu"][l:l + 1, :].broadcast_to([128, RW_IN]), writes=["mu"], sem="w")
                    S.op("dve", lambda e: e.tensor_scalar(omm[:], mu[:], -1.0, 1.0, ALU.mult, ALU.add),
                         reads=["mu"], writes=["omm"])
                    for c in range(8):
                        sg_ = stg[c % 2]
                        sk_ = f"stg{c % 2}"
                        S.dma("sp", sg_[:], w["w_in"][l, c * 128:(c + 1) * 128, 0:RW_IN], writes=[sk_], sem=f"wp{c % 2}")
                        S.op("dve", lambda e, c=c, sg_=sg_: e.tensor_tensor(wm[:, c, 0:RW_IN], sg_[:], omm[:], ALU.mult),
                             reads=[sk_, "omm"], writes=["wm"])
                        S.op("pool", lambda e, c=c, sg_=sg_: e.tensor_tensor(wm[:, c, RW_IN:2 * RW_IN], sg_[:], mu[:], ALU.mult),
                             reads=[sk_, "mu"], writes=["wm"])
                    S.barrier()

                xts = [sb(f"mx{i}", [128, D], F32, ms) for i in range(2)]
                hb = sb("mh", [128, D], BF16, ms)
                junk = hb
                ss = sb("mss", [128, 2], F32, ms)
                hT = sb("mhT", [128, 8, 128], BF16, ms)
                hTp = sb("mhTp", [128, 8, 128], BF16, ms)
                carry = sb("mcarry", [128, 8, 2], BF16, ms)
                ropet = [sb(f"ropet{i}", [128, 64], F32, ms) for i in range(2)]
                G = {i: sb(f"G{i}", [128, 512], F32, ms) for i in (1, 2, 3, 4, 6, 7, 8, 9, 10)}
                G5 = sb("G5", [128, 1024], F32, ms)

                def bf3(t, h):
                    return t[:].bitcast(BF16).rearrange("p (h t) -> p h t", h=h)
                qk_f = Wi = G[1][:]
                rg_s = Wn = G[2][:]
                rot = We = G[3][:]
                o_f = Wh = G[4][:]
                Pm = [bf3(G[1], 8), bf3(G[2], 8)]
                Qm = [bf3(G[3], 8), bf3(G[4], 8)]
                ra = G5[:, 0:256]
                rb = G5[:, 256:512]
                kk = G5[:, 0:512]
                bp = G5[:, 512:1024]
                TTf = G5[:].rearrange("p (h t) -> p h t", h=8)
                sigw = G[6][:]
                TTb = bf3(G[6], 8)
                r_f = y_f = G[7][:]
                k_f = G[8][:]
                mT = bf3(G[8], 8)
                g9 = G[9][:].bitcast(BF16)
                g10 = G[10][:].bitcast(BF16)
                tm4 = [g9[:, 0:512], g9[:, 512:1024], g10[:, 0:512], g10[:, 512:1024]]
                Xb = g10[:, 0:512].rearrange("p (h v) -> p h v", h=8)
                Ub = g10[:, 512:1024].rearrange("p (h v) -> p h v", h=8)
                S.alias.update({"qk_f": "G1", "Wi": "G1", "Pm0": "G1", "rg_s": "G2", "Wn": "G2", "Pm1": "G2",
                                "rot": "G3", "We": "G3", "Qm0": "G3", "o_f": "G4", "Wh": "G4", "Qm1": "G4",
                                "ra": "G5", "rb": "G5", "kk": "G5", "bp": "G5", "TTf": "G5", "sigw": "G6", "TTb": "G6",
                                "r_f": "G7", "y_f": "G7", "k_f": "G8", "mT": "G8", "tm0": "G9", "tm1": "G9",
                                "tm2": "G10", "tm3": "G10", "Xb": "G10", "Ub": "G10", "mjunk": "mh"})
                v_b = sb("v_b", [128, 512], BF16, ms)
                lwT = sb("lwT", [128, 128], BF16, ms)
                lgT = sb("lgT", [128, 128], BF16, ms)
                rv_b = sb("rv_b", [128, 512], BF16, ms)
                a_f = sb("a_f", [128, 512], F32, ms)
                g_f = sb("g_f", [128, 512], F32, ms)
                WC = sb("WC", [128, 8], F32, ms)
                sighl = sb("sighl", [128, 1024], BF16, ms)
                t0 = sb("t0", [128, 512], F32, ms)
                t1 = sb("t1", [128, 512], F32, ms)
                k_h = sb("k_h", [128, 512], F32, ms)
                s8 = sb("s8", [128, 8], F32, ms)
                bon8 = sb("bon8", [128, 8], F32, ms)
                arT = sb("arT", [128, 8, 2, 128], BF16, ms)
                bT = sb("bT", [128, 8, 128], BF16, ms)
                kT = sb("kT", [128, 8, 128], BF16, ms)
                Bh = sb("Bh", [128, 8, 128], BF16, ms)
                Kh = sb("Kh", [128, 8, 128], BF16, ms)
                QA = sb("QA", [128, 8, 2, 128], BF16, ms)
                KA = sb("KA", [128, 8, 2, 128], BF16, ms)
                Sf = sb("Sf", [128, 8, 64], F32, ms)
                Sb = sb("Sb", [128, 8, 64], BF16, ms)
                mixed = sb("mixed", [128, D], BF16, ms)
                qt_b = sb("qt_b", [128, 256], BF16, ms)
                kp_b = sb("kp_b", [128, 256], BF16, ms)
                rKh = sb("rKh", [128, 4, 128], BF16, ms)
                rqT = sb("rqT", [128, 4, 128], BF16, ms)
                rkT = sb("rkT", [128, 4, 128], BF16, ms)
                Sc = sb("Sc", [128, 4, 128], BF16, ms)
                RSf = sb("RSf", [128, 4, 128], F32, ms)
                RSb = sb("RSb", [128, 4, 128], BF16, ms)
                s4 = sb("s4", [128, 4], F32, ms)

                for tname, tt in [("arT", arT), ("bT", bT), ("kT", kT), ("Bh", Bh), ("Kh", Kh), ("rKh", rKh),
                                  ("rqT", rqT), ("rkT", rkT), ("Sb", Sb), ("RSb", RSb)]:
                    S.op("pool", lambda e, tt=tt: e.memset(tt[:], 0.0), writes=[tname])
                S.op("dve", lambda e: e.memset(Sf[:], 0.0), writes=["Sf"])
                S.op("dve", lambda e: e.memset(RSf[:], 0.0), writes=["RSf"])
                S.op("dve", lambda e: e.memset(carry[:], 0.0), writes=["mcarry"])

                def bc3(ap2, n_in, n_out):
                    return ap2.unsqueeze(2).to_broadcast([ap2.shape[0], n_in, n_out])

                def bch(ap2, nh, ncol):
                    return ap2.unsqueeze(1).to_broadcast([ap2.shape[0], nh, ncol])

                def v3(ap2, a, b_):
                    return ap2.rearrange("p (a b) -> p a b", a=a)

                src = src_x[0]
                import os as _os
                _stop = _os.environ.get("MIX_STOP", "")

                def chk(tag):
                    if tag == _stop:
                        raise _StopTile()

                for n in range(nt):
                  try:
                    xt = xts[n % 2]
                    mxk = f"mx{n % 2}"
                    for s in range(2):
                        r0 = s * seq + n * 64
                        S.dma("sp", xt[s * 64:(s + 1) * 64, :], src[r0:r0 + 64, :], writes=[mxk], sem=f"ml{n % 2}")
                    S.op("act", lambda e, xt=xt: e.activation(junk[:], xt[:], AF.Square, accum_out=ss[:, 0:1]),
                         reads=[mxk], writes=["mjunk", "mss"])
                    rstd_ops(ss, 1, "mss")
                    S.op("dve", lambda e, xt=xt: e.scalar_tensor_tensor(hb[:], xt[:], ss[:, 0:1], gain[:], ALU.mult, ALU.mult),
                         reads=[mxk, "mss", "mgain"], writes=["mh"])
                    chk("C")
                    ps, pk = getps()
                    psb = ps[:].bitcast(BF16)
                    for c in range(8):
                        S.op("pe", lambda e, c=c, psb=psb: e.transpose(psb[:, c * 128:(c + 1) * 128],
                                                                       hb[:, c * 128:(c + 1) * 128], ident_b[:]),
                             reads=["mh", "ident_b"], writes=[pk])
                    S.op("act", lambda e, psb=psb: e.activation(
                        hT[:], psb.rearrange("p (c t) -> p c t", c=8), AF.Copy),
                        reads=[pk], writes=["mhT"])
                    hT4 = hT[:].rearrange("p c (s t) -> p c s t", s=2)
                    hTp4 = hTp[:].rearrange("p c (s t) -> p c s t", s=2)
                    S.op("pool", lambda e: e.tensor_copy(hTp4[:, :, :, 1:64], hT4[:, :, :, 0:63]),
                         reads=["mhT"], writes=["mhTp"])
                    S.op("pool", lambda e: e.tensor_copy(hTp4[:, :, :, 0], carry[:]),
                         reads=["mcarry"], writes=["mhTp"])
                    S.op("pool", lambda e: e.tensor_copy(carry[:], hT4[:, :, :, 63]),
                         reads=["mhT"], writes=["mcarry"])

                    def cur(c):
                        return hT[:, c, :]

                    def prev(c):
                        return hTp[:, c, :]

                    chk("D")
                    def proj_rw(ps_ap, col0, ncol, pk):
                        for c in range(8):
                            S.op("pe", lambda e, c=c: e.matmul(ps_ap, cur(c), wm[:, c, col0:col0 + ncol],
                                                               start=(c == 0), stop=False),
                                 reads=["mhT", "wm"], writes=[pk])
                        for c in range(8):
                            S.op("pe", lambda e, c=c: e.matmul(ps_ap, prev(c), wm[:, c, RW_IN + col0:RW_IN + col0 + ncol],
                                                               start=False, stop=(c == 7)),
                                 reads=["mhTp", "wm"], writes=[pk])

                    ps, pk = getps()
                    proj_rw(ps[:], 0, 512, pk)
                    S.op("act", lambda e, ps=ps: e.activation(r_f[:], ps[:], AF.Copy), reads=[pk], writes=["r_f"])
                    ps, pk = getps()
                    proj_rw(ps[:], 512, 512, pk)
                    S.op("dve", lambda e, ps=ps: e.tensor_copy(k_f[:], ps[:]), reads=[pk], writes=["k_f"])
                    ps, pk = getps()
                    proj_rw(ps[:], 1024, 512, pk)
                    S.op("dve", lambda e, ps=ps: e.tensor_copy(v_b[:], ps[:]), reads=[pk], writes=["v_b"])
                    ps, pk = getps()
                    for gi in range(2):
                        col0 = 1536 + gi * 128
                        for c in range(8):
                            S.op("pe", lambda e, c=c, gi=gi, col0=col0, ps=ps: e.matmul(
                                ps[:, gi * 128:(gi + 1) * 128], wm[:, c, col0:col0 + 128], cur(c),
                                start=(c == 0), stop=False), reads=["mhT", "wm"], writes=[pk])
                        for c in range(8):
                            S.op("pe", lambda e, c=c, gi=gi, col0=col0, ps=ps: e.matmul(
                                ps[:, gi * 128:(gi + 1) * 128], wm[:, c, RW_IN + col0:RW_IN + col0 + 128], prev(c),
                                start=False, stop=(c == 7)), reads=["mhTp", "wm"], writes=[pk])
                    S.op("act", lambda e, ps=ps: e.activation(lwT[0:64, :], ps[0:64, 0:128], AF.Tanh), reads=[pk], writes=["lwT"])
                    S.op("dve", lambda e, ps=ps: e.tensor_copy(lwT[64:128, :], ps[64:128, 0:128]), reads=[pk], writes=["lwT"])
                    S.op("act", lambda e, ps=ps: e.activation(lgT[:], ps[:, 128:256], AF.Sigmoid), reads=[pk], writes=["lgT"])
                    ps, pk = getps()
                    for c in range(8):
                        S.op("pe", lambda e, c=c, ps=ps: e.matmul(ps[:], cur(c), wm[:, c, 3584:4096],
                                                                  start=(c == 0), stop=(c == 7)),
                             reads=["mhT", "wm"], writes=[pk])
                    S.op("dve", lambda e, ps=ps: e.tensor_copy(qk_f[:], ps[:]), reads=[pk], writes=["qk_f"])
                    ps, pk = getps()
                    for c in range(8):
                        S.op("pe", lambda e, c=c, ps=ps: e.matmul(ps[:], cur(c), wm[:, c, 4096:4608],
                                                                  start=(c == 0), stop=(c == 7)),
                             reads=["mhT", "wm"], writes=[pk])
                    S.op("act", lambda e, ps=ps: e.activation(rv_b[:], ps[:], AF.Copy), reads=[pk], writes=["rv_b"])
                    ps, pk = getps()
                    for c in range(8):
                        S.op("pe", lambda e, c=c, ps=ps: e.matmul(ps[:], cur(c), wm[:, c, 4608:5120],
                                                                  start=(c == 0), stop=(c == 7)),
                             reads=["mhT", "wm"], writes=[pk])
                    S.op("act", lambda e, ps=ps: e.activation(rg_s[:], ps[:], AF.Silu), reads=[pk], writes=["rg_s"])

                    chk("K")
                    cs_ = ropet[n % 2]
                    ropek = f"ropet{n % 2}"
                    S.dma("sp", cs_[:], cd["rope"][:, n * 64:(n + 1) * 64], writes=[ropek], sem=f"rp{n % 2}")
                    cosb = cs_[:, 0:32].unsqueeze(1).to_broadcast([128, 8, 32])
                    sinb = cs_[:, 32:64].unsqueeze(1).to_broadcast([128, 8, 32])
                    qk4 = qk_f[:].rearrange("p (g a d) -> p g a d", g=8, a=2)
                    rot4 = rot[:].rearrange("p (g a d) -> p g a d", g=8, a=2)
                    ra3 = v3(ra[:], 8, 32)
                    rb3 = v3(rb[:], 8, 32)
                    S.op("dve", lambda e, cosb=cosb, sinb=sinb: e.tensor_tensor(ra3, qk4[:, :, 0, :], cosb, ALU.mult), reads=["qk_f", ropek], writes=["ra"])
                    S.op("pool", lambda e, cosb=cosb, sinb=sinb: e.tensor_tensor(rb3, qk4[:, :, 1, :], sinb, ALU.mult), reads=["qk_f", ropek], writes=["rb"])
                    S.op("dve", lambda e: e.tensor_tensor(rot4[:, :, 0, :], ra3, rb3, ALU.subtract), reads=["ra", "rb"], writes=["rot"])
                    S.op("dve", lambda e, cosb=cosb, sinb=sinb: e.tensor_tensor(ra3, qk4[:, :, 0, :], sinb, ALU.mult), reads=["qk_f", ropek], writes=["ra"])
                    S.op("pool", lambda e, cosb=cosb, sinb=sinb: e.tensor_tensor(rb3, qk4[:, :, 1, :], cosb, ALU.mult), reads=["qk_f", ropek], writes=["rb"])
                    S.op("dve", lambda e: e.tensor_tensor(rot4[:, :, 1, :], ra3, rb3, ALU.add), reads=["ra", "rb"], writes=["rot"])
                    S.op("dve", lambda e: e.tensor_tensor(v3(qt_b[:], 4, 64), v3(rot[:, 0:256], 4, 64), bc3(qkw[:, 0:4], 4, 64), ALU.mult),
                         reads=["rot", "qkw"], writes=["qt_b"])
                    S.op("pool", lambda e: e.tensor_copy(kp_b[:], rot[:, 256:512]), reads=["rot"], writes=["kp_b"])
                    for s in range(2):
                        sl_ = slice(s * 64, (s + 1) * 64)
                        S.op("dve" if s == 0 else "pool", lambda e, s=s, sl_=sl_: e.tensor_tensor(
                            rKh[sl_, :, s * 64:(s + 1) * 64], v3(rot[sl_, 256:512], 4, 64), bc3(qkw[sl_, 4:8], 4, 64), ALU.mult),
                            reads=["rot", "qkw"], writes=["rKh"])
                    chk("K1")
                    ps, pk = getps()
                    psb = ps[:].bitcast(BF16)
                    for pr in range(2):
                        S.op("pe", lambda e, pr=pr, psb=psb: e.transpose(psb[:, pr * 128:(pr + 1) * 128],
                                                                         qt_b[:, pr * 128:(pr + 1) * 128], ident_b[:]),
                             reads=["qt_b", "ident_b"], writes=[pk])
                        S.op("pe", lambda e, pr=pr, psb=psb: e.transpose(psb[:, (2 + pr) * 128:(3 + pr) * 128],
                                                                         kp_b[:, pr * 128:(pr + 1) * 128], ident_b[:]),
                             reads=["kp_b", "ident_b"], writes=[pk])
                    pv = psb[:, 0:512].rearrange("p (g t) -> p g t", g=4)
                    k_ = 0
                    for gi, (dst, dk) in enumerate([(rqT, "rqT"), (rkT, "rkT")]):
                        dst5 = dst[:].rearrange("p (pr par) t -> p pr par t", par=2)
                        for par in range(2):
                            for s in range(2):
                                d_ = dst5[s * 64:(s + 1) * 64, :, par, s * 64:(s + 1) * 64]
                                i_ = pv[par * 64:(par + 1) * 64, gi * 2:gi * 2 + 2, s * 64:(s + 1) * 64]
                                if k_ % 2 == 0:
                                    S.op("act", lambda e, d_=d_, i_=i_: e.activation(d_, i_, AF.Copy), reads=[pk], writes=[dk])
                                else:
                                    S.op("dve", lambda e, d_=d_, i_=i_: e.tensor_copy(d_, i_), reads=[pk], writes=[dk])
                                k_ += 1
                    chk("K2")
                    ps, pk = getps()
                    for h in range(4):
                        S.op("pe", lambda e, h=h, ps=ps: e.matmul(ps[:, h * 128:(h + 1) * 128], rkT[:, h, :], rqT[:, h, :],
                                                                  start=True, stop=True), reads=["rkT", "rqT"], writes=[pk])
                    S.op("dve", lambda e, ps=ps: e.tensor_tensor(Sc[:], v3(ps[:], 4, 128), v3(dmask[:], 4, 128), ALU.mult),
                         reads=[pk, "dmask"], writes=["Sc"])
                    ps, pk = getps()
                    for h in range(4):
                        S.op("pe", lambda e, h=h, ps=ps: e.matmul(ps[:, h * 128:(h + 1) * 128], Sc[:, h, :],
                                                                  rv_b[:, h * 128:(h + 1) * 128], start=True, stop=False),
                             reads=["Sc", "rv_b"], writes=[pk])
                        S.op("pe", lambda e, h=h, ps=ps: e.matmul(ps[:, h * 128:(h + 1) * 128], rqT[:, h, :], RSb[:, h, :],
                                                                  start=False, stop=True), reads=["rqT", "RSb"], writes=[pk])
                    S.op("act", lambda e, ps=ps: e.activation(o_f[:], ps[:], AF.Copy), reads=[pk], writes=["o_f"])
                    ps, pk = getps()
                    for h in range(4):
                        S.op("pe", lambda e, h=h, ps=ps: e.matmul(ps[:, h * 128:(h + 1) * 128], rKh[:, h, :],
                                                                  rv_b[:, h * 128:(h + 1) * 128], start=True, stop=True),
                             reads=["rKh", "rv_b"], writes=[pk])
                    S.op("dve", lambda e: e.tensor_tensor(RSf[:], RSf[:], bc3(qkw[:, 8:12], 4, 128), ALU.mult),
                         reads=["RSf", "qkw"], writes=["RSf"])
                    S.op("dve", lambda e, ps=ps: e.tensor_tensor(RSf[:], RSf[:], v3(ps[:], 4, 128), ALU.add),
                         reads=[pk, "RSf"], writes=["RSf"])
                    S.op("pool", lambda e: e.tensor_copy(RSb[:], RSf[:]), reads=["RSf"], writes=["RSb"])
                    S.op("pool", lambda e: e.tensor_tensor(t1[:], o_f[:], o_f[:], ALU.mult), reads=["o_f"], writes=["t1"])
                    S.op("dve", lambda e: e.tensor_reduce(s4[:], v3(t1[:], 4, 128), AX.X, ALU.add), reads=["t1"], writes=["s4"])
                    S.op("dve", lambda e: e.tensor_scalar(s4[:], s4[:], 1.0 / 128, 1e-6, ALU.mult, ALU.add), reads=["s4"], writes=["s4"])
                    S.op("act", lambda e: e.activation(s4[:], s4[:], AF.Sqrt), reads=["s4"], writes=["s4"])
                    S.op("dve", lambda e: e.reciprocal(s4[:], s4[:]), reads=["s4"], writes=["s4"])
                    S.op("dve", lambda e: e.tensor_tensor(v3(o_f[:], 4, 128), v3(o_f[:], 4, 128), bc3(s4[:], 4, 128), ALU.mult),
                         reads=["o_f", "s4"], writes=["o_f"])
                    S.op("pool", lambda e: e.tensor_tensor(mixed[:, 512:1024], o_f[:], rg_s[:], ALU.mult),
                         reads=["o_f", "rg_s"], writes=["mixed"])

                    chk("E")
                    ps, pk = getps()
                    S.op("pe", lambda e, ps=ps: e.matmul(ps[:], lwT[:], wal[:, 0:512], start=True, stop=True),
                         reads=["lwT", "wal"], writes=[pk])
                    S.op("dve", lambda e, ps=ps: e.tensor_tensor(t0[:], ps[:], bcn["w0"][:], ALU.add),
                         reads=[pk, "bc_w0"], writes=["t0"])
                    S.op("act", lambda e: e.activation(sigw[:], t0[:], AF.Sigmoid), reads=["t0"], writes=["sigw"])
                    ps, pk = getps()
                    S.op("pe", lambda e, ps=ps: e.matmul(ps[:], lwT[:], wal[:, 512:1024], start=True, stop=True),
                         reads=["lwT", "wal"], writes=[pk])
                    S.op("dve", lambda e, ps=ps: e.tensor_tensor(t1[:], ps[:], bcn["a0"][:], ALU.add),
                         reads=[pk, "bc_a0"], writes=["t1"])
                    S.op("act", lambda e: e.activation(a_f[:], t1[:], AF.Sigmoid), reads=["t1"], writes=["a_f"])
                    ps, pk = getps()
                    S.op("pe", lambda e, ps=ps: e.matmul(ps[:], lgT[:], glu[:], start=True, stop=True),
                         reads=["lgT", "glu"], writes=[pk])
                    S.op("act", lambda e, ps=ps: e.activation(g_f[:], ps[:], AF.Copy), reads=[pk], writes=["g_f"])
                    S.op("pool", lambda e: e.tensor_copy(sighl[:, 0:512], sigw[:]), reads=["sigw"], writes=["sighl"])
                    S.op("dve", lambda e: e.tensor_tensor(sighl[:, 512:1024], sigw[:], sighl[:, 0:512], ALU.subtract),
                         reads=["sigw", "sighl"], writes=["sighl"])
                    ps, pk = getps()
                    for hl in range(2):
                        S.op("pe", lambda e, ps=ps, hl=hl: e.matmul(ps[:], tri_i[:], sighl[:, hl * 512:(hl + 1) * 512],
                                                                    start=(hl == 0), stop=(hl == 1)),
                             reads=["tri_i", "sighl"], writes=[pk])
                    S.op("act", lambda e, ps=ps: e.activation(Wi[:], ps[:], AF.Exp, scale=-C_DEC), reads=[pk], writes=["Wi"])
                    S.op("act", lambda e, ps=ps: e.activation(Wn[:], ps[:], AF.Exp, scale=C_DEC), reads=[pk], writes=["Wn"])
                    S.op("dve", lambda e, ps=ps: e.tensor_tensor(t0[:], sigw[:], ps[:], ALU.subtract),
                         reads=[pk, "sigw"], writes=["t0"])
                    S.op("act", lambda e: e.activation(We[:], t0[:], AF.Exp, scale=C_DEC), reads=["t0"], writes=["We"])
                    ps, pk = getps()
                    for hl in range(2):
                        S.op("pe", lambda e, ps=ps, hl=hl: e.matmul(ps[:], tri_r[:], sighl[:, hl * 512:(hl + 1) * 512],
                                                                    start=(hl == 0), stop=(hl == 1)),
                             reads=["tri_r", "sighl"], writes=[pk])
                    S.op("act", lambda e, ps=ps: e.activation(Wh[:], ps[:], AF.Exp, scale=-C_DEC), reads=[pk], writes=["Wh"])
                    ps, pk = getps()
                    for pr in range(4):
                        for hl in range(2):
                            S.op("pe", lambda e, pr=pr, ps=ps, hl=hl: e.matmul(
                                ps[:, pr * 2:pr * 2 + 2], sighl[:, hl * 512 + pr * 128:hl * 512 + (pr + 1) * 128],
                                seqind[:], start=(hl == 0), stop=(hl == 1)),
                                reads=["sighl", "seqind"], writes=[pk])
                    WC4 = WC[:].rearrange("p (pr par) -> p pr par", par=2)
                    for s in range(2):
                        for par in range(2):
                            S.op("act", lambda e, s=s, par=par, ps=ps: e.activation(
                                WC4[s * 64:(s + 1) * 64, :, par],
                                ps[par * 64:(par + 1) * 64, 0:8].rearrange("p (pr s) -> p pr s", s=2)[:, :, s], AF.Exp, scale=-C_DEC),
                                reads=[pk], writes=["WC"])

                    chk("F")
                    S.op("pool", lambda e: e.tensor_tensor(t1[:], k_f[:], bcn["k_k"][:], ALU.mult),
                         reads=["k_f", "bc_k_k"], writes=["t1"])
                    S.op("pool", lambda e: e.tensor_tensor(t0[:], t1[:], t1[:], ALU.mult), reads=["t1"], writes=["t0"])
                    S.op("dve", lambda e: e.tensor_reduce(s8[:], v3(t0[:], 8, 64), AX.X, ALU.add),
                         reads=["t0"], writes=["s8"])
                    S.op("dve", lambda e: e.tensor_scalar_max(s8[:], s8[:], 1e-24), reads=["s8"], writes=["s8"])
                    S.op("act", lambda e: e.activation(s8[:], s8[:], AF.Sqrt), reads=["s8"], writes=["s8"])
                    S.op("dve", lambda e: e.reciprocal(s8[:], s8[:]), reads=["s8"], writes=["s8"])
                    S.op("dve", lambda e: e.tensor_tensor(v3(kk[:], 8, 64), v3(t1[:], 8, 64), bc3(s8[:], 8, 64), ALU.mult),
                         reads=["t1", "s8"], writes=["kk"])
                    S.op("dve", lambda e: e.scalar_tensor_tensor(t0[:], a_f[:], -1.0, bcn["k_a"][:], ALU.add, ALU.mult),
                         reads=["a_f", "bc_k_a"], writes=["t0"])
                    S.op("dve", lambda e: e.scalar_tensor_tensor(k_h[:], t0[:], 1.0, k_f[:], ALU.add, ALU.mult),
                         reads=["t0", "k_f"], writes=["k_h"])
                    S.op("dve", lambda e: e.tensor_tensor(bp[:], kk[:], a_f[:], ALU.mult), reads=["kk", "a_f"], writes=["bp"])
                    S.op("dve", lambda e: e.scalar_tensor_tensor(tm4[0][:], kk[:], -1.0, We[:], ALU.mult, ALU.mult),
                         reads=["kk", "We"], writes=["tm0"])
                    S.op("pool", lambda e: e.tensor_tensor(tm4[1][:], r_f[:], Wi[:], ALU.mult), reads=["r_f", "Wi"], writes=["tm1"])
                    S.op("dve", lambda e: e.tensor_tensor(tm4[2][:], bp[:], Wn[:], ALU.mult), reads=["bp", "Wn"], writes=["tm2"])
                    S.op("pool", lambda e: e.tensor_tensor(tm4[3][:], k_h[:], Wn[:], ALU.mult), reads=["k_h", "Wn"], writes=["tm3"])
                    chk("F1")
                    for s in range(2):
                        sl_ = slice(s * 64, (s + 1) * 64)
                        S.op("dve" if s == 0 else "pool", lambda e, s=s, sl_=sl_: e.tensor_tensor(
                            Bh[sl_, :, s * 64:(s + 1) * 64], v3(bp[sl_, :], 8, 64), v3(Wh[sl_, :], 8, 64), ALU.mult),
                            reads=["bp", "Wh"], writes=["Bh"])
                        S.op("pool" if s == 0 else "dve", lambda e, s=s, sl_=sl_: e.tensor_tensor(
                            Kh[sl_, :, s * 64:(s + 1) * 64], v3(k_h[sl_, :], 8, 64), v3(Wh[sl_, :], 8, 64), ALU.mult),
                            reads=["k_h", "Wh"], writes=["Kh"])
                    chk("F2")
                    S.op("pool", lambda e: e.tensor_tensor(t0[:], r_f[:], k_h[:], ALU.mult), reads=["r_f", "k_h"], writes=["t0"])
                    S.op("pool", lambda e: e.tensor_tensor(t0[:], t0[:], bcn["r_k"][:], ALU.mult),
                         reads=["t0", "bc_r_k"], writes=["t0"])
                    S.op("dve", lambda e: e.tensor_reduce(bon8[:], v3(t0[:], 8, 64), AX.X, ALU.add),
                         reads=["t0"], writes=["bon8"])
                    chk("F3")
                    dkeys = ["arT", "arT", "bT", "kT"]
                    for qi in range(4):
                        ps, pk = getps()
                        psb = ps[:].bitcast(BF16)
                        for pr in range(4):
                            S.op("pe", lambda e, qi=qi, pr=pr, psb=psb: e.transpose(
                                psb[:, pr * 128:(pr + 1) * 128], tm4[qi][:, pr * 128:(pr + 1) * 128], ident_b[:]),
                                reads=[f"tm{qi}", "ident_b"], writes=[pk])
                        pv = psb[:, 0:512].rearrange("p (pr t) -> p pr t", pr=4)
                        if qi < 2:
                            dst = arT[:, :, qi, :]
                        elif qi == 2:
                            dst = bT[:]
                        else:
                            dst = kT[:]
                        dst5 = dst.rearrange("p (pr par) t -> p pr par t", par=2)
                        k_ = 0
                        for par in range(2 if (not _os.environ.get("MIX_NOEVAC") or str(qi) in _os.environ.get("MIX_EVACQ", "")) else 0):
                            for s in range(2):
                                d_ = dst5[s * 64:(s + 1) * 64, :, par, s * 64:(s + 1) * 64]
                                i_ = pv[par * 64:(par + 1) * 64, :, s * 64:(s + 1) * 64]
                                if k_ % 2 == 0:
                                    S.op("act", lambda e, d_=d_, i_=i_: e.activation(d_, i_, AF.Copy),
                                         reads=[pk], writes=[dkeys[qi]])
                                else:
                                    S.op("dve", lambda e, d_=d_, i_=i_: e.tensor_copy(d_, i_),
                                         reads=[pk], writes=[dkeys[qi]])
                                k_ += 1

                    chk("G")
                    for hp in range(4):
                        ps, pk = getps()
                        for hh in range(2):
                            h = hp * 2 + hh
                            S.op("pe", lambda e, h=h, hh=hh, ps=ps: e.matmul(
                                ps[:, hh * 256:(hh + 1) * 256], bT[:, h, :], arT[:, h, :, :].rearrange("p a t -> p (a t)"), start=True, stop=True),
                                reads=["bT", "arT"], writes=[pk])
                        S.op("dve", lambda e, hp=hp, ps=ps: e.tensor_tensor(
                            QA[:, hp * 2:hp * 2 + 2, :, :].rearrange("p h a t -> p h (a t)"),
                            v3(ps[:], 2, 256), bch(m1[:], 2, 256), ALU.mult),
                            reads=[pk, "m1"], writes=["QA"])
                        ps, pk = getps()
                        for hh in range(2):
                            h = hp * 2 + hh
                            S.op("pe", lambda e, h=h, hh=hh, ps=ps: e.matmul(
                                ps[:, hh * 256:(hh + 1) * 256], kT[:, h, :], arT[:, h, :, :].rearrange("p a t -> p (a t)"), start=True, stop=True),
                                reads=["kT", "arT"], writes=[pk])
                        S.op("dve", lambda e, hp=hp, ps=ps: e.tensor_tensor(
                            KA[:, hp * 2:hp * 2 + 2, :, :].rearrange("p h a t -> p h (a t)"),
                            v3(ps[:], 2, 256), bch(m1[:], 2, 256), ALU.mult),
                            reads=[pk, "m1"], writes=["KA"])
                    for hq in range(2):
                        ps, pk = getps()
                        for hh in range(4):
                            h = hq * 4 + hh
                            S.op("pe", lambda e, h=h, hh=hh, ps=ps: e.matmul(
                                ps[:, hh * 128:(hh + 1) * 128], arT[:, h, 0, :], bT[:, h, :], start=True, stop=True),
                                reads=["bT", "arT"], writes=[pk])
                        S.op("dve", lambda e, hq=hq, ps=ps: e.tensor_tensor(
                            Pm[0][:, hq * 4:hq * 4 + 4, :], v3(ps[:], 4, 128), bch(msl[:], 4, 128), ALU.mult),
                            reads=[pk, "msl"], writes=["Pm0"])
                    chk("H")
                    S.op("pool", lambda e: e.tensor_tensor(TTf[:], QA[:, :, 0, :], bch(ident_f[:], 8, 128), ALU.add),
                         reads=["QA", "ident_f"], writes=["TTf"])
                    S.op("pool", lambda e: e.tensor_copy(TTb[:], TTf[:]), reads=["TTf"], writes=["TTb"])
                    for lvl in range(1, 6):
                        pi_, po_ = (lvl - 1) % 2, lvl % 2

                        def Qprev(h, lvl=lvl, pi_=pi_):
                            return QA[:, h, 0, :] if lvl == 1 else Qm[pi_][:, h, :]
                        qprev_key = "QA" if lvl == 1 else f"Qm{pi_}"
                        for hq in range(2):
                            ps, pk = getps()
                            for hh in range(4):
                                h = hq * 4 + hh
                                S.op("pe", lambda e, h=h, hh=hh, ps=ps, Qprev=Qprev, pi_=pi_: e.matmul(
                                    ps[:, hh * 128:(hh + 1) * 128], Qprev(h), Pm[pi_][:, h, :], start=True, stop=True),
                                    reads=[qprev_key, f"Pm{pi_}"], writes=[pk])
                            S.op("act", lambda e, hq=hq, ps=ps, po_=po_: e.activation(
                                Pm[po_][:, hq * 4:hq * 4 + 4, :], v3(ps[:], 4, 128), AF.Copy),
                                reads=[pk], writes=[f"Pm{po_}"])
                        if lvl < 5:
                            for hq in range(2):
                                ps, pk = getps()
                                for hh in range(4):
                                    h = hq * 4 + hh
                                    S.op("pe", lambda e, h=h, hh=hh, ps=ps, Qprev=Qprev, pi_=pi_: e.matmul(
                                        ps[:, hh * 128:(hh + 1) * 128], Pm[pi_][:, h, :], Qprev(h), start=True, stop=True),
                                        reads=[qprev_key, f"Pm{pi_}"], writes=[pk])
                                S.op("dve", lambda e, hq=hq, ps=ps, po_=po_: e.tensor_copy(
                                    Qm[po_][:, hq * 4:hq * 4 + 4, :], v3(ps[:], 4, 128)),
                                    reads=[pk], writes=[f"Qm{po_}"])
                        pss = []
                        for hq in range(2):
                            ps, pk = getps()
                            pss.append((ps, pk))
                            for hh in range(4):
                                h = hq * 4 + hh
                                S.op("pe", lambda e, h=h, hh=hh, ps=ps, po_=po_: e.matmul(
                                    ps[:, hh * 128:(hh + 1) * 128], Pm[po_][:, h, :], TTb[:, h, :], start=True, stop=True),
                                    reads=[f"Pm{po_}", "TTb"], writes=[pk])
                        for hq in range(2):
                            ps, pk = pss[hq]
                            S.op("dve", lambda e, hq=hq, ps=ps: e.tensor_tensor(
                                TTf[:, hq * 4:hq * 4 + 4, :], TTf[:, hq * 4:hq * 4 + 4, :], v3(ps[:], 4, 128), ALU.add),
                                reads=[pk, "TTf"], writes=["TTf"])
                        S.op("pool", lambda e: e.tensor_copy(TTb[:], TTf[:]), reads=["TTf"], writes=["TTb"])

                    chk("I")
                    ps, pk = getps()
                    for h in range(8):
                        S.op("pe", lambda e, h=h, ps=ps: e.matmul(ps[:, h * 64:(h + 1) * 64], arT[:, h, 0, :], Sb[:, h, :],
                                                                  start=True, stop=False), reads=["arT", "Sb"], writes=[pk])
                        S.op("pe", lambda e, h=h, ps=ps: e.matmul(ps[:, h * 64:(h + 1) * 64], KA[:, h, 0, :],
                                                                  v_b[:, h * 64:(h + 1) * 64], start=False, stop=True),
                             reads=["KA", "v_b"], writes=[pk])
                    S.op("act", lambda e, ps=ps: e.activation(Xb[:], v3(ps[:], 8, 64), AF.Copy), reads=[pk], writes=["Xb"])
                    ps, pk = getps()
                    for h in range(8):
                        S.op("pe", lambda e, h=h, ps=ps: e.matmul(ps[:, h * 64:(h + 1) * 64], TTb[:, h, :], Xb[:, h, :],
                                                                  start=True, stop=True), reads=["TTb", "Xb"], writes=[pk])
                    S.op("dve", lambda e, ps=ps: e.tensor_copy(Ub[:], v3(ps[:], 8, 64)), reads=[pk], writes=["Ub"])
                    ps, pk = getps()
                    for h in range(8):
                        S.op("pe", lambda e, h=h, ps=ps: e.matmul(ps[:, h * 64:(h + 1) * 64], arT[:, h, 1, :], Sb[:, h, :],
                                                                  start=True, stop=False), reads=["arT", "Sb"], writes=[pk])
                        S.op("pe", lambda e, h=h, ps=ps: e.matmul(ps[:, h * 64:(h + 1) * 64], QA[:, h, 1, :], Ub[:, h, :],
                                                                  start=False, stop=False), reads=["QA", "Ub"], writes=[pk])
                        S.op("pe", lambda e, h=h, ps=ps: e.matmul(ps[:, h * 64:(h + 1) * 64], KA[:, h, 1, :],
                                                                  v_b[:, h * 64:(h + 1) * 64], start=False, stop=True),
                             reads=["KA", "v_b"], writes=[pk])
                    S.op("act", lambda e, ps=ps: e.activation(y_f[:], ps[:], AF.Copy), reads=[pk], writes=["y_f"])
                    ps, pk = getps()
                    for h in range(8):
                        S.op("pe", lambda e, h=h, ps=ps: e.matmul(ps[:, h * 64:(h + 1) * 64], Bh[:, h, :], Ub[:, h, :],
                                                                  start=True, stop=False), reads=["Bh", "Ub"], writes=[pk])
                        S.op("pe", lambda e, h=h, ps=ps: e.matmul(ps[:, h * 64:(h + 1) * 64], Kh[:, h, :],
                                                                  v_b[:, h * 64:(h + 1) * 64], start=False, stop=True),
                             reads=["Kh", "v_b"], writes=[pk])
                    S.op("dve", lambda e: e.tensor_tensor(Sf[:], Sf[:], bc3(WC[:], 8, 64), ALU.mult),
                         reads=["Sf", "WC"], writes=["Sf"])
                    S.op("dve", lambda e, ps=ps: e.tensor_tensor(Sf[:], Sf[:], v3(ps[:], 8, 64), ALU.add),
                         reads=[pk, "Sf"], writes=["Sf"])
                    S.op("pool", lambda e: e.tensor_copy(Sb[:], Sf[:]), reads=["Sf"], writes=["Sb"])

                    chk("J")
                    y3 = v3(y_f[:], 8, 64)
                    S.op("dve", lambda e: e.tensor_reduce(s8[:], y3, AX.X, ALU.add), reads=["y_f"], writes=["s8"])
                    S.op("dve", lambda e: e.tensor_scalar(s8[:], s8[:], 1.0 / 64, None, ALU.mult), reads=["s8"], writes=["s8"])
                    S.op("dve", lambda e: e.tensor_tensor(y3, y3, bc3(s8[:], 8, 64), ALU.subtract),
                         reads=["y_f", "s8"], writes=["y_f"])
                    S.op("pool", lambda e: e.tensor_tensor(t0[:], y_f[:], y_f[:], ALU.mult), reads=["y_f"], writes=["t0"])
                    S.op("dve", lambda e: e.tensor_reduce(s8[:], v3(t0[:], 8, 64), AX.X, ALU.add), reads=["t0"], writes=["s8"])
                    S.op("dve", lambda e: e.tensor_scalar(s8[:], s8[:], 1.0 / 64, 64e-5, ALU.mult, ALU.add),
                         reads=["s8"], writes=["s8"])
                    S.op("act", lambda e: e.activation(s8[:], s8[:], AF.Sqrt), reads=["s8"], writes=["s8"])
                    S.op("dve", lambda e: e.reciprocal(s8[:], s8[:]), reads=["s8"], writes=["s8"])
                    S.op("dve", lambda e: e.tensor_tensor(y3, y3, bc3(s8[:], 8, 64), ALU.mult), reads=["y_f", "s8"], writes=["y_f"])
                    S.op("pool", lambda e: e.tensor_tensor(y_f[:], y_f[:], bcn["ln_x_w"][:], ALU.mult),
                         reads=["y_f", "bc_ln_x_w"], writes=["y_f"])
                    S.op("pool", lambda e: e.tensor_tensor(y_f[:], y_f[:], bcn["ln_x_b"][:], ALU.add),
                         reads=["y_f", "bc_ln_x_b"], writes=["y_f"])
                    S.op("dve", lambda e: e.tensor_tensor(v3(t0[:], 8, 64), v3(v_b[:], 8, 64), bc3(bon8[:], 8, 64), ALU.mult),
                         reads=["v_b", "bon8"], writes=["t0"])
                    S.op("pool", lambda e: e.tensor_tensor(y_f[:], y_f[:], t0[:], ALU.add), reads=["y_f", "t0"], writes=["y_f"])
                    S.op("dve", lambda e: e.tensor_tensor(mixed[:, 0:512], y_f[:], g_f[:], ALU.mult),
                         reads=["y_f", "g_f"], writes=["mixed"])

                    chk("L")
                    ps, pk = getps()
                    psb = ps[:].bitcast(BF16)
                    for c in range(8):
                        S.op("pe", lambda e, c=c, psb=psb: e.transpose(psb[:, c * 128:(c + 1) * 128],
                                                                       mixed[:, c * 128:(c + 1) * 128], ident_b[:]),
                             reads=["mixed", "ident_b"], writes=[pk])
                    S.op("act", lambda e, psb=psb: e.activation(mT[:], psb.rearrange("p (c t) -> p c t", c=8), AF.Copy),
                         reads=[pk], writes=["mT"])
                    for nh in range(2):
                        ps, pk = getps()
                        for c in range(8):
                            S.op("pe", lambda e, c=c, nh=nh, ps=ps: e.matmul(ps[:], mT[:, c, :], wo[:, c, nh * 512:(nh + 1) * 512],
                                                                             start=(c == 0), stop=(c == 7)),
                                 reads=["mT", "wo"], writes=[pk])
                        S.op("dve", lambda e, nh=nh, ps=ps, xt=xt: e.tensor_tensor(xt[:, nh * 512:(nh + 1) * 512],
                                                                            xt[:, nh * 512:(nh + 1) * 512], ps[:], ALU.add),
                             reads=[pk, mxk], writes=[mxk])
                  except _StopTile:
                    pass
                  for s in range(2):
                        r0 = s * seq + n * 64
                        S.dma("sp", out[r0:r0 + 64, :], xt[s * 64:(s + 1) * 64, :], reads=[mxk], sem=f"ms{n % 2}")
                S.barrier()
            src_x[0] = out

        for l in range(depth):
            if do_ffn:
                ffn_phase(l, 1, False)
            if do_mix:
                mix_phase(l)
            if do_ffn:
                ffn_phase(l, 2, final_norm and l == depth - 1)
        S.emit(final_waits=[k for k in S.dma_counts if k.startswith("fs") or k.startswith("ms")])
    return nc


_CACHE = {}


def kernel(**inputs):
    x = np.ascontiguousarray(inputs["x"], dtype=np.float32)
    B, seq, d = x.shape
    depth = inputs["w_in"].shape[0]
    key = (seq, depth)
    if key not in _CACHE:
        _CACHE[key] = build_program(seq, depth)
    nc = _CACHE[key]
    consts = make_consts(seq)
    shared = {}
    for k, v in inputs.items():
        if k == "x":
            continue
        a = np.ascontiguousarray(v, dtype=np.float32)
        if k == "r_k":
            a = a.reshape(depth, 512)
        if k == "final_norm":
            a = a.reshape(1, D)
        shared[k] = a
    for k in CONST_ORDER:
        shared["c_" + k] = consts[k]
    ncores = B // 2
    in_maps = []
    for c in range(ncores):
        m = dict(shared)
        m["x"] = x[2 * c:2 * c + 2].reshape(2 * seq, d)
        in_maps.append(m)
    res = run_bass_kernel_spmd(nc, in_maps, core_ids=list(range(ncores)))
    outs = [r["out"].reshape(2, seq, d) for r in res.results]
    return np.concatenate(outs, axis=0).astype(np.float32)
```

```python
import contextlib
import numpy as np
import concourse.bass as bass
import concourse.mybir as mybir
from concourse.bass_utils import run_bass_kernel_spmd

F32 = mybir.dt.float32
BF16 = mybir.dt.bfloat16
AF = mybir.ActivationFunctionType
ALU = mybir.AluOpType
AX = mybir.AxisListType

D = 1024
DFF = 2816
NF = DFF // 128
PROJ = 3328
RW_IN = 1792
NCORES = 8
ENGS = ("pe", "act", "dve", "pool", "sp")
C_DEC = float(np.exp(-0.5))


class _StopTile(Exception):
    pass


class _Op:
    __slots__ = ("eng", "fn", "deps", "dma_deps", "idx", "needs_inc", "count",
                 "dma_sem", "epoch")


class Sched:
    def __init__(self, nc):
        self.nc = nc
        self.streams = {e: [] for e in ENGS}
        self.last_w = {}
        self.readers = {}
        self.seen = {e: {} for e in ENGS}
        self.seen_dma = {e: {} for e in ENGS}
        self.dma_counts = {}
        self.epoch = 0
        self.alias = {}

    def _new(self, eng, fn):
        o = _Op()
        o.eng = eng
        o.fn = fn
        o.deps = []
        o.dma_deps = []
        o.idx = len(self.streams[eng])
        o.needs_inc = False
        o.count = None
        o.dma_sem = None
        o.epoch = self.epoch
        return o

    def _collect(self, op, reads, writes):
        deps = []
        for k in reads:
            w = self.last_w.get(k)
            if w is not None:
                deps.append((w, "raw"))
        for k in writes:
            w = self.last_w.get(k)
            if w is not None:
                deps.append((w, "waw"))
            for r in self.readers.get(k, ()):
                deps.append((r, "war"))
        e = op.eng
        for d, kind in deps:
            if d is op:
                continue
            if d.dma_sem is not None:
                cnt = self.dma_counts[d.dma_sem]
                if self.seen_dma[e].get(d.dma_sem, 0) < cnt:
                    self.seen_dma[e][d.dma_sem] = cnt
                    op.dma_deps.append((d.dma_sem, cnt))
                continue
            if d.epoch != self.epoch:
                continue
            if d.eng == e and e == "pe":
                continue
            if self.seen[e].get(d.eng, -1) >= d.idx:
                continue
            self.seen[e][d.eng] = d.idx
            d.needs_inc = True
            op.deps.append(d)
        for k in reads:
            self.readers.setdefault(k, []).append(op)
        for k in writes:
            self.last_w[k] = op
            self.readers[k] = []

    def op(self, eng, fn, reads=(), writes=()):
        reads = [self.alias.get(k, k) for k in reads]
        writes = [self.alias.get(k, k) for k in writes]
        o = self._new(eng, fn)
        self._collect(o, reads, writes)
        self.streams[eng].append(o)
        return o

    def dma(self, queue, out, in_, reads=(), writes=(), sem="dma0"):
        def fn(eng, out=out, in_=in_):
            return eng.dma_start(out=out, in_=in_)
        sem = f"{sem}_{queue}"
        o = self.op(queue, fn, reads, writes)
        o.dma_sem = sem
        self.dma_counts[sem] = self.dma_counts.get(sem, 0) + 1
        return o

    def barrier(self):
        lasts = {}
        for e in ENGS:
            for o in reversed(self.streams[e]):
                if o.epoch != self.epoch:
                    break
                if o.dma_sem is None and o.fn is not None:
                    lasts[e] = o
                    break
        for e in ENGS:
            o = self._new(e, None)
            for e2, l in lasts.items():
                if e2 == e and e == "pe":
                    continue
                if self.seen[e].get(e2, -1) < l.idx:
                    l.needs_inc = True
                    o.deps.append(l)
            for s, c in self.dma_counts.items():
                if self.seen_dma[e].get(s, 0) < c:
                    self.seen_dma[e][s] = c
                    o.dma_deps.append((s, c))
            self.streams[e].append(o)
        self.epoch += 1
        self.seen = {e: {} for e in ENGS}

    def emit(self, final_waits=()):
        nc = self.nc
        n_epochs = self.epoch + 1
        for e in ENGS:
            c = 0
            ep = 0
            for o in self.streams[e]:
                if o.epoch != ep:
                    ep = o.epoch
                    c = 0
                if o.needs_inc:
                    c += 1
                    o.count = c
        with contextlib.ExitStack() as st:
            esem = {}
            for e in ENGS:
                used = set(o.epoch for o in self.streams[e] if o.needs_inc)
                for ep in sorted(used):
                    esem[(e, ep)] = st.enter_context(nc.semaphore(f"s_{e}_{ep}"))
            dsem = {s: st.enter_context(nc.semaphore(f"d_{s}")) for s in self.dma_counts}
            block = st.enter_context(nc.Block())

            def replay(e, eng):
                for o in self.streams[e]:
                    for d in o.deps:
                        eng.wait_ge(esem[(d.eng, d.epoch)], d.count)
                    for s, c in o.dma_deps:
                        eng.wait_ge(dsem[s], 16 * c)
                    if o.fn is None:
                        continue
                    ins = o.fn(eng)
                    if o.dma_sem is not None:
                        ins.then_inc(dsem[o.dma_sem], 16)
                    elif o.needs_inc:
                        ins.then_inc(esem[(o.eng, o.epoch)], 1)
                if e == "sp":
                    for s in final_waits:
                        eng.wait_ge(dsem[s], 16 * self.dma_counts[s])

            @block.tensor
            def _(eng):
                replay("pe", eng)

            @block.scalar
            def _(eng):
                replay("act", eng)

            @block.vector
            def _(eng):
                replay("dve", eng)

            @block.gpsimd
            def _(eng):
                replay("pool", eng)

            @block.sync
            def _(eng):
                replay("sp", eng)


def make_consts(seq):
    nt = seq // 64
    p = np.arange(128)
    s_of = p // 64
    t_of = p % 64
    same = (s_of[:, None] == s_of[None, :])
    c = {}
    c["ident"] = np.eye(128, dtype=np.float32)
    c["tri_i"] = (1.0 * (same & (t_of[:, None] <= t_of[None, :]))).astype(np.float32)
    c["tri_r"] = (1.0 * (same & (t_of[:, None] > t_of[None, :]))).astype(np.float32)
    seqind = np.zeros((128, 2), np.float32)
    seqind[p, s_of] = 1.0
    c["seqind"] = seqind
    su = (same & (t_of[:, None] < t_of[None, :])).astype(np.float32)
    ui = (same & (t_of[:, None] <= t_of[None, :])).astype(np.float32)
    sl = (same & (t_of[:, None] > t_of[None, :])).astype(np.float32)
    c["m1"] = np.concatenate([su, ui], axis=1)
    c["msl"] = sl
    H = 4
    log_g = np.log(1.0 - np.power(2.0, -5.0 - np.arange(H, dtype=np.float64)))
    j = t_of[:, None].astype(np.float64)
    i = t_of[None, :].astype(np.float64)
    dm = np.zeros((128, H, 128), np.float64)
    for h in range(H):
        dm[:, h, :] = same * np.exp(log_g[h] * (np.abs(i - j) - (i + 1.0))) * 0.125
    c["dmask"] = dm.reshape(128, H * 128).astype(np.float32)
    qw = np.exp(log_g[None, :] * (t_of[:, None] + 1.0))
    kw = np.exp(log_g[None, :] * (63.0 - t_of[:, None])) * 0.125
    cd = np.broadcast_to(np.exp(log_g * 64.0)[None, :], (128, H))
    c["qkw"] = np.concatenate([qw, kw, cd], axis=1).astype(np.float32)
    half = 32
    inv_freq = (1.0 / (np.float32(10000.0) ** np.linspace(0.0, 1.0, half, dtype=np.float32))).astype(np.float32)
    pos = np.arange(seq, dtype=np.float32)
    ang = (pos[:, None] * inv_freq[None, :]).astype(np.float32).astype(np.float64)
    cs = np.concatenate([np.cos(ang), np.sin(ang)], axis=1).astype(np.float32)
    cs = cs.reshape(nt, 64, 64).transpose(1, 0, 2)
    c["rope"] = np.ascontiguousarray(np.concatenate([cs, cs], axis=0).reshape(128, nt * 64))
    return c


CONST_ORDER = ["ident", "tri_i", "tri_r", "seqind", "m1", "msl", "dmask", "qkw", "rope"]


def build_program(seq, depth, do_ffn=True, do_mix=True, final_norm=True):
    nc = bass.Bass("TRN2", target_bir_lowering=False)
    ntok = 2 * seq
    nt = seq // 64

    def din(name, shape):
        return nc.dram_tensor(name, list(shape), F32, kind="ExternalInput").ap()

    x_in = din("x", [ntok, D])
    w = {}
    for nm, shp in [("ffn1_norm", [depth, D]), ("ffn1_w_gate", [depth, D, DFF]), ("ffn1_w_up", [depth, D, DFF]),
                    ("ffn1_w_down", [depth, DFF, D]), ("mix_norm", [depth, D]), ("w_in", [depth, D, PROJ]),
                    ("shift_mu", [depth, RW_IN]), ("w0", [depth, 512]), ("w_lora_up", [depth, 64, 512]),
                    ("a0", [depth, 512]), ("a_lora_up", [depth, 64, 512]), ("g_lora_up", [depth, 128, 512]),
                    ("k_k", [depth, 512]), ("k_a", [depth, 512]), ("r_k", [depth, 512]),
                    ("ln_x_w", [depth, 512]), ("ln_x_b", [depth, 512]), ("w_out", [depth, D, D]),
                    ("ffn2_norm", [depth, D]), ("ffn2_w_gate", [depth, D, DFF]), ("ffn2_w_up", [depth, D, DFF]),
                    ("ffn2_w_down", [depth, DFF, D]), ("final_norm", [1, D])]:
        w[nm] = din(nm, shp)
    cshape = {"ident": 128, "tri_i": 128, "tri_r": 128, "seqind": 2, "m1": 256, "msl": 128,
              "dmask": 512, "qkw": 12, "rope": nt * 64}
    cd = {k: din("c_" + k, [128, v]) for k, v in cshape.items()}
    out = nc.dram_tensor("out", [ntok, D], F32, kind="ExternalOutput").ap()

    S = Sched(nc)
    st = contextlib.ExitStack()
    with st:
        uid = [0]

        def sb(name, shape, dt=F32, stack=st):
            uid[0] += 1
            return stack.enter_context(nc.sbuf_tensor(f"{name}_{uid[0]}", list(shape), dt))

        banks = [st.enter_context(nc.psum_tensor(f"ps{i}", [128, 512], F32)) for i in range(8)]
        pctr = [0]

        def getps():
            i = pctr[0] % 8
            pctr[0] += 1
            return banks[i], f"ps{i}"

        ident_b = sb("ident_b", [128, 128], BF16)
        tri_i = sb("tri_i", [128, 128], BF16)
        tri_r = sb("tri_r", [128, 128], BF16)
        seqind = sb("seqind", [128, 2], BF16)
        m1 = sb("m1", [128, 256])
        msl = sb("msl", [128, 128])
        ident_f = sb("ident_f", [128, 128])
        dmask = sb("dmask", [128, 512])
        qkw = sb("qkw", [128, 12])
        S.dma("pool", ident_b[:], cd["ident"], writes=["ident_b"], sem="c")
        S.dma("sp", ident_f[:], cd["ident"], writes=["ident_f"], sem="c")
        S.dma("pool", tri_i[:], cd["tri_i"], writes=["tri_i"], sem="c")
        S.dma("pool", tri_r[:], cd["tri_r"], writes=["tri_r"], sem="c")
        S.dma("pool", seqind[:], cd["seqind"], writes=["seqind"], sem="c")
        S.dma("sp", m1[:], cd["m1"], writes=["m1"], sem="c")
        S.dma("sp", msl[:], cd["msl"], writes=["msl"], sem="c")
        S.dma("sp", dmask[:], cd["dmask"], writes=["dmask"], sem="c")
        S.dma("sp", qkw[:], cd["qkw"], writes=["qkw"], sem="c")

        src_x = [x_in]

        def rstd_ops(ss, n, tag):
            S.op("dve", lambda e: e.tensor_scalar(ss[:, 0:n], ss[:, 0:n], 1.0 / D, 1e-6, ALU.mult, ALU.add),
                 reads=[tag], writes=[tag])
            S.op("act", lambda e: e.activation(ss[:, 0:n], ss[:, 0:n], AF.Sqrt), reads=[tag], writes=[tag])
            S.op("dve", lambda e: e.reciprocal(ss[:, 0:n], ss[:, 0:n]), reads=[tag], writes=[tag])

        def ffn_phase(l, which, last):
            TB = 256
            nblk = ntok // TB
            with contextlib.ExitStack() as fs:
                wg = sb("wg", [128, 8, DFF], BF16, fs)
                wu = sb("wu", [128, 8, DFF], BF16, fs)
                wd = sb("wd", [128, NF, D], BF16, fs)
                gain = sb("gain", [128, D], F32, fs)
                xt = [sb(f"fx{i}", [128, 2, D], F32, fs) for i in range(2)]
                hb = sb("fh", [128, 2, D], BF16, fs)
                hT = sb("fhT", [128, 8, TB], BF16, fs)
                aT = sb("faT", [128, NF, TB], BF16, fs)
                sg = [sb(f"fsg{i}", [128, TB], F32, fs) for i in range(2)]
                junk = sb("fjunk", [128, D], BF16, fs)
                ss = [sb(f"fss{i}", [128, 4], F32, fs) for i in range(2)]
                if last:
                    fin_bc = sb("fin_bc", [128, D], F32, fs)
                    S.dma("sp", fin_bc[:], w["final_norm"][0:1, :].broadcast_to([128, D]), writes=["fin_bc"], sem="w")
                pre = "ffn1" if which == 1 else "ffn2"
                S.dma("sp", gain[:], w[pre + "_norm"][l:l + 1, :].broadcast_to([128, D]), writes=["gain"], sem="w")
                for c in range(8):
                    S.dma("pool", wg[:, c, :], w[pre + "_w_gate"][l, c * 128:(c + 1) * 128, :], writes=["wg"], sem="wgu")
                    S.dma("pool", wu[:, c, :], w[pre + "_w_up"][l, c * 128:(c + 1) * 128, :], writes=["wu"], sem="wgu")
                for f in range(NF):
                    S.dma("pool", wd[:, f, :], w[pre + "_w_down"][l, f * 128:(f + 1) * 128, :], writes=["wd"], sem="wd")

                def load(b):
                    i = b % 2
                    src = src_x[0]
                    for j in range(2):
                        r0 = b * TB + j * 128
                        S.dma("sp", xt[i][:, j, :], src[r0:r0 + 128, :], writes=[f"fx{i}"], sem=f"fl{i}")

                def norm(b):
                    i = b % 2
                    X = xt[i]
                    xk = f"fx{i}"
                    ssb = ss[i]
                    sk = f"fss{i}"
                    for j in range(2):
                        S.op("act", lambda e, j=j, X=X, ssb=ssb: e.activation(junk[:], X[:, j, :], AF.Square,
                                                                              accum_out=ssb[:, j:j + 1]),
                             reads=[xk], writes=["fjunk", sk])
                    rstd_ops(ssb, 2, sk)
                    for j in range(2):
                        S.op("dve", lambda e, j=j, X=X, ssb=ssb: e.scalar_tensor_tensor(
                            hb[:, j, :], X[:, j, :], ssb[:, j:j + 1], gain[:], ALU.mult, ALU.mult),
                            reads=[xk, sk, "gain"], writes=["fh"])

                load(0)
                norm(0)
                for b in range(nblk):
                    i = b % 2
                    X = xt[i]
                    xk = f"fx{i}"
                    if b + 1 < nblk:
                        load(b + 1)
                    ssb = ss[i]
                    sk = f"fss{i}"
                    for half in range(2):
                        ps, pk = getps()
                        psb = ps[:].bitcast(BF16)
                        for cc in range(4):
                            c = half * 4 + cc
                            for j in range(2):
                                S.op("pe", lambda e, c=c, cc=cc, j=j, psb=psb: e.transpose(
                                    psb[:, cc * 256 + j * 128: cc * 256 + (j + 1) * 128],
                                    hb[:, j, c * 128:(c + 1) * 128], ident_b[:]),
                                    reads=["fh", "ident_b"], writes=[pk])
                        eng = "act" if half == 0 else "dve"
                        if eng == "act":
                            S.op("act", lambda e, half=half, psb=psb: e.activation(
                                hT[:, half * 4:(half + 1) * 4, :], psb.rearrange("p (c t) -> p c t", c=4), AF.Copy),
                                reads=[pk], writes=["fhT"])
                        else:
                            S.op("dve", lambda e, half=half, psb=psb: e.tensor_copy(
                                hT[:, half * 4:(half + 1) * 4, :], psb.rearrange("p (c t) -> p c t", c=4)),
                                reads=[pk], writes=["fhT"])
                    for f in range(NF):
                        ps, pk = getps()
                        for c in range(8):
                            S.op("pe", lambda e, c=c, f=f, ps=ps: e.matmul(
                                ps[:, 0:TB], wg[:, c, f * 128:(f + 1) * 128], hT[:, c, :],
                                start=(c == 0), stop=(c == 7)), reads=["wg", "fhT"], writes=[pk])
                        for c in range(8):
                            S.op("pe", lambda e, c=c, f=f, ps=ps: e.matmul(
                                ps[:, TB:2 * TB], wu[:, c, f * 128:(f + 1) * 128], hT[:, c, :],
                                start=(c == 0), stop=(c == 7)), reads=["wu", "fhT"], writes=[pk])
                        sgb = sg[f % 2]
                        sgk = f"fsg{f % 2}"
                        S.op("act", lambda e, ps=ps, sgb=sgb: e.activation(sgb[:], ps[:, 0:TB], AF.Silu),
                             reads=[pk], writes=[sgk])
                        S.op("dve", lambda e, ps=ps, sgb=sgb, f=f: e.tensor_tensor(
                            aT[:, f, :], sgb[:], ps[:, TB:2 * TB], ALU.mult),
                            reads=[pk, sgk], writes=["faT"])
                    if b + 1 < nblk:
                        norm(b + 1)
                    for j in range(2):
                        for n in range(2):
                            ps, pk = getps()
                            for f in range(NF):
                                S.op("pe", lambda e, f=f, j=j, n=n, ps=ps: e.matmul(
                                    ps[:], aT[:, f, j * 128:(j + 1) * 128], wd[:, f, n * 512:(n + 1) * 512],
                                    start=(f == 0), stop=(f == NF - 1)), reads=["faT", "wd"], writes=[pk])
                            S.op("dve", lambda e, j=j, n=n, ps=ps, X=X: e.scalar_tensor_tensor(
                                X[:, j, n * 512:(n + 1) * 512], ps[:], 0.5, X[:, j, n * 512:(n + 1) * 512],
                                ALU.mult, ALU.add), reads=[pk, xk], writes=[xk])
                    if last:
                        for j in range(2):
                            S.op("act", lambda e, j=j, X=X, ssb=ssb: e.activation(
                                junk[:], X[:, j, :], AF.Square, accum_out=ssb[:, 2 + j:3 + j]),
                                reads=[xk], writes=["fjunk", sk])
                        S.op("dve", lambda e, ssb=ssb: e.tensor_scalar(ssb[:, 2:4], ssb[:, 2:4], 1.0 / D, 1e-6,
                                                                       ALU.mult, ALU.add), reads=[sk], writes=[sk])
                        S.op("act", lambda e, ssb=ssb: e.activation(ssb[:, 2:4], ssb[:, 2:4], AF.Sqrt),
                             reads=[sk], writes=[sk])
                        S.op("dve", lambda e, ssb=ssb: e.reciprocal(ssb[:, 2:4], ssb[:, 2:4]), reads=[sk], writes=[sk])
                        for j in range(2):
                            S.op("dve", lambda e, j=j, X=X, ssb=ssb: e.scalar_tensor_tensor(
                                X[:, j, :], X[:, j, :], ssb[:, 2 + j:3 + j], fin_bc[:], ALU.mult, ALU.mult),
                                reads=[xk, sk, "fin_bc"], writes=[xk])
                    for j in range(2):
                        r0 = b * TB + j * 128
                        S.dma("sp", out[r0:r0 + 128, :], X[:, j, :], reads=[xk], sem=f"fs{i}")
                S.barrier()
            src_x[0] = out

        def mix_phase(l):
            with contextlib.ExitStack() as ms:
                wm = sb("wm", [128, 8, 5120], BF16, ms)
                wo = sb("wo", [128, 8, D], BF16, ms)
                wal = sb("wal", [128, 1024], BF16, ms)
                glu = sb("glu", [128, 512], BF16, ms)
                gain = sb("mgain", [128, D], F32, ms)
                bcn = {}
                for nm in ["w0", "a0", "k_k", "k_a", "r_k", "ln_x_w", "ln_x_b"]:
                    bcn[nm] = sb("bc_" + nm, [128, 512], F32, ms)
                    S.dma("sp", bcn[nm][:], w[nm][l:l + 1, :].broadcast_to([128, 512]), writes=["bc_" + nm], sem="w")
                S.dma("sp", gain[:], w["mix_norm"][l:l + 1, :].broadcast_to([128, D]), writes=["mgain"], sem="w")
                S.op("pool", lambda e: e.memset(wal[:], 0.0), writes=["wal"])
                S.dma("pool", wal[0:64, 0:512], w["w_lora_up"][l], writes=["wal"], sem="w")
                S.dma("pool", wal[64:128, 512:1024], w["a_lora_up"][l], writes=["wal"], sem="w")
                S.dma("pool", glu[:], w["g_lora_up"][l], writes=["glu"], sem="w")
                for c in range(8):
                    S.dma("pool", wo[:, c, :], w["w_out"][l, c * 128:(c + 1) * 128, :], writes=["wo"], sem="w")
                    S.dma("pool", wm[:, c, 3584:5120], w["w_in"][l, c * 128:(c + 1) * 128, RW_IN:PROJ],
                          writes=["wm"], sem="w")
                with contextlib.ExitStack() as ps_:
                    mu = sb("mu", [128, RW_IN], F32, ps_)
                    omm = sb("omm", [128, RW_IN], F32, ps_)
                    stg = [sb(f"stg{i}", [128, RW_IN], F32, ps_) for i in range(2)]
                    S.dma("sp", mu[:], w["shift_mu"][l:l + 1, :].broadcast_to([128, RW_IN]), writes=["mu"], sem="w")
                    S.op("dve", lambda e: e.tensor_scalar(omm[:], mu[:], -1.0, 1.0, ALU.mult, ALU.add),
                         reads=["mu"], writes=["omm"])
                    for c in range(8):
                        sg_ = stg[c % 2]
                        sk_ = f"stg{c % 2}"
                        S.dma("sp", sg_[:], w["w_in"][l, c * 128:(c + 1) * 128, 0:RW_IN], writes=[sk_], sem=f"wp{c % 2}")
                        S.op("dve", lambda e, c=c, sg_=sg_: e.tensor_tensor(wm[:, c, 0:RW_IN], sg_[:], omm[:], ALU.mult),
                             reads=[sk_, "omm"], writes=["wm"])
                        S.op("pool", lambda e, c=c, sg_=sg_: e.tensor_tensor(wm[:, c, RW_IN:2 * RW_IN], sg_[:], mu[:], ALU.mult),
                             reads=[sk_, "mu"], writes=["wm"])
                    S.barrier()

                xts = [sb(f"mx{i}", [128, D], F32, ms) for i in range(2)]
                hb = sb("mh", [128, D], BF16, ms)
                junk = hb
                ss = sb("mss", [128, 2], F32, ms)
                hT = sb("mhT", [128, 8, 128], BF16, ms)
                hTp = sb("mhTp", [128, 8, 128], BF16, ms)
                carry = sb("mcarry", [128, 8, 2], BF16, ms)
                ropet = [sb(f"ropet{i}", [128, 64], F32, ms) for i in range(2)]
                G = {i: sb(f"G{i}", [128, 512], F32, ms) for i in (1, 2, 3, 4, 6, 7, 8, 9, 10)}
                G5 = sb("G5", [128, 1024], F32, ms)

                def bf3(t, h):
                    return t[:].bitcast(BF16).rearrange("p (h t) -> p h t", h=h)
                qk_f = Wi = G[1][:]
                rg_s = Wn = G[2][:]
                rot = We = G[3][:]
                o_f = Wh = G[4][:]
                Pm = [bf3(G[1], 8), bf3(G[2], 8)]
                Qm = [bf3(G[3], 8), bf3(G[4], 8)]
                ra = G5[:, 0:256]
                rb = G5[:, 256:512]
                kk = G5[:, 0:512]
                bp = G5[:, 512:1024]
                g5b = G5[:].bitcast(BF16)
                Pp = g5b[:, 1024:2048].rearrange("p (h t) -> p h t", h=8)
                sigw = G[6][:]
                TTbs = [bf3(G[6], 8), g5b[:, 0:1024].rearrange("p (h t) -> p h t", h=8)]
                r_f = y_f = G[7][:]
                k_f = G[8][:]
                mT = bf3(G[8], 8)
                g9 = G[9][:].bitcast(BF16)
                g10 = G[10][:].bitcast(BF16)
                tm4 = [g9[:, 0:512], g9[:, 512:1024], g10[:, 0:512], g10[:, 512:1024]]
                Xb = g10[:, 0:512].rearrange("p (h v) -> p h v", h=8)
                Ub = g10[:, 512:1024].rearrange("p (h v) -> p h v", h=8)
                S.alias.update({"qk_f": "G1", "Wi": "G1", "Pm0": "G1", "rg_s": "G2", "Wn": "G2", "Pm1": "G2",
                                "rot": "G3", "We": "G3", "Qm0": "G3", "o_f": "G4", "Wh": "G4", "Qm1": "G4",
                                "ra": "G5", "rb": "G5", "kk": "G5", "bp": "G5", "TTb1": "G5", "Pp": "G5", "sigw": "G6", "TTb0": "G6",
                                "r_f": "G7", "y_f": "G7", "k_f": "G8", "mT": "G8", "tm0": "G9", "tm1": "G9",
                                "tm2": "G10", "tm3": "G10", "Xb": "G10", "Ub": "G10", "mjunk": "mh"})
                v_b = sb("v_b", [128, 512], BF16, ms)
                lwT = sb("lwT", [128, 128], BF16, ms)
                lgT = sb("lgT", [128, 128], BF16, ms)
                rv_b = sb("rv_b", [128, 512], BF16, ms)
                a_f = sb("a_f", [128, 512], F32, ms)
                g_f = sb("g_f", [128, 512], F32, ms)
                WC = sb("WC", [128, 8], F32, ms)
                sighl = sb("sighl", [128, 1024], BF16, ms)
                t0 = sb("t0", [128, 512], F32, ms)
                t1 = sb("t1", [128, 512], F32, ms)
                k_h = sb("k_h", [128, 512], F32, ms)
                s8 = sb("s8", [128, 8], F32, ms)
                bon8 = sb("bon8", [128, 8], F32, ms)
                arT = sb("arT", [128, 8, 2, 128], BF16, ms)
                bT = sb("bT", [128, 8, 128], BF16, ms)
                kT = sb("kT", [128, 8, 128], BF16, ms)
                Bh = sb("Bh", [128, 8, 128], BF16, ms)
                Kh = sb("Kh", [128, 8, 128], BF16, ms)
                QA = sb("QA", [128, 8, 2, 128], BF16, ms)
                KA = sb("KA", [128, 8, 2, 128], BF16, ms)
                Sf = sb("Sf", [128, 8, 64], F32, ms)
                Sb = sb("Sb", [128, 8, 64], BF16, ms)
                mixed = sb("mixed", [128, D], BF16, ms)
                qt_b = sb("qt_b", [128, 256], BF16, ms)
                kp_b = sb("kp_b", [128, 256], BF16, ms)
                rKh = sb("rKh", [128, 4, 128], BF16, ms)
                rqT = sb("rqT", [128, 4, 128], BF16, ms)
                rkT = sb("rkT", [128, 4, 128], BF16, ms)
                Sc = sb("Sc", [128, 4, 128], BF16, ms)
                RSf = sb("RSf", [128, 4, 128], F32, ms)
                RSb = sb("RSb", [128, 4, 128], BF16, ms)
                s4 = sb("s4", [128, 4], F32, ms)

                for tname, tt in [("arT", arT), ("bT", bT), ("kT", kT), ("Bh", Bh), ("Kh", Kh), ("rKh", rKh),
                                  ("rqT", rqT), ("rkT", rkT), ("Sb", Sb), ("RSb", RSb)]:
                    S.op("pool", lambda e, tt=tt: e.memset(tt[:], 0.0), writes=[tname])
                S.op("dve", lambda e: e.memset(Sf[:], 0.0), writes=["Sf"])
                S.op("dve", lambda e: e.memset(RSf[:], 0.0), writes=["RSf"])
                S.op("dve", lambda e: e.memset(carry[:], 0.0), writes=["mcarry"])

                def bc3(ap2, n_in, n_out):
                    return ap2.unsqueeze(2).to_broadcast([ap2.shape[0], n_in, n_out])

                def bch(ap2, nh, ncol):
                    return ap2.unsqueeze(1).to_broadcast([ap2.shape[0], nh, ncol])

                def v3(ap2, a, b_):
                    return ap2.rearrange("p (a b) -> p a b", a=a)

                src = src_x[0]
                import os as _os
                _stop = _os.environ.get("MIX_STOP", "")

                def chk(tag):
                    if tag == _stop:
                        raise _StopTile()

                for n in range(nt):
                  try:
                    xt = xts[n % 2]
                    mxk = f"mx{n % 2}"
                    for s in range(2):
                        r0 = s * seq + n * 64
                        S.dma("sp", xt[s * 64:(s + 1) * 64, :], src[r0:r0 + 64, :], writes=[mxk], sem=f"ml{n % 2}")
                    S.op("act", lambda e, xt=xt: e.activation(junk[:], xt[:], AF.Square, accum_out=ss[:, 0:1]),
                         reads=[mxk], writes=["mjunk", "mss"])
                    rstd_ops(ss, 1, "mss")
                    S.op("dve", lambda e, xt=xt: e.scalar_tensor_tensor(hb[:], xt[:], ss[:, 0:1], gain[:], ALU.mult, ALU.mult),
                         reads=[mxk, "mss", "mgain"], writes=["mh"])
                    chk("C")
                    ps, pk = getps()
                    psb = ps[:].bitcast(BF16)
                    for c in range(8):
                        S.op("pe", lambda e, c=c, psb=psb: e.transpose(psb[:, c * 128:(c + 1) * 128],
                                                                       hb[:, c * 128:(c + 1) * 128], ident_b[:]),
                             reads=["mh", "ident_b"], writes=[pk])
                    S.op("act", lambda e, psb=psb: e.activation(
                        hT[:], psb.rearrange("p (c t) -> p c t", c=8), AF.Copy),
                        reads=[pk], writes=["mhT"])
                    hT4 = hT[:].rearrange("p c (s t) -> p c s t", s=2)
                    hTp4 = hTp[:].rearrange("p c (s t) -> p c s t", s=2)
                    S.op("pool", lambda e: e.tensor_copy(hTp4[:, :, :, 1:64], hT4[:, :, :, 0:63]),
                         reads=["mhT"], writes=["mhTp"])
                    S.op("pool", lambda e: e.tensor_copy(hTp4[:, :, :, 0], carry[:]),
                         reads=["mcarry"], writes=["mhTp"])
                    S.op("pool", lambda e: e.tensor_copy(carry[:], hT4[:, :, :, 63]),
                         reads=["mhT"], writes=["mcarry"])

                    def cur(c):
                        return hT[:, c, :]

                    def prev(c):
                        return hTp[:, c, :]

                    chk("D")
                    def proj_rw(ps_ap, col0, ncol, pk):
                        for c in range(8):
                            S.op("pe", lambda e, c=c: e.matmul(ps_ap, cur(c), wm[:, c, col0:col0 + ncol],
                                                               start=(c == 0), stop=False),
                                 reads=["mhT", "wm"], writes=[pk])
                        for c in range(8):
                            S.op("pe", lambda e, c=c: e.matmul(ps_ap, prev(c), wm[:, c, RW_IN + col0:RW_IN + col0 + ncol],
                                                               start=False, stop=(c == 7)),
                                 reads=["mhTp", "wm"], writes=[pk])

                    ps, pk = getps()
                    proj_rw(ps[:], 0, 512, pk)
                    S.op("act", lambda e, ps=ps: e.activation(r_f[:], ps[:], AF.Copy), reads=[pk], writes=["r_f"])
                    ps, pk = getps()
                    proj_rw(ps[:], 512, 512, pk)
                    S.op("dve", lambda e, ps=ps: e.tensor_copy(k_f[:], ps[:]), reads=[pk], writes=["k_f"])
                    ps, pk = getps()
                    proj_rw(ps[:], 1024, 512, pk)
                    S.op("dve", lambda e, ps=ps: e.tensor_copy(v_b[:], ps[:]), reads=[pk], writes=["v_b"])
                    ps, pk = getps()
                    for gi in range(2):
                        col0 = 1536 + gi * 128
                        for c in range(8):
                            S.op("pe", lambda e, c=c, gi=gi, col0=col0, ps=ps: e.matmul(
                                ps[:, gi * 128:(gi + 1) * 128], wm[:, c, col0:col0 + 128], cur(c),
                                start=(c == 0), stop=False), reads=["mhT", "wm"], writes=[pk])
                        for c in range(8):
                            S.op("pe", lambda e, c=c, gi=gi, col0=col0, ps=ps: e.matmul(
                                ps[:, gi * 128:(gi + 1) * 128], wm[:, c, RW_IN + col0:RW_IN + col0 + 128], prev(c),
                                start=False, stop=(c == 7)), reads=["mhTp", "wm"], writes=[pk])
                    S.op("act", lambda e, ps=ps: e.activation(lwT[0:64, :], ps[0:64, 0:128], AF.Tanh), reads=[pk], writes=["lwT"])
                    S.op("act", lambda e, ps=ps: e.activation(lwT[64:128, :], ps[64:128, 0:128], AF.Copy), reads=[pk], writes=["lwT"])
                    S.op("act", lambda e, ps=ps: e.activation(lgT[:], ps[:, 128:256], AF.Sigmoid), reads=[pk], writes=["lgT"])
                    ps, pk = getps()
                    for c in range(8):
                        S.op("pe", lambda e, c=c, ps=ps: e.matmul(ps[:], cur(c), wm[:, c, 3584:4096],
                                                                  start=(c == 0), stop=(c == 7)),
                             reads=["mhT", "wm"], writes=[pk])
                    S.op("dve", lambda e, ps=ps: e.tensor_copy(qk_f[:], ps[:]), reads=[pk], writes=["qk_f"])
                    ps, pk = getps()
                    for c in range(8):
                        S.op("pe", lambda e, c=c, ps=ps: e.matmul(ps[:], cur(c), wm[:, c, 4096:4608],
                                                                  start=(c == 0), stop=(c == 7)),
                             reads=["mhT", "wm"], writes=[pk])
                    S.op("act", lambda e, ps=ps: e.activation(rv_b[:], ps[:], AF.Copy), reads=[pk], writes=["rv_b"])
                    ps, pk = getps()
                    for c in range(8):
                        S.op("pe", lambda e, c=c, ps=ps: e.matmul(ps[:], cur(c), wm[:, c, 4608:5120],
                                                                  start=(c == 0), stop=(c == 7)),
                             reads=["mhT", "wm"], writes=[pk])
                    S.op("act", lambda e, ps=ps: e.activation(rg_s[:], ps[:], AF.Silu), reads=[pk], writes=["rg_s"])

                    chk("K")
                    cs_ = ropet[n % 2]
                    ropek = f"ropet{n % 2}"
                    S.dma("sp", cs_[:], cd["rope"][:, n * 64:(n + 1) * 64], writes=[ropek], sem=f"rp{n % 2}")
                    cosb = cs_[:, 0:32].unsqueeze(1).to_broadcast([128, 8, 32])
                    sinb = cs_[:, 32:64].unsqueeze(1).to_broadcast([128, 8, 32])
                    qk4 = qk_f[:].rearrange("p (g a d) -> p g a d", g=8, a=2)
                    rot4 = rot[:].rearrange("p (g a d) -> p g a d", g=8, a=2)
                    ra3 = v3(ra[:], 8, 32)
                    rb3 = v3(rb[:], 8, 32)
                    S.op("dve", lambda e, cosb=cosb, sinb=sinb: e.tensor_tensor(ra3, qk4[:, :, 0, :], cosb, ALU.mult), reads=["qk_f", ropek], writes=["ra"])
                    S.op("pool", lambda e, cosb=cosb, sinb=sinb: e.tensor_tensor(rb3, qk4[:, :, 1, :], sinb, ALU.mult), reads=["qk_f", ropek], writes=["rb"])
                    S.op("dve", lambda e: e.tensor_tensor(rot4[:, :, 0, :], ra3, rb3, ALU.subtract), reads=["ra", "rb"], writes=["rot"])
                    S.op("dve", lambda e, cosb=cosb, sinb=sinb: e.tensor_tensor(ra3, qk4[:, :, 0, :], sinb, ALU.mult), reads=["qk_f", ropek], writes=["ra"])
                    S.op("pool", lambda e, cosb=cosb, sinb=sinb: e.tensor_tensor(rb3, qk4[:, :, 1, :], cosb, ALU.mult), reads=["qk_f", ropek], writes=["rb"])
                    S.op("dve", lambda e: e.tensor_tensor(rot4[:, :, 1, :], ra3, rb3, ALU.add), reads=["ra", "rb"], writes=["rot"])
                    S.op("dve", lambda e: e.tensor_tensor(v3(qt_b[:], 4, 64), v3(rot[:, 0:256], 4, 64), bc3(qkw[:, 0:4], 4, 64), ALU.mult),
                         reads=["rot", "qkw"], writes=["qt_b"])
                    S.op("pool", lambda e: e.tensor_copy(kp_b[:], rot[:, 256:512]), reads=["rot"], writes=["kp_b"])
                    for s in range(2):
                        sl_ = slice(s * 64, (s + 1) * 64)
                        S.op("dve" if s == 0 else "pool", lambda e, s=s, sl_=sl_: e.tensor_tensor(
                            rKh[sl_, :, s * 64:(s + 1) * 64], v3(rot[sl_, 256:512], 4, 64), bc3(qkw[sl_, 4:8], 4, 64), ALU.mult),
                            reads=["rot", "qkw"], writes=["rKh"])
                    chk("K1")
                    for gi, (srcb, skey, dst, dk) in enumerate([(qt_b, "qt_b", rqT, "rqT"), (kp_b, "kp_b", rkT, "rkT")]):
                        ps, pk = getps()
                        psb = ps[:].bitcast(BF16)
                        for pr in range(2):
                            S.op("pe", lambda e, pr=pr, psb=psb, srcb=srcb: e.transpose(
                                psb[:, pr * 128:(pr + 1) * 128], srcb[:, pr * 128:(pr + 1) * 128], ident_b[:]),
                                reads=[skey, "ident_b"], writes=[pk])
                        pv = psb[:, 0:256].rearrange("p (g t) -> p g t", g=2)
                        dst5 = dst[:].rearrange("p (pr par) t -> p pr par t", par=2)
                        for par in range(2):
                            for s in range(2):
                                d_ = dst5[s * 64:(s + 1) * 64, :, par, s * 64:(s + 1) * 64]
                                i_ = pv[par * 64:(par + 1) * 64, :, s * 64:(s + 1) * 64]
                                S.op("dve", lambda e, d_=d_, i_=i_: e.tensor_copy(d_, i_), reads=[pk], writes=[dk])
                    chk("K2")
                    ps, pk = getps()
                    for h in range(4):
                        S.op("pe", lambda e, h=h, ps=ps: e.matmul(ps[:, h * 128:(h + 1) * 128], rkT[:, h, :], rqT[:, h, :],
                                                                  start=True, stop=True), reads=["rkT", "rqT"], writes=[pk])
                    S.op("dve", lambda e, ps=ps: e.tensor_tensor(Sc[:], v3(ps[:], 4, 128), v3(dmask[:], 4, 128), ALU.mult),
                         reads=[pk, "dmask"], writes=["Sc"])
                    ps, pk = getps()
                    for h in range(4):
                        S.op("pe", lambda e, h=h, ps=ps: e.matmul(ps[:, h * 128:(h + 1) * 128], Sc[:, h, :],
                                                                  rv_b[:, h * 128:(h + 1) * 128], start=True, stop=False),
                             reads=["Sc", "rv_b"], writes=[pk])
                        S.op("pe", lambda e, h=h, ps=ps: e.matmul(ps[:, h * 128:(h + 1) * 128], rqT[:, h, :], RSb[:, h, :],
                                                                  start=False, stop=True), reads=["rqT", "RSb"], writes=[pk])
                    S.op("act", lambda e, ps=ps: e.activation(o_f[:], ps[:], AF.Copy), reads=[pk], writes=["o_f"])
                    ps, pk = getps()
                    for h in range(4):
                        S.op("pe", lambda e, h=h, ps=ps: e.matmul(ps[:, h * 128:(h + 1) * 128], rKh[:, h, :],
                                                                  rv_b[:, h * 128:(h + 1) * 128], start=True, stop=True),
                             reads=["rKh", "rv_b"], writes=[pk])
                    S.op("dve", lambda e: e.tensor_tensor(RSf[:], RSf[:], bc3(qkw[:, 8:12], 4, 128), ALU.mult),
                         reads=["RSf", "qkw"], writes=["RSf"])
                    S.op("dve", lambda e, ps=ps: e.tensor_tensor(RSf[:], RSf[:], v3(ps[:], 4, 128), ALU.add),
                         reads=[pk, "RSf"], writes=["RSf"])
                    S.op("pool", lambda e: e.tensor_copy(RSb[:], RSf[:]), reads=["RSf"], writes=["RSb"])
                    S.op("pool", lambda e: e.tensor_tensor(t1[:], o_f[:], o_f[:], ALU.mult), reads=["o_f"], writes=["t1"])
                    S.op("dve", lambda e: e.tensor_reduce(s4[:], v3(t1[:], 4, 128), AX.X, ALU.add), reads=["t1"], writes=["s4"])
                    S.op("dve", lambda e: e.tensor_scalar(s4[:], s4[:], 1.0 / 128, 1e-6, ALU.mult, ALU.add), reads=["s4"], writes=["s4"])
                    S.op("act", lambda e: e.activation(s4[:], s4[:], AF.Sqrt), reads=["s4"], writes=["s4"])
                    S.op("dve", lambda e: e.reciprocal(s4[:], s4[:]), reads=["s4"], writes=["s4"])
                    S.op("dve", lambda e: e.tensor_tensor(v3(o_f[:], 4, 128), v3(o_f[:], 4, 128), bc3(s4[:], 4, 128), ALU.mult),
                         reads=["o_f", "s4"], writes=["o_f"])
                    S.op("pool", lambda e: e.tensor_tensor(mixed[:, 512:1024], o_f[:], rg_s[:], ALU.mult),
                         reads=["o_f", "rg_s"], writes=["mixed"])

                    chk("E")
                    ps, pk = getps()
                    S.op("pe", lambda e, ps=ps: e.matmul(ps[:], lwT[:], wal[:, 0:512], start=True, stop=True),
                         reads=["lwT", "wal"], writes=[pk])
                    S.op("dve", lambda e, ps=ps: e.tensor_tensor(t0[:], ps[:], bcn["w0"][:], ALU.add),
                         reads=[pk, "bc_w0"], writes=["t0"])
                    S.op("act", lambda e: e.activation(sigw[:], t0[:], AF.Sigmoid), reads=["t0"], writes=["sigw"])
                    ps, pk = getps()
                    S.op("pe", lambda e, ps=ps: e.matmul(ps[:], lwT[:], wal[:, 512:1024], start=True, stop=True),
                         reads=["lwT", "wal"], writes=[pk])
                    S.op("dve", lambda e, ps=ps: e.tensor_tensor(t1[:], ps[:], bcn["a0"][:], ALU.add),
                         reads=[pk, "bc_a0"], writes=["t1"])
                    S.op("act", lambda e: e.activation(a_f[:], t1[:], AF.Sigmoid), reads=["t1"], writes=["a_f"])
                    ps, pk = getps()
                    S.op("pe", lambda e, ps=ps: e.matmul(ps[:], lgT[:], glu[:], start=True, stop=True),
                         reads=["lgT", "glu"], writes=[pk])
                    S.op("act", lambda e, ps=ps: e.activation(g_f[:], ps[:], AF.Copy), reads=[pk], writes=["g_f"])
                    S.op("pool", lambda e: e.tensor_copy(sighl[:, 0:512], sigw[:]), reads=["sigw"], writes=["sighl"])
                    S.op("dve", lambda e: e.tensor_tensor(sighl[:, 512:1024], sigw[:], sighl[:, 0:512], ALU.subtract),
                         reads=["sigw", "sighl"], writes=["sighl"])
                    ps, pk = getps()
                    for hl in range(2):
                        S.op("pe", lambda e, ps=ps, hl=hl: e.matmul(ps[:], tri_i[:], sighl[:, hl * 512:(hl + 1) * 512],
                                                                    start=(hl == 0), stop=(hl == 1)),
                             reads=["tri_i", "sighl"], writes=[pk])
                    S.op("act", lambda e, ps=ps: e.activation(Wi[:], ps[:], AF.Exp, scale=-C_DEC), reads=[pk], writes=["Wi"])
                    S.op("act", lambda e, ps=ps: e.activation(Wn[:], ps[:], AF.Exp, scale=C_DEC), reads=[pk], writes=["Wn"])
                    S.op("act", lambda e: e.activation(t0[:], sigw[:], AF.Exp, scale=C_DEC), reads=["sigw"], writes=["t0"])
                    S.op("dve", lambda e: e.tensor_tensor(We[:], Wi[:], t0[:], ALU.mult), reads=["Wi", "t0"], writes=["We"])
                    ps, pk = getps()
                    for hl in range(2):
                        S.op("pe", lambda e, ps=ps, hl=hl: e.matmul(ps[:], tri_r[:], sighl[:, hl * 512:(hl + 1) * 512],
                                                                    start=(hl == 0), stop=(hl == 1)),
                             reads=["tri_r", "sighl"], writes=[pk])
                    S.op("act", lambda e, ps=ps: e.activation(Wh[:], ps[:], AF.Exp, scale=-C_DEC), reads=[pk], writes=["Wh"])
                    ps, pk = getps()
                    for pr in range(4):
                        for hl in range(2):
                            S.op("pe", lambda e, pr=pr, ps=ps, hl=hl: e.matmul(
                                ps[:, pr * 2:pr * 2 + 2], sighl[:, hl * 512 + pr * 128:hl * 512 + (pr + 1) * 128],
                                seqind[:], start=(hl == 0), stop=(hl == 1)),
                                reads=["sighl", "seqind"], writes=[pk])
                    WC4 = WC[:].rearrange("p (pr par) -> p pr par", par=2)
                    for s in range(2):
                        for par in range(2):
                            S.op("act", lambda e, s=s, par=par, ps=ps: e.activation(
                                WC4[s * 64:(s + 1) * 64, :, par],
                                ps[par * 64:(par + 1) * 64, 0:8].rearrange("p (pr s) -> p pr s", s=2)[:, :, s], AF.Exp, scale=-C_DEC),
                                reads=[pk], writes=["WC"])

                    chk("F")
                    S.op("pool", lambda e: e.tensor_tensor(t1[:], k_f[:], bcn["k_k"][:], ALU.mult),
                         reads=["k_f", "bc_k_k"], writes=["t1"])
                    S.op("pool", lambda e: e.tensor_tensor(t0[:], t1[:], t1[:], ALU.mult), reads=["t1"], writes=["t0"])
                    S.op("dve", lambda e: e.tensor_reduce(s8[:], v3(t0[:], 8, 64), AX.X, ALU.add),
                         reads=["t0"], writes=["s8"])
                    S.op("dve", lambda e: e.tensor_scalar_max(s8[:], s8[:], 1e-24), reads=["s8"], writes=["s8"])
                    S.op("act", lambda e: e.activation(s8[:], s8[:], AF.Sqrt), reads=["s8"], writes=["s8"])
                    S.op("dve", lambda e: e.reciprocal(s8[:], s8[:]), reads=["s8"], writes=["s8"])
                    S.op("dve", lambda e: e.tensor_tensor(v3(kk[:], 8, 64), v3(t1[:], 8, 64), bc3(s8[:], 8, 64), ALU.mult),
                         reads=["t1", "s8"], writes=["kk"])
                    S.op("dve", lambda e: e.scalar_tensor_tensor(t0[:], a_f[:], -1.0, bcn["k_a"][:], ALU.add, ALU.mult),
                         reads=["a_f", "bc_k_a"], writes=["t0"])
                    S.op("dve", lambda e: e.scalar_tensor_tensor(k_h[:], t0[:], 1.0, k_f[:], ALU.add, ALU.mult),
                         reads=["t0", "k_f"], writes=["k_h"])
                    S.op("dve", lambda e: e.tensor_tensor(bp[:], kk[:], a_f[:], ALU.mult), reads=["kk", "a_f"], writes=["bp"])
                    S.op("dve", lambda e: e.scalar_tensor_tensor(tm4[0][:], kk[:], -1.0, We[:], ALU.mult, ALU.mult),
                         reads=["kk", "We"], writes=["tm0"])
                    S.op("pool", lambda e: e.tensor_tensor(tm4[1][:], r_f[:], Wi[:], ALU.mult), reads=["r_f", "Wi"], writes=["tm1"])
                    S.op("dve", lambda e: e.tensor_tensor(tm4[2][:], bp[:], Wn[:], ALU.mult), reads=["bp", "Wn"], writes=["tm2"])
                    S.op("pool", lambda e: e.tensor_tensor(tm4[3][:], k_h[:], Wn[:], ALU.mult), reads=["k_h", "Wn"], writes=["tm3"])
                    chk("F1")
                    for s in range(2):
                        sl_ = slice(s * 64, (s + 1) * 64)
                        S.op("dve" if s == 0 else "pool", lambda e, s=s, sl_=sl_: e.tensor_tensor(
                            Bh[sl_, :, s * 64:(s + 1) * 64], v3(bp[sl_, :], 8, 64), v3(Wh[sl_, :], 8, 64), ALU.mult),
                            reads=["bp", "Wh"], writes=["Bh"])
                        S.op("pool" if s == 0 else "dve", lambda e, s=s, sl_=sl_: e.tensor_tensor(
                            Kh[sl_, :, s * 64:(s + 1) * 64], v3(k_h[sl_, :], 8, 64), v3(Wh[sl_, :], 8, 64), ALU.mult),
                            reads=["k_h", "Wh"], writes=["Kh"])
                    chk("F2")
                    S.op("pool", lambda e: e.tensor_tensor(t0[:], r_f[:], k_h[:], ALU.mult), reads=["r_f", "k_h"], writes=["t0"])
                    S.op("pool", lambda e: e.tensor_tensor(t0[:], t0[:], bcn["r_k"][:], ALU.mult),
                         reads=["t0", "bc_r_k"], writes=["t0"])
                    S.op("dve", lambda e: e.tensor_reduce(bon8[:], v3(t0[:], 8, 64), AX.X, ALU.add),
                         reads=["t0"], writes=["bon8"])
                    chk("F3")
                    dkeys = ["arT", "arT", "bT", "kT"]
                    for qi in range(4):
                        ps, pk = getps()
                        psb = ps[:].bitcast(BF16)
                        for pr in range(4):
                            S.op("pe", lambda e, qi=qi, pr=pr, psb=psb: e.transpose(
                                psb[:, pr * 128:(pr + 1) * 128], tm4[qi][:, pr * 128:(pr + 1) * 128], ident_b[:]),
                                reads=[f"tm{qi}", "ident_b"], writes=[pk])
                        pv = psb[:, 0:512].rearrange("p (pr t) -> p pr t", pr=4)
                        if qi < 2:
                            dst = arT[:, :, qi, :]
                        elif qi == 2:
                            dst = bT[:]
                        else:
                            dst = kT[:]
                        dst5 = dst.rearrange("p (pr par) t -> p pr par t", par=2)
                        k_ = 0
                        for par in range(2 if (not _os.environ.get("MIX_NOEVAC") or str(qi) in _os.environ.get("MIX_EVACQ", "")) else 0):
                            for s in range(2):
                                d_ = dst5[s * 64:(s + 1) * 64, :, par, s * 64:(s + 1) * 64]
                                i_ = pv[par * 64:(par + 1) * 64, :, s * 64:(s + 1) * 64]
                                S.op("dve", lambda e, d_=d_, i_=i_: e.tensor_copy(d_, i_),
                                     reads=[pk], writes=[dkeys[qi]])
                                k_ += 1

                    chk("G")
                    for hp in range(4):
                        ps, pk = getps()
                        for hh in range(2):
                            h = hp * 2 + hh
                            S.op("pe", lambda e, h=h, hh=hh, ps=ps: e.matmul(
                                ps[:, hh * 256:(hh + 1) * 256], bT[:, h, :], arT[:, h, :, :].rearrange("p a t -> p (a t)"), start=True, stop=True),
                                reads=["bT", "arT"], writes=[pk])
                        S.op("dve", lambda e, hp=hp, ps=ps: e.tensor_tensor(
                            QA[:, hp * 2:hp * 2 + 2, :, :].rearrange("p h a t -> p h (a t)"),
                            v3(ps[:], 2, 256), bch(m1[:], 2, 256), ALU.mult),
                            reads=[pk, "m1"], writes=["QA"])
                        ps, pk = getps()
                        for hh in range(2):
                            h = hp * 2 + hh
                            S.op("pe", lambda e, h=h, hh=hh, ps=ps: e.matmul(
                                ps[:, hh * 256:(hh + 1) * 256], kT[:, h, :], arT[:, h, :, :].rearrange("p a t -> p (a t)"), start=True, stop=True),
                                reads=["kT", "arT"], writes=[pk])
                        S.op("dve", lambda e, hp=hp, ps=ps: e.tensor_tensor(
                            KA[:, hp * 2:hp * 2 + 2, :, :].rearrange("p h a t -> p h (a t)"),
                            v3(ps[:], 2, 256), bch(m1[:], 2, 256), ALU.mult),
                            reads=[pk, "m1"], writes=["KA"])
                    for hq in range(2):
                        ps, pk = getps()
                        for hh in range(4):
                            h = hq * 4 + hh
                            S.op("pe", lambda e, h=h, hh=hh, ps=ps: e.matmul(
                                ps[:, hh * 128:(hh + 1) * 128], arT[:, h, 0, :], bT[:, h, :], start=True, stop=True),
                                reads=["bT", "arT"], writes=[pk])
                        S.op("dve", lambda e, hq=hq, ps=ps: e.tensor_tensor(
                            Pm[0][:, hq * 4:hq * 4 + 4, :], v3(ps[:], 4, 128), bch(msl[:], 4, 128), ALU.mult),
                            reads=[pk, "msl"], writes=["Pm0"])
                    chk("H")
                    S.op("pool", lambda e: e.tensor_tensor(TTbs[0][:], QA[:, :, 0, :], bch(ident_f[:], 8, 128), ALU.add),
                         reads=["QA", "ident_f"], writes=["TTb0"])
                    for lvl in range(1, 6):
                        pi_, po_ = (lvl - 1) % 2, lvl % 2

                        def Qprev(h, lvl=lvl, pi_=pi_):
                            return QA[:, h, 0, :] if lvl == 1 else Qm[pi_][:, h, :]
                        qprev_key = "QA" if lvl == 1 else f"Qm{pi_}"
                        for hq in range(2):
                            ps, pk = getps()
                            for hh in range(4):
                                h = hq * 4 + hh
                                S.op("pe", lambda e, h=h, hh=hh, ps=ps, Qprev=Qprev, pi_=pi_: e.matmul(
                                    ps[:, hh * 128:(hh + 1) * 128], Qprev(h), Pm[pi_][:, h, :], start=True, stop=True),
                                    reads=[qprev_key, f"Pm{pi_}"], writes=[pk])
                            S.op("dve", lambda e, hq=hq, ps=ps: e.tensor_tensor(
                                Pp[:, hq * 4:hq * 4 + 4, :], v3(ps[:], 4, 128), bch(ident_f[:], 4, 128), ALU.add),
                                reads=[pk, "ident_f"], writes=["Pp"])
                            if lvl < 5:
                                S.op("dve", lambda e, hq=hq, ps=ps, po_=po_: e.tensor_copy(
                                    Pm[po_][:, hq * 4:hq * 4 + 4, :], v3(ps[:], 4, 128)),
                                    reads=[pk], writes=[f"Pm{po_}"])
                        if lvl < 5:
                            for hq in range(2):
                                ps, pk = getps()
                                for hh in range(4):
                                    h = hq * 4 + hh
                                    S.op("pe", lambda e, h=h, hh=hh, ps=ps, Qprev=Qprev, pi_=pi_: e.matmul(
                                        ps[:, hh * 128:(hh + 1) * 128], Pm[pi_][:, h, :], Qprev(h), start=True, stop=True),
                                        reads=[qprev_key, f"Pm{pi_}"], writes=[pk])
                                S.op("act", lambda e, hq=hq, ps=ps, po_=po_: e.activation(
                                    Qm[po_][:, hq * 4:hq * 4 + 4, :], v3(ps[:], 4, 128), AF.Copy),
                                    reads=[pk], writes=[f"Qm{po_}"])
                        for hq in range(2):
                            ps, pk = getps()
                            for hh in range(4):
                                h = hq * 4 + hh
                                S.op("pe", lambda e, h=h, hh=hh, ps=ps, pi_=pi_: e.matmul(
                                    ps[:, hh * 128:(hh + 1) * 128], Pp[:, h, :], TTbs[pi_][:, h, :], start=True, stop=True),
                                    reads=["Pp", f"TTb{pi_}"], writes=[pk])
                            if hq == 0:
                                S.op("act", lambda e, hq=hq, ps=ps, po_=po_: e.activation(
                                    TTbs[po_][:, hq * 4:hq * 4 + 4, :], v3(ps[:], 4, 128), AF.Copy),
                                    reads=[pk], writes=[f"TTb{po_}"])
                            else:
                                S.op("dve", lambda e, hq=hq, ps=ps, po_=po_: e.tensor_copy(
                                    TTbs[po_][:, hq * 4:hq * 4 + 4, :], v3(ps[:], 4, 128)),
                                    reads=[pk], writes=[f"TTb{po_}"])
                    TTb = TTbs[1]

                    chk("I")
                    ps, pk = getps()
                    for h in range(8):
                        S.op("pe", lambda e, h=h, ps=ps: e.matmul(ps[:, h * 64:(h + 1) * 64], arT[:, h, 0, :], Sb[:, h, :],
                                                                  start=True, stop=False), reads=["arT", "Sb"], writes=[pk])
                        S.op("pe", lambda e, h=h, ps=ps: e.matmul(ps[:, h * 64:(h + 1) * 64], KA[:, h, 0, :],
                                                                  v_b[:, h * 64:(h + 1) * 64], start=False, stop=True),
                             reads=["KA", "v_b"], writes=[pk])
                    S.op("act", lambda e, ps=ps: e.activation(Xb[:], v3(ps[:], 8, 64), AF.Copy), reads=[pk], writes=["Xb"])
                    ps, pk = getps()
                    for h in range(8):
                        S.op("pe", lambda e, h=h, ps=ps: e.matmul(ps[:, h * 64:(h + 1) * 64], TTb[:, h, :], Xb[:, h, :],
                                                                  start=True, stop=True), reads=["TTb1", "Xb"], writes=[pk])
                    S.op("dve", lambda e, ps=ps: e.tensor_copy(Ub[:], v3(ps[:], 8, 64)), reads=[pk], writes=["Ub"])
                    ps, pk = getps()
                    for h in range(8):
                        S.op("pe", lambda e, h=h, ps=ps: e.matmul(ps[:, h * 64:(h + 1) * 64], arT[:, h, 1, :], Sb[:, h, :],
                                                                  start=True, stop=False), reads=["arT", "Sb"], writes=[pk])
                        S.op("pe", lambda e, h=h, ps=ps: e.matmul(ps[:, h * 64:(h + 1) * 64], QA[:, h, 1, :], Ub[:, h, :],
                                                                  start=False, stop=False), reads=["QA", "Ub"], writes=[pk])
                        S.op("pe", lambda e, h=h, ps=ps: e.matmul(ps[:, h * 64:(h + 1) * 64], KA[:, h, 1, :],
                                                                  v_b[:, h * 64:(h + 1) * 64], start=False, stop=True),
                             reads=["KA", "v_b"], writes=[pk])
                    S.op("act", lambda e, ps=ps: e.activation(y_f[:], ps[:], AF.Copy), reads=[pk], writes=["y_f"])
                    ps, pk = getps()
                    for h in range(8):
                        S.op("pe", lambda e, h=h, ps=ps: e.matmul(ps[:, h * 64:(h + 1) * 64], Bh[:, h, :], Ub[:, h, :],
                                                                  start=True, stop=False), reads=["Bh", "Ub"], writes=[pk])
                        S.op("pe", lambda e, h=h, ps=ps: e.matmul(ps[:, h * 64:(h + 1) * 64], Kh[:, h, :],
                                                                  v_b[:, h * 64:(h + 1) * 64], start=False, stop=True),
                             reads=["Kh", "v_b"], writes=[pk])
                    S.op("dve", lambda e: e.tensor_tensor(Sf[:], Sf[:], bc3(WC[:], 8, 64), ALU.mult),
                         reads=["Sf", "WC"], writes=["Sf"])
                    S.op("dve", lambda e, ps=ps: e.tensor_tensor(Sf[:], Sf[:], v3(ps[:], 8, 64), ALU.add),
                         reads=[pk, "Sf"], writes=["Sf"])
                    S.op("pool", lambda e: e.tensor_copy(Sb[:], Sf[:]), reads=["Sf"], writes=["Sb"])

                    chk("J")
                    y3 = v3(y_f[:], 8, 64)
                    S.op("dve", lambda e: e.tensor_reduce(s8[:], y3, AX.X, ALU.add), reads=["y_f"], writes=["s8"])
                    S.op("dve", lambda e: e.tensor_scalar(s8[:], s8[:], 1.0 / 64, None, ALU.mult), reads=["s8"], writes=["s8"])
                    S.op("dve", lambda e: e.tensor_tensor(y3, y3, bc3(s8[:], 8, 64), ALU.subtract),
                         reads=["y_f", "s8"], writes=["y_f"])
                    S.op("pool", lambda e: e.tensor_tensor(t0[:], y_f[:], y_f[:], ALU.mult), reads=["y_f"], writes=["t0"])
                    S.op("dve", lambda e: e.tensor_reduce(s8[:], v3(t0[:], 8, 64), AX.X, ALU.add), reads=["t0"], writes=["s8"])
                    S.op("dve", lambda e: e.tensor_scalar(s8[:], s8[:], 1.0 / 64, 64e-5, ALU.mult, ALU.add),
                         reads=["s8"], writes=["s8"])
                    S.op("act", lambda e: e.activation(s8[:], s8[:], AF.Sqrt), reads=["s8"], writes=["s8"])
                    S.op("dve", lambda e: e.reciprocal(s8[:], s8[:]), reads=["s8"], writes=["s8"])
                    S.op("dve", lambda e: e.tensor_tensor(y3, y3, bc3(s8[:], 8, 64), ALU.mult), reads=["y_f", "s8"], writes=["y_f"])
                    S.op("pool", lambda e: e.tensor_tensor(y_f[:], y_f[:], bcn["ln_x_w"][:], ALU.mult),
                         reads=["y_f", "bc_ln_x_w"], writes=["y_f"])
                    S.op("pool", lambda e: e.tensor_tensor(y_f[:], y_f[:], bcn["ln_x_b"][:], ALU.add),
                         reads=["y_f", "bc_ln_x_b"], writes=["y_f"])
                    S.op("dve", lambda e: e.tensor_tensor(v3(t0[:], 8, 64), v3(v_b[:], 8, 64), bc3(bon8[:], 8, 64), ALU.mult),
                         reads=["v_b", "bon8"], writes=["t0"])
                    S.op("pool", lambda e: e.tensor_tensor(y_f[:], y_f[:], t0[:], ALU.add), reads=["y_f", "t0"], writes=["y_f"])
                    S.op("dve", lambda e: e.tensor_tensor(mixed[:, 0:512], y_f[:], g_f[:], ALU.mult),
                         reads=["y_f", "g_f"], writes=["mixed"])

                    chk("L")
                    ps, pk = getps()
                    psb = ps[:].bitcast(BF16)
                    for c in range(8):
                        S.op("pe", lambda e, c=c, psb=psb: e.transpose(psb[:, c * 128:(c + 1) * 128],
                                                                       mixed[:, c * 128:(c + 1) * 128], ident_b[:]),
                             reads=["mixed", "ident_b"], writes=[pk])
                    S.op("act", lambda e, psb=psb: e.activation(mT[:], psb.rearrange("p (c t) -> p c t", c=8), AF.Copy),
                         reads=[pk], writes=["mT"])
                    for nh in range(2):
                        ps, pk = getps()
                        for c in range(8):
                            S.op("pe", lambda e, c=c, nh=nh, ps=ps: e.matmul(ps[:], mT[:, c, :], wo[:, c, nh * 512:(nh + 1) * 512],
                                                                             start=(c == 0), stop=(c == 7)),
                                 reads=["mT", "wo"], writes=[pk])
                        S.op("dve", lambda e, nh=nh, ps=ps, xt=xt: e.tensor_tensor(xt[:, nh * 512:(nh + 1) * 512],
                                                                            xt[:, nh * 512:(nh + 1) * 512], ps[:], ALU.add),
                             reads=[pk, mxk], writes=[mxk])
                  except _StopTile:
                    pass
                  for s in range(2):
                        r0 = s * seq + n * 64
                        S.dma("sp", out[r0:r0 + 64, :], xt[s * 64:(s + 1) * 64, :], reads=[mxk], sem=f"ms{n % 2}")
                S.barrier()
            src_x[0] = out

        for l in range(depth):
            if do_ffn:
                ffn_phase(l, 1, False)
            if do_mix:
                mix_phase(l)
            if do_ffn:
                ffn_phase(l, 2, final_norm and l == depth - 1)
        S.emit(final_waits=[k for k in S.dma_counts if k.startswith("fs") or k.startswith("ms")])
    return nc


_CACHE = {}


def kernel(**inputs):
    x = np.ascontiguousarray(inputs["x"], dtype=np.float32)
    B, seq, d = x.shape
    depth = inputs["w_in"].shape[0]
    key = (seq, depth)
    if key not in _CACHE:
        _CACHE[key] = build_program(seq, depth)
    nc = _CACHE[key]
    consts = make_consts(seq)
    shared = {}
    for k, v in inputs.items():
        if k == "x":
            continue
        a = np.ascontiguousarray(v, dtype=np.float32)
        if k == "r_k":
            a = a.reshape(depth, 512)
        if k == "final_norm":
            a = a.reshape(1, D)
        shared[k] = a
    for k in CONST_ORDER:
        shared["c_" + k] = consts[k]
    ncores = B // 2
    in_maps = []
    for c in range(ncores):
        m = dict(shared)
        m["x"] = x[2 * c:2 * c + 2].reshape(2 * seq, d)
        in_maps.append(m)
    res = run_bass_kernel_spmd(nc, in_maps, core_ids=list(range(ncores)))
    outs = [r["out"].reshape(2, seq, d) for r in res.results]
    return np.concatenate(outs, axis=0).astype(np.float32)
```

```python
import contextlib
import numpy as np
import concourse.bass as bass
import concourse.mybir as mybir
from concourse.bass_utils import run_bass_kernel_spmd

F32 = mybir.dt.float32
BF16 = mybir.dt.bfloat16
AF = mybir.ActivationFunctionType
ALU = mybir.AluOpType
AX = mybir.AxisListType

D = 1024
DFF = 2816
NF = DFF // 128
PROJ = 3328
RW_IN = 1792
NCORES = 8
ENGS = ("pe", "act", "dve", "pool", "sp")
C_DEC = float(np.exp(-0.5))


class _StopTile(Exception):
    pass


class _Op:
    __slots__ = ("eng", "fn", "deps", "dma_deps", "idx", "needs_inc", "count",
                 "dma_sem", "epoch")


class Sched:
    def __init__(self, nc):
        self.nc = nc
        self.streams = {e: [] for e in ENGS}
        self.last_w = {}
        self.readers = {}
        self.seen = {e: {} for e in ENGS}
        self.seen_dma = {e: {} for e in ENGS}
        self.dma_counts = {}
        self.epoch = 0
        self.alias = {}

    def _new(self, eng, fn):
        o = _Op()
        o.eng = eng
        o.fn = fn
        o.deps = []
        o.dma_deps = []
        o.idx = len(self.streams[eng])
        o.needs_inc = False
        o.count = None
        o.dma_sem = None
        o.epoch = self.epoch
        return o

    def _collect(self, op, reads, writes):
        deps = []
        for k in reads:
            w = self.last_w.get(k)
            if w is not None:
                deps.append((w, "raw"))
        for k in writes:
            w = self.last_w.get(k)
            if w is not None:
                deps.append((w, "waw"))
            for r in self.readers.get(k, ()):
                deps.append((r, "war"))
        e = op.eng
        for d, kind in deps:
            if d is op:
                continue
            if d.dma_sem is not None:
                cnt = self.dma_counts[d.dma_sem]
                if self.seen_dma[e].get(d.dma_sem, 0) < cnt:
                    self.seen_dma[e][d.dma_sem] = cnt
                    op.dma_deps.append((d.dma_sem, cnt))
                continue
            if d.epoch != self.epoch:
                continue
            if d.eng == e and e == "pe":
                continue
            if self.seen[e].get(d.eng, -1) >= d.idx:
                continue
            self.seen[e][d.eng] = d.idx
            d.needs_inc = True
            op.deps.append(d)
        for k in reads:
            self.readers.setdefault(k, []).append(op)
        for k in writes:
            self.last_w[k] = op
            self.readers[k] = []

    def op(self, eng, fn, reads=(), writes=()):
        reads = [self.alias.get(k, k) for k in reads]
        writes = [self.alias.get(k, k) for k in writes]
        o = self._new(eng, fn)
        self._collect(o, reads, writes)
        self.streams[eng].append(o)
        return o

    def dma(self, queue, out, in_, reads=(), writes=(), sem="dma0"):
        def fn(eng, out=out, in_=in_):
            return eng.dma_start(out=out, in_=in_)
        sem = f"{sem}_{queue}"
        o = self.op(queue, fn, reads, writes)
        o.dma_sem = sem
        self.dma_counts[sem] = self.dma_counts.get(sem, 0) + 1
        return o

    def barrier(self):
        lasts = {}
        for e in ENGS:
            for o in reversed(self.streams[e]):
                if o.epoch != self.epoch:
                    break
                if o.dma_sem is None and o.fn is not None:
                    lasts[e] = o
                    break
        for e in ENGS:
            o = self._new(e, None)
            for e2, l in lasts.items():
                if e2 == e and e == "pe":
                    continue
                if self.seen[e].get(e2, -1) < l.idx:
                    l.needs_inc = True
                    o.deps.append(l)
            for s, c in self.dma_counts.items():
                if self.seen_dma[e].get(s, 0) < c:
                    self.seen_dma[e][s] = c
                    o.dma_deps.append((s, c))
            self.streams[e].append(o)
        self.epoch += 1
        self.seen = {e: {} for e in ENGS}

    def emit(self, final_waits=()):
        nc = self.nc
        n_epochs = self.epoch + 1
        for e in ENGS:
            c = 0
            ep = 0
            for o in self.streams[e]:
                if o.epoch != ep:
                    ep = o.epoch
                    c = 0
                if o.needs_inc:
                    c += 1
                    o.count = c
        with contextlib.ExitStack() as st:
            esem = {}
            for e in ENGS:
                used = set(o.epoch for o in self.streams[e] if o.needs_inc)
                for ep in sorted(used):
                    esem[(e, ep)] = st.enter_context(nc.semaphore(f"s_{e}_{ep}"))
            dsem = {s: st.enter_context(nc.semaphore(f"d_{s}")) for s in self.dma_counts}
            block = st.enter_context(nc.Block())

            def replay(e, eng):
                for o in self.streams[e]:
                    for d in o.deps:
                        eng.wait_ge(esem[(d.eng, d.epoch)], d.count)
                    for s, c in o.dma_deps:
                        eng.wait_ge(dsem[s], 16 * c)
                    if o.fn is None:
                        continue
                    ins = o.fn(eng)
                    if o.dma_sem is not None:
                        ins.then_inc(dsem[o.dma_sem], 16)
                    elif o.needs_inc:
                        ins.then_inc(esem[(o.eng, o.epoch)], 1)
                if e == "sp":
                    for s in final_waits:
                        eng.wait_ge(dsem[s], 16 * self.dma_counts[s])

            @block.tensor
            def _(eng):
                replay("pe", eng)

            @block.scalar
            def _(eng):
                replay("act", eng)

            @block.vector
            def _(eng):
                replay("dve", eng)

            @block.gpsimd
            def _(eng):
                replay("pool", eng)

            @block.sync
            def _(eng):
                replay("sp", eng)


def make_consts(seq):
    nt = seq // 64
    p = np.arange(128)
    s_of = p // 64
    t_of = p % 64
    same = (s_of[:, None] == s_of[None, :])
    c = {}
    c["ident"] = np.eye(128, dtype=np.float32)
    c["tri_i"] = (1.0 * (same & (t_of[:, None] <= t_of[None, :]))).astype(np.float32)
    c["tri_r"] = (1.0 * (same & (t_of[:, None] > t_of[None, :]))).astype(np.float32)
    seqind = np.zeros((128, 2), np.float32)
    seqind[p, s_of] = 1.0
    c["seqind"] = seqind
    su = (same & (t_of[:, None] < t_of[None, :])).astype(np.float32)
    ui = (same & (t_of[:, None] <= t_of[None, :])).astype(np.float32)
    sl = (same & (t_of[:, None] > t_of[None, :])).astype(np.float32)
    c["m1"] = np.concatenate([su, ui], axis=1)
    c["msl"] = sl
    H = 4
    log_g = np.log(1.0 - np.power(2.0, -5.0 - np.arange(H, dtype=np.float64)))
    j = t_of[:, None].astype(np.float64)
    i = t_of[None, :].astype(np.float64)
    dm = np.zeros((128, H, 128), np.float64)
    for h in range(H):
        dm[:, h, :] = same * np.exp(log_g[h] * (np.abs(i - j) - (i + 1.0))) * 0.125
    c["dmask"] = dm.reshape(128, H * 128).astype(np.float32)
    qw = np.exp(log_g[None, :] * (t_of[:, None] + 1.0))
    kw = np.exp(log_g[None, :] * (63.0 - t_of[:, None])) * 0.125
    cd = np.broadcast_to(np.exp(log_g * 64.0)[None, :], (128, H))
    c["qkw"] = np.concatenate([qw, kw, cd], axis=1).astype(np.float32)
    half = 32
    inv_freq = (1.0 / (np.float32(10000.0) ** np.linspace(0.0, 1.0, half, dtype=np.float32))).astype(np.float32)
    pos = np.arange(seq, dtype=np.float32)
    ang = (pos[:, None] * inv_freq[None, :]).astype(np.float32).astype(np.float64)
    cs = np.concatenate([np.cos(ang), np.sin(ang)], axis=1).astype(np.float32)
    cs = cs.reshape(nt, 64, 64).transpose(1, 0, 2)
    c["rope"] = np.ascontiguousarray(np.concatenate([cs, cs], axis=0).reshape(128, nt * 64))
    return c


CONST_ORDER = ["ident", "tri_i", "tri_r", "seqind", "m1", "msl", "dmask", "qkw", "rope"]


def build_program(seq, depth, do_ffn=True, do_mix=True, final_norm=True):
    nc = bass.Bass("TRN2", target_bir_lowering=False)
    ntok = 2 * seq
    nt = seq // 64

    def din(name, shape):
        return nc.dram_tensor(name, list(shape), F32, kind="ExternalInput").ap()

    x_in = din("x", [ntok, D])
    w = {}
    for nm, shp in [("ffn1_norm", [depth, D]), ("ffn1_w_gate", [depth, D, DFF]), ("ffn1_w_up", [depth, D, DFF]),
                    ("ffn1_w_down", [depth, DFF, D]), ("mix_norm", [depth, D]), ("w_in", [depth, D, PROJ]),
                    ("shift_mu", [depth, RW_IN]), ("w0", [depth, 512]), ("w_lora_up", [depth, 64, 512]),
                    ("a0", [depth, 512]), ("a_lora_up", [depth, 64, 512]), ("g_lora_up", [depth, 128, 512]),
                    ("k_k", [depth, 512]), ("k_a", [depth, 512]), ("r_k", [depth, 512]),
                    ("ln_x_w", [depth, 512]), ("ln_x_b", [depth, 512]), ("w_out", [depth, D, D]),
                    ("ffn2_norm", [depth, D]), ("ffn2_w_gate", [depth, D, DFF]), ("ffn2_w_up", [depth, D, DFF]),
                    ("ffn2_w_down", [depth, DFF, D]), ("final_norm", [1, D])]:
        w[nm] = din(nm, shp)
    cshape = {"ident": 128, "tri_i": 128, "tri_r": 128, "seqind": 2, "m1": 256, "msl": 128,
              "dmask": 512, "qkw": 12, "rope": nt * 64}
    cd = {k: din("c_" + k, [128, v]) for k, v in cshape.items()}
    out = nc.dram_tensor("out", [ntok, D], F32, kind="ExternalOutput").ap()

    S = Sched(nc)
    st = contextlib.ExitStack()
    with st:
        uid = [0]

        def sb(name, shape, dt=F32, stack=st):
            uid[0] += 1
            return stack.enter_context(nc.sbuf_tensor(f"{name}_{uid[0]}", list(shape), dt))

        banks = [st.enter_context(nc.psum_tensor(f"ps{i}", [128, 512], F32)) for i in range(8)]
        pctr = [0]

        def getps():
            i = pctr[0] % 8
            pctr[0] += 1
            return banks[i], f"ps{i}"

        ident_b = sb("ident_b", [128, 128], BF16)
        tri_i = sb("tri_i", [128, 128], BF16)
        tri_r = sb("tri_r", [128, 128], BF16)
        seqind = sb("seqind", [128, 2], BF16)
        m1 = sb("m1", [128, 256])
        msl = sb("msl", [128, 128])
        ident_f = sb("ident_f", [128, 128])
        dmask = sb("dmask", [128, 512])
        qkw = sb("qkw", [128, 12])
        S.dma("pool", ident_b[:], cd["ident"], writes=["ident_b"], sem="c")
        S.dma("sp", ident_f[:], cd["ident"], writes=["ident_f"], sem="c")
        S.dma("pool", tri_i[:], cd["tri_i"], writes=["tri_i"], sem="c")
        S.dma("pool", tri_r[:], cd["tri_r"], writes=["tri_r"], sem="c")
        S.dma("pool", seqind[:], cd["seqind"], writes=["seqind"], sem="c")
        S.dma("sp", m1[:], cd["m1"], writes=["m1"], sem="c")
        S.dma("sp", msl[:], cd["msl"], writes=["msl"], sem="c")
        S.dma("sp", dmask[:], cd["dmask"], writes=["dmask"], sem="c")
        S.dma("sp", qkw[:], cd["qkw"], writes=["qkw"], sem="c")

        src_x = [x_in]

        def rstd_ops(ss, n, tag):
            S.op("dve", lambda e: e.tensor_scalar(ss[:, 0:n], ss[:, 0:n], 1.0 / D, 1e-6, ALU.mult, ALU.add),
                 reads=[tag], writes=[tag])
            S.op("act", lambda e: e.activation(ss[:, 0:n], ss[:, 0:n], AF.Sqrt), reads=[tag], writes=[tag])
            S.op("dve", lambda e: e.reciprocal(ss[:, 0:n], ss[:, 0:n]), reads=[tag], writes=[tag])

        def ffn_phase(l, which, last):
            TB = 256
            nblk = ntok // TB
            with contextlib.ExitStack() as fs:
                wg = sb("wg", [128, 8, DFF], BF16, fs)
                wu = sb("wu", [128, 8, DFF], BF16, fs)
                wd = sb("wd", [128, NF, D], BF16, fs)
                gain = sb("gain", [128, D], F32, fs)
                xt = [sb(f"fx{i}", [128, 2, D], F32, fs) for i in range(2)]
                hb = sb("fh", [128, 2, D], BF16, fs)
                hT = sb("fhT", [128, 8, TB], BF16, fs)
                aT = sb("faT", [128, NF, TB], BF16, fs)
                sg = [sb(f"fsg{i}", [128, TB], F32, fs) for i in range(2)]
                junk = sb("fjunk", [128, D], BF16, fs)
                ss = [sb(f"fss{i}", [128, 4], F32, fs) for i in range(2)]
                if last:
                    fin_bc = sb("fin_bc", [128, D], F32, fs)
                    S.dma("sp", fin_bc[:], w["final_norm"][0:1, :].broadcast_to([128, D]), writes=["fin_bc"], sem="w")
                pre = "ffn1" if which == 1 else "ffn2"
                S.dma("sp", gain[:], w[pre + "_norm"][l:l + 1, :].broadcast_to([128, D]), writes=["gain"], sem="w")
                for c in range(8):
                    S.dma("pool", wg[:, c, :], w[pre + "_w_gate"][l, c * 128:(c + 1) * 128, :], writes=["wg"], sem="wgu")
                    S.dma("pool", wu[:, c, :], w[pre + "_w_up"][l, c * 128:(c + 1) * 128, :], writes=["wu"], sem="wgu")
                for f in range(NF):
                    S.dma("pool", wd[:, f, :], w[pre + "_w_down"][l, f * 128:(f + 1) * 128, :], writes=["wd"], sem="wd")

                def load(b):
                    i = b % 2
                    src = src_x[0]
                    for j in range(2):
                        r0 = b * TB + j * 128
                        S.dma("sp", xt[i][:, j, :], src[r0:r0 + 128, :], writes=[f"fx{i}"], sem=f"fl{i}")

                def norm(b):
                    i = b % 2
                    X = xt[i]
                    xk = f"fx{i}"
                    ssb = ss[i]
                    sk = f"fss{i}"
                    for j in range(2):
                        S.op("act", lambda e, j=j, X=X, ssb=ssb: e.activation(junk[:], X[:, j, :], AF.Square,
                                                                              accum_out=ssb[:, j:j + 1]),
                             reads=[xk], writes=["fjunk", sk])
                    rstd_ops(ssb, 2, sk)
                    for j in range(2):
                        S.op("dve", lambda e, j=j, X=X, ssb=ssb: e.scalar_tensor_tensor(
                            hb[:, j, :], X[:, j, :], ssb[:, j:j + 1], gain[:], ALU.mult, ALU.mult),
                            reads=[xk, sk, "gain"], writes=["fh"])

                load(0)
                norm(0)
                for b in range(nblk):
                    i = b % 2
                    X = xt[i]
                    xk = f"fx{i}"
                    if b + 1 < nblk:
                        load(b + 1)
                    ssb = ss[i]
                    sk = f"fss{i}"
                    for half in range(2):
                        ps, pk = getps()
                        psb = ps[:].bitcast(BF16)
                        for cc in range(4):
                            c = half * 4 + cc
                            for j in range(2):
                                S.op("pe", lambda e, c=c, cc=cc, j=j, psb=psb: e.transpose(
                                    psb[:, cc * 256 + j * 128: cc * 256 + (j + 1) * 128],
                                    hb[:, j, c * 128:(c + 1) * 128], ident_b[:]),
                                    reads=["fh", "ident_b"], writes=[pk])
                        eng = "act" if half == 0 else "dve"
                        if eng == "act":
                            S.op("act", lambda e, half=half, psb=psb: e.activation(
                                hT[:, half * 4:(half + 1) * 4, :], psb.rearrange("p (c t) -> p c t", c=4), AF.Copy),
                                reads=[pk], writes=["fhT"])
                        else:
                            S.op("dve", lambda e, half=half, psb=psb: e.tensor_copy(
                                hT[:, half * 4:(half + 1) * 4, :], psb.rearrange("p (c t) -> p c t", c=4)),
                                reads=[pk], writes=["fhT"])
                    for f in range(NF):
                        ps, pk = getps()
                        for c in range(8):
                            S.op("pe", lambda e, c=c, f=f, ps=ps: e.matmul(
                                ps[:, 0:TB], wg[:, c, f * 128:(f + 1) * 128], hT[:, c, :],
                                start=(c == 0), stop=(c == 7)), reads=["wg", "fhT"], writes=[pk])
                        for c in range(8):
                            S.op("pe", lambda e, c=c, f=f, ps=ps: e.matmul(
                                ps[:, TB:2 * TB], wu[:, c, f * 128:(f + 1) * 128], hT[:, c, :],
                                start=(c == 0), stop=(c == 7)), reads=["wu", "fhT"], writes=[pk])
                        sgb = sg[f % 2]
                        sgk = f"fsg{f % 2}"
                        S.op("act", lambda e, ps=ps, sgb=sgb: e.activation(sgb[:], ps[:, 0:TB], AF.Silu),
                             reads=[pk], writes=[sgk])
                        S.op("dve", lambda e, ps=ps, sgb=sgb, f=f: e.tensor_tensor(
                            aT[:, f, :], sgb[:], ps[:, TB:2 * TB], ALU.mult),
                            reads=[pk, sgk], writes=["faT"])
                    if b + 1 < nblk:
                        norm(b + 1)
                    for j in range(2):
                        for n in range(2):
                            ps, pk = getps()
                            for f in range(NF):
                                S.op("pe", lambda e, f=f, j=j, n=n, ps=ps: e.matmul(
                                    ps[:], aT[:, f, j * 128:(j + 1) * 128], wd[:, f, n * 512:(n + 1) * 512],
                                    start=(f == 0), stop=(f == NF - 1)), reads=["faT", "wd"], writes=[pk])
                            S.op("dve", lambda e, j=j, n=n, ps=ps, X=X: e.scalar_tensor_tensor(
                                X[:, j, n * 512:(n + 1) * 512], ps[:], 0.5, X[:, j, n * 512:(n + 1) * 512],
                                ALU.mult, ALU.add), reads=[pk, xk], writes=[xk])
                    if last:
                        for j in range(2):
                            S.op("act", lambda e, j=j, X=X, ssb=ssb: e.activation(
                                junk[:], X[:, j, :], AF.Square, accum_out=ssb[:, 2 + j:3 + j]),
                                reads=[xk], writes=["fjunk", sk])
                        S.op("dve", lambda e, ssb=ssb: e.tensor_scalar(ssb[:, 2:4], ssb[:, 2:4], 1.0 / D, 1e-6,
                                                                       ALU.mult, ALU.add), reads=[sk], writes=[sk])
                        S.op("act", lambda e, ssb=ssb: e.activation(ssb[:, 2:4], ssb[:, 2:4], AF.Sqrt),
                             reads=[sk], writes=[sk])
                        S.op("dve", lambda e, ssb=ssb: e.reciprocal(ssb[:, 2:4], ssb[:, 2:4]), reads=[sk], writes=[sk])
                        for j in range(2):
                            S.op("dve", lambda e, j=j, X=X, ssb=ssb: e.scalar_tensor_tensor(
                                X[:, j, :], X[:, j, :], ssb[:, 2 + j:3 + j], fin_bc[:], ALU.mult, ALU.mult),
                                reads=[xk, sk, "fin_bc"], writes=[xk])
                    for j in range(2):
                        r0 = b * TB + j * 128
                        S.dma("sp", out[r0:r0 + 128, :], X[:, j, :], reads=[xk], sem=f"fs{i}")
                S.barrier()
            src_x[0] = out

        def mix_phase(l):
            with contextlib.ExitStack() as ms:
                wm = sb("wm", [128, 8, 5120], BF16, ms)
                wo = sb("wo", [128, 8, D], BF16, ms)
                wal = sb("wal", [128, 1024], BF16, ms)
                glu = sb("glu", [128, 512], BF16, ms)
                gain = sb("mgain", [128, D], F32, ms)
                bcn = {}
                for nm in ["w0", "a0", "k_k", "k_a", "r_k", "ln_x_w", "ln_x_b"]:
                    bcn[nm] = sb("bc_" + nm, [128, 512], F32, ms)
                    S.dma("sp", bcn[nm][:], w[nm][l:l + 1, :].broadcast_to([128, 512]), writes=["bc_" + nm], sem="w")
                S.dma("sp", gain[:], w["mix_norm"][l:l + 1, :].broadcast_to([128, D]), writes=["mgain"], sem="w")
                S.op("pool", lambda e: e.memset(wal[:], 0.0), writes=["wal"])
                S.dma("pool", wal[0:64, 0:512], w["w_lora_up"][l], writes=["wal"], sem="w")
                S.dma("pool", wal[64:128, 512:1024], w["a_lora_up"][l], writes=["wal"], sem="w")
                S.dma("pool", glu[:], w["g_lora_up"][l], writes=["glu"], sem="w")
                for c in range(8):
                    S.dma("pool", wo[:, c, :], w["w_out"][l, c * 128:(c + 1) * 128, :], writes=["wo"], sem="w")
                    S.dma("pool", wm[:, c, 3584:5120], w["w_in"][l, c * 128:(c + 1) * 128, RW_IN:PROJ],
                          writes=["wm"], sem="w")
                with contextlib.ExitStack() as ps_:
                    mu = sb("mu", [128, RW_IN], F32, ps_)
                    omm = sb("omm", [128, RW_IN], F32, ps_)
                    stg = [sb(f"stg{i}", [128, RW_IN], F32, ps_) for i in range(2)]
                    S.dma("sp", mu[:], w["shift_mu"][l:l + 1, :].broadcast_to([128, RW_IN]), writes=["mu"], sem="w")
                    S.op("dve", lambda e: e.tensor_scalar(omm[:], mu[:], -1.0, 1.0, ALU.mult, ALU.add),
                         reads=["mu"], writes=["omm"])
                    for c in range(8):
                        sg_ = stg[c % 2]
                        sk_ = f"stg{c % 2}"
                        S.dma("sp", sg_[:], w["w_in"][l, c * 128:(c + 1) * 128, 0:RW_IN], writes=[sk_], sem=f"wp{c % 2}")
                        S.op("dve", lambda e, c=c, sg_=sg_: e.tensor_tensor(wm[:, c, 0:RW_IN], sg_[:], omm[:], ALU.mult),
                             reads=[sk_, "omm"], writes=["wm"])
                        S.op("pool", lambda e, c=c, sg_=sg_: e.tensor_tensor(wm[:, c, RW_IN:2 * RW_IN], sg_[:], mu[:], ALU.mult),
                             reads=[sk_, "mu"], writes=["wm"])
                    S.barrier()

                xts = [sb(f"mx{i}", [128, D], F32, ms) for i in range(2)]
                hb = sb("mh", [128, D], BF16, ms)
                junk = hb
                ss = sb("mss", [128, 2], F32, ms)
                hT = sb("mhT", [128, 8, 128], BF16, ms)
                hTp = sb("mhTp", [128, 8, 128], BF16, ms)
                carry = sb("mcarry", [128, 8, 2], BF16, ms)
                ropet = [sb(f"ropet{i}", [128, 64], F32, ms) for i in range(2)]
                G = {i: sb(f"G{i}", [128, 512], F32, ms) for i in (1, 2, 3, 4, 6, 7, 8, 9, 10)}
                G5 = sb("G5", [128, 1024], F32, ms)

                def bf3(t, h):
                    return t[:].bitcast(BF16).rearrange("p (h t) -> p h t", h=h)
                qk_f = Wi = G[1][:]
                rg_s = Wn = G[2][:]
                rot = We = G[3][:]
                o_f = Wh = G[4][:]
                Pm = [bf3(G[1], 8), bf3(G[2], 8)]
                Qm = [bf3(G[3], 8), bf3(G[4], 8)]
                ra = G5[:, 0:256]
                rb = G5[:, 256:512]
                kk = G5[:, 0:512]
                bp = G5[:, 512:1024]
                g5b = G5[:].bitcast(BF16)
                Pp = g5b[:, 1024:2048].rearrange("p (h t) -> p h t", h=8)
                sigw = G[6][:]
                TTbs = [bf3(G[6], 8), g5b[:, 0:1024].rearrange("p (h t) -> p h t", h=8)]
                r_f = y_f = G[7][:]
                k_f = G[8][:]
                mT = bf3(G[8], 8)
                g9 = G[9][:].bitcast(BF16)
                g10 = G[10][:].bitcast(BF16)
                tm4 = [g9[:, 0:512], g9[:, 512:1024], g10[:, 0:512], g10[:, 512:1024]]
                Xb = g10[:, 0:512].rearrange("p (h v) -> p h v", h=8)
                Ub = g10[:, 512:1024].rearrange("p (h v) -> p h v", h=8)
                S.alias.update({"qk_f": "G1", "Wi": "G1", "Pm0": "G1", "rg_s": "G2", "Wn": "G2", "Pm1": "G2",
                                "rot": "G3", "We": "G3", "Qm0": "G3", "o_f": "G4", "Wh": "G4", "Qm1": "G4",
                                "ra": "G5", "rb": "G5", "kk": "G5", "bp": "G5", "TTb1": "G5", "Pp": "G5", "sigw": "G6", "TTb0": "G6",
                                "r_f": "G7", "y_f": "G7", "k_f": "G8", "mT": "G8", "tm0": "G9", "tm1": "G9",
                                "tm2": "G10", "tm3": "G10", "Xb": "G10", "Ub": "G10", "mjunk": "mh"})
                v_b = sb("v_b", [128, 512], BF16, ms)
                lwT = sb("lwT", [128, 128], BF16, ms)
                lgT = sb("lgT", [128, 128], BF16, ms)
                rv_b = sb("rv_b", [128, 512], BF16, ms)
                a_f = sb("a_f", [128, 512], F32, ms)
                g_f = sb("g_f", [128, 512], F32, ms)
                WC = sb("WC", [128, 8], F32, ms)
                sighl = sb("sighl", [128, 1024], BF16, ms)
                t0 = sb("t0", [128, 512], F32, ms)
                t1 = sb("t1", [128, 512], F32, ms)
                k_h = sb("k_h", [128, 512], F32, ms)
                s8 = sb("s8", [128, 8], F32, ms)
                bon8 = sb("bon8", [128, 8], F32, ms)
                arT = sb("arT", [128, 8, 2, 128], BF16, ms)
                bT = sb("bT", [128, 8, 128], BF16, ms)
                kT = sb("kT", [128, 8, 128], BF16, ms)
                Bh = sb("Bh", [128, 8, 128], BF16, ms)
                Kh = sb("Kh", [128, 8, 128], BF16, ms)
                QA = sb("QA", [128, 8, 2, 128], BF16, ms)
                KA = sb("KA", [128, 8, 2, 128], BF16, ms)
                Sf = sb("Sf", [128, 8, 64], F32, ms)
                Sb = sb("Sb", [128, 8, 64], BF16, ms)
                mixed = sb("mixed", [128, D], BF16, ms)
                qt_b = sb("qt_b", [128, 256], BF16, ms)
                kp_b = sb("kp_b", [128, 256], BF16, ms)
                rKh = sb("rKh", [128, 4, 128], BF16, ms)
                rqT = sb("rqT", [128, 4, 128], BF16, ms)
                rkT = sb("rkT", [128, 4, 128], BF16, ms)
                Sc = sb("Sc", [128, 4, 128], BF16, ms)
                RSf = sb("RSf", [128, 4, 128], F32, ms)
                RSb = sb("RSb", [128, 4, 128], BF16, ms)
                s4 = sb("s4", [128, 4], F32, ms)

                for tname, tt in [("arT", arT), ("bT", bT), ("kT", kT), ("Bh", Bh), ("Kh", Kh), ("rKh", rKh),
                                  ("rqT", rqT), ("rkT", rkT), ("Sb", Sb), ("RSb", RSb)]:
                    S.op("pool", lambda e, tt=tt: e.memset(tt[:], 0.0), writes=[tname])
                S.op("dve", lambda e: e.memset(Sf[:], 0.0), writes=["Sf"])
                S.op("dve", lambda e: e.memset(RSf[:], 0.0), writes=["RSf"])
                S.op("dve", lambda e: e.memset(carry[:], 0.0), writes=["mcarry"])

                def bc3(ap2, n_in, n_out):
                    return ap2.unsqueeze(2).to_broadcast([ap2.shape[0], n_in, n_out])

                def bch(ap2, nh, ncol):
                    return ap2.unsqueeze(1).to_broadcast([ap2.shape[0], nh, ncol])

                def v3(ap2, a, b_):
                    return ap2.rearrange("p (a b) -> p a b", a=a)

                src = src_x[0]
                import os as _os
                _stop = _os.environ.get("MIX_STOP", "")

                def chk(tag):
                    if tag == _stop:
                        raise _StopTile()

                def front(n):
                    xt = xts[n % 2]
                    mxk = f"mx{n % 2}"
                    for s in range(2):
                        r0 = s * seq + n * 64
                        S.dma("sp", xt[s * 64:(s + 1) * 64, :], src[r0:r0 + 64, :], writes=[mxk], sem=f"ml{n % 2}")
                    S.op("act", lambda e, xt=xt: e.activation(junk[:], xt[:], AF.Square, accum_out=ss[:, 0:1]),
                         reads=[mxk], writes=["mjunk", "mss"])
                    rstd_ops(ss, 1, "mss")
                    S.op("dve", lambda e, xt=xt: e.scalar_tensor_tensor(hb[:], xt[:], ss[:, 0:1], gain[:], ALU.mult, ALU.mult),
                         reads=[mxk, "mss", "mgain"], writes=["mh"])
                    ps, pk = getps()
                    psb = ps[:].bitcast(BF16)
                    for c in range(8):
                        S.op("pe", lambda e, c=c, psb=psb: e.transpose(psb[:, c * 128:(c + 1) * 128],
                                                                       hb[:, c * 128:(c + 1) * 128], ident_b[:]),
                             reads=["mh", "ident_b"], writes=[pk])
                    S.op("act", lambda e, psb=psb: e.activation(
                        hT[:], psb.rearrange("p (c t) -> p c t", c=8), AF.Copy),
                        reads=[pk], writes=["mhT"])
                    hT4 = hT[:].rearrange("p c (s t) -> p c s t", s=2)
                    hTp4 = hTp[:].rearrange("p c (s t) -> p c s t", s=2)
                    S.op("pool", lambda e: e.tensor_copy(hTp4[:, :, :, 1:64], hT4[:, :, :, 0:63]),
                         reads=["mhT"], writes=["mhTp"])
                    S.op("pool", lambda e: e.tensor_copy(hTp4[:, :, :, 0], carry[:]),
                         reads=["mcarry"], writes=["mhTp"])
                    S.op("pool", lambda e: e.tensor_copy(carry[:], hT4[:, :, :, 63]),
                         reads=["mhT"], writes=["mcarry"])


                front(0)
                for n in range(nt):
                  try:
                    xt = xts[n % 2]
                    mxk = f"mx{n % 2}"
                    def cur(c):
                        return hT[:, c, :]

                    def prev(c):
                        return hTp[:, c, :]

                    chk("D")
                    def proj_rw(ps_ap, col0, ncol, pk):
                        for c in range(8):
                            S.op("pe", lambda e, c=c: e.matmul(ps_ap, cur(c), wm[:, c, col0:col0 + ncol],
                                                               start=(c == 0), stop=False),
                                 reads=["mhT", "wm"], writes=[pk])
                        for c in range(8):
                            S.op("pe", lambda e, c=c: e.matmul(ps_ap, prev(c), wm[:, c, RW_IN + col0:RW_IN + col0 + ncol],
                                                               start=False, stop=(c == 7)),
                                 reads=["mhTp", "wm"], writes=[pk])

                    ps, pk = getps()
                    proj_rw(ps[:], 0, 512, pk)
                    S.op("act", lambda e, ps=ps: e.activation(r_f[:], ps[:], AF.Copy), reads=[pk], writes=["r_f"])
                    ps, pk = getps()
                    proj_rw(ps[:], 512, 512, pk)
                    S.op("dve", lambda e, ps=ps: e.tensor_copy(k_f[:], ps[:]), reads=[pk], writes=["k_f"])
                    ps, pk = getps()
                    proj_rw(ps[:], 1024, 512, pk)
                    S.op("dve", lambda e, ps=ps: e.tensor_copy(v_b[:], ps[:]), reads=[pk], writes=["v_b"])
                    ps, pk = getps()
                    for gi in range(2):
                        col0 = 1536 + gi * 128
                        for c in range(8):
                            S.op("pe", lambda e, c=c, gi=gi, col0=col0, ps=ps: e.matmul(
                                ps[:, gi * 128:(gi + 1) * 128], wm[:, c, col0:col0 + 128], cur(c),
                                start=(c == 0), stop=False), reads=["mhT", "wm"], writes=[pk])
                        for c in range(8):
                            S.op("pe", lambda e, c=c, gi=gi, col0=col0, ps=ps: e.matmul(
                                ps[:, gi * 128:(gi + 1) * 128], wm[:, c, RW_IN + col0:RW_IN + col0 + 128], prev(c),
                                start=False, stop=(c == 7)), reads=["mhTp", "wm"], writes=[pk])
                    S.op("act", lambda e, ps=ps: e.activation(lwT[0:64, :], ps[0:64, 0:128], AF.Tanh), reads=[pk], writes=["lwT"])
                    S.op("act", lambda e, ps=ps: e.activation(lwT[64:128, :], ps[64:128, 0:128], AF.Copy), reads=[pk], writes=["lwT"])
                    S.op("act", lambda e, ps=ps: e.activation(lgT[:], ps[:, 128:256], AF.Sigmoid), reads=[pk], writes=["lgT"])
                    ps, pk = getps()
                    for c in range(8):
                        S.op("pe", lambda e, c=c, ps=ps: e.matmul(ps[:], cur(c), wm[:, c, 3584:4096],
                                                                  start=(c == 0), stop=(c == 7)),
                             reads=["mhT", "wm"], writes=[pk])
                    S.op("dve", lambda e, ps=ps: e.tensor_copy(qk_f[:], ps[:]), reads=[pk], writes=["qk_f"])
                    ps, pk = getps()
                    for c in range(8):
                        S.op("pe", lambda e, c=c, ps=ps: e.matmul(ps[:], cur(c), wm[:, c, 4096:4608],
                                                                  start=(c == 0), stop=(c == 7)),
                             reads=["mhT", "wm"], writes=[pk])
                    S.op("act", lambda e, ps=ps: e.activation(rv_b[:], ps[:], AF.Copy), reads=[pk], writes=["rv_b"])
                    ps, pk = getps()
                    for c in range(8):
                        S.op("pe", lambda e, c=c, ps=ps: e.matmul(ps[:], cur(c), wm[:, c, 4608:5120],
                                                                  start=(c == 0), stop=(c == 7)),
                             reads=["mhT", "wm"], writes=[pk])
                    S.op("act", lambda e, ps=ps: e.activation(rg_s[:], ps[:], AF.Silu), reads=[pk], writes=["rg_s"])

                    chk("K")
                    if n + 1 < nt:
                        front(n + 1)
                    cs_ = ropet[n % 2]
                    ropek = f"ropet{n % 2}"
                    S.dma("sp", cs_[:], cd["rope"][:, n * 64:(n + 1) * 64], writes=[ropek], sem=f"rp{n % 2}")
                    cosb = cs_[:, 0:32].unsqueeze(1).to_broadcast([128, 8, 32])
                    sinb = cs_[:, 32:64].unsqueeze(1).to_broadcast([128, 8, 32])
                    qk4 = qk_f[:].rearrange("p (g a d) -> p g a d", g=8, a=2)
                    rot4 = rot[:].rearrange("p (g a d) -> p g a d", g=8, a=2)
                    ra3 = v3(ra[:], 8, 32)
                    rb3 = v3(rb[:], 8, 32)
                    S.op("dve", lambda e, cosb=cosb, sinb=sinb: e.tensor_tensor(ra3, qk4[:, :, 0, :], cosb, ALU.mult), reads=["qk_f", ropek], writes=["ra"])
                    S.op("pool", lambda e, cosb=cosb, sinb=sinb: e.tensor_tensor(rb3, qk4[:, :, 1, :], sinb, ALU.mult), reads=["qk_f", ropek], writes=["rb"])
                    S.op("dve", lambda e: e.tensor_tensor(rot4[:, :, 0, :], ra3, rb3, ALU.subtract), reads=["ra", "rb"], writes=["rot"])
                    S.op("dve", lambda e, cosb=cosb, sinb=sinb: e.tensor_tensor(ra3, qk4[:, :, 0, :], sinb, ALU.mult), reads=["qk_f", ropek], writes=["ra"])
                    S.op("pool", lambda e, cosb=cosb, sinb=sinb: e.tensor_tensor(rb3, qk4[:, :, 1, :], cosb, ALU.mult), reads=["qk_f", ropek], writes=["rb"])
                    S.op("dve", lambda e: e.tensor_tensor(rot4[:, :, 1, :], ra3, rb3, ALU.add), reads=["ra", "rb"], writes=["rot"])
                    S.op("dve", lambda e: e.tensor_tensor(v3(qt_b[:], 4, 64), v3(rot[:, 0:256], 4, 64), bc3(qkw[:, 0:4], 4, 64), ALU.mult),
                         reads=["rot", "qkw"], writes=["qt_b"])
                    S.op("pool", lambda e: e.tensor_copy(kp_b[:], rot[:, 256:512]), reads=["rot"], writes=["kp_b"])
                    for s in range(2):
                        sl_ = slice(s * 64, (s + 1) * 64)
                        S.op("dve" if s == 0 else "pool", lambda e, s=s, sl_=sl_: e.tensor_tensor(
                            rKh[sl_, :, s * 64:(s + 1) * 64], v3(rot[sl_, 256:512], 4, 64), bc3(qkw[sl_, 4:8], 4, 64), ALU.mult),
                            reads=["rot", "qkw"], writes=["rKh"])
                    chk("K1")
                    for gi, (srcb, skey, dst, dk) in enumerate([(qt_b, "qt_b", rqT, "rqT"), (kp_b, "kp_b", rkT, "rkT")]):
                        ps, pk = getps()
                        psb = ps[:].bitcast(BF16)
                        for pr in range(2):
                            S.op("pe", lambda e, pr=pr, psb=psb, srcb=srcb: e.transpose(
                                psb[:, pr * 128:(pr + 1) * 128], srcb[:, pr * 128:(pr + 1) * 128], ident_b[:]),
                                reads=[skey, "ident_b"], writes=[pk])
                        pv = psb[:, 0:256].rearrange("p (g t) -> p g t", g=2)
                        dst5 = dst[:].rearrange("p (pr par) t -> p pr par t", par=2)
                        for par in range(2):
                            for s in range(2):
                                d_ = dst5[s * 64:(s + 1) * 64, :, par, s * 64:(s + 1) * 64]
                                i_ = pv[par * 64:(par + 1) * 64, :, s * 64:(s + 1) * 64]
                                S.op("dve", lambda e, d_=d_, i_=i_: e.tensor_copy(d_, i_), reads=[pk], writes=[dk])
                    chk("K2")
                    ps, pk = getps()
                    for h in range(4):
                        S.op("pe", lambda e, h=h, ps=ps: e.matmul(ps[:, h * 128:(h + 1) * 128], rkT[:, h, :], rqT[:, h, :],
                                                                  start=True, stop=True), reads=["rkT", "rqT"], writes=[pk])
                    S.op("dve", lambda e, ps=ps: e.tensor_tensor(Sc[:], v3(ps[:], 4, 128), v3(dmask[:], 4, 128), ALU.mult),
                         reads=[pk, "dmask"], writes=["Sc"])
                    ps, pk = getps()
                    for h in range(4):
                        S.op("pe", lambda e, h=h, ps=ps: e.matmul(ps[:, h * 128:(h + 1) * 128], Sc[:, h, :],
                                                                  rv_b[:, h * 128:(h + 1) * 128], start=True, stop=False),
                             reads=["Sc", "rv_b"], writes=[pk])
                        S.op("pe", lambda e, h=h, ps=ps: e.matmul(ps[:, h * 128:(h + 1) * 128], rqT[:, h, :], RSb[:, h, :],
                                                                  start=False, stop=True), reads=["rqT", "RSb"], writes=[pk])
                    S.op("act", lambda e, ps=ps: e.activation(o_f[:], ps[:], AF.Copy), reads=[pk], writes=["o_f"])
                    ps, pk = getps()
                    for h in range(4):
                        S.op("pe", lambda e, h=h, ps=ps: e.matmul(ps[:, h * 128:(h + 1) * 128], rKh[:, h, :],
                                                                  rv_b[:, h * 128:(h + 1) * 128], start=True, stop=True),
                             reads=["rKh", "rv_b"], writes=[pk])
                    S.op("dve", lambda e: e.tensor_tensor(RSf[:], RSf[:], bc3(qkw[:, 8:12], 4, 128), ALU.mult),
                         reads=["RSf", "qkw"], writes=["RSf"])
                    S.op("dve", lambda e, ps=ps: e.tensor_tensor(RSf[:], RSf[:], v3(ps[:], 4, 128), ALU.add),
                         reads=[pk, "RSf"], writes=["RSf"])
                    S.op("pool", lambda e: e.tensor_copy(RSb[:], RSf[:]), reads=["RSf"], writes=["RSb"])
                    S.op("pool", lambda e: e.tensor_tensor(t1[:], o_f[:], o_f[:], ALU.mult), reads=["o_f"], writes=["t1"])
                    S.op("dve", lambda e: e.tensor_reduce(s4[:], v3(t1[:], 4, 128), AX.X, ALU.add), reads=["t1"], writes=["s4"])
                    S.op("dve", lambda e: e.tensor_scalar(s4[:], s4[:], 1.0 / 128, 1e-6, ALU.mult, ALU.add), reads=["s4"], writes=["s4"])
                    S.op("act", lambda e: e.activation(s4[:], s4[:], AF.Sqrt), reads=["s4"], writes=["s4"])
                    S.op("dve", lambda e: e.reciprocal(s4[:], s4[:]), reads=["s4"], writes=["s4"])
                    S.op("dve", lambda e: e.tensor_tensor(v3(o_f[:], 4, 128), v3(o_f[:], 4, 128), bc3(s4[:], 4, 128), ALU.mult),
                         reads=["o_f", "s4"], writes=["o_f"])
                    S.op("pool", lambda e: e.tensor_tensor(mixed[:, 512:1024], o_f[:], rg_s[:], ALU.mult),
                         reads=["o_f", "rg_s"], writes=["mixed"])

                    chk("E")
                    ps, pk = getps()
                    S.op("pe", lambda e, ps=ps: e.matmul(ps[:], lwT[:], wal[:, 0:512], start=True, stop=True),
                         reads=["lwT", "wal"], writes=[pk])
                    S.op("dve", lambda e, ps=ps: e.tensor_tensor(t0[:], ps[:], bcn["w0"][:], ALU.add),
                         reads=[pk, "bc_w0"], writes=["t0"])
                    S.op("act", lambda e: e.activation(sigw[:], t0[:], AF.Sigmoid), reads=["t0"], writes=["sigw"])
                    ps, pk = getps()
                    S.op("pe", lambda e, ps=ps: e.matmul(ps[:], lwT[:], wal[:, 512:1024], start=True, stop=True),
                         reads=["lwT", "wal"], writes=[pk])
                    S.op("dve", lambda e, ps=ps: e.tensor_tensor(t1[:], ps[:], bcn["a0"][:], ALU.add),
                         reads=[pk, "bc_a0"], writes=["t1"])
                    S.op("act", lambda e: e.activation(a_f[:], t1[:], AF.Sigmoid), reads=["t1"], writes=["a_f"])
                    ps, pk = getps()
                    S.op("pe", lambda e, ps=ps: e.matmul(ps[:], lgT[:], glu[:], start=True, stop=True),
                         reads=["lgT", "glu"], writes=[pk])
                    S.op("act", lambda e, ps=ps: e.activation(g_f[:], ps[:], AF.Copy), reads=[pk], writes=["g_f"])
                    S.op("pool", lambda e: e.tensor_copy(sighl[:, 0:512], sigw[:]), reads=["sigw"], writes=["sighl"])
                    S.op("dve", lambda e: e.tensor_tensor(sighl[:, 512:1024], sigw[:], sighl[:, 0:512], ALU.subtract),
                         reads=["sigw", "sighl"], writes=["sighl"])
                    ps, pk = getps()
                    for hl in range(2):
                        S.op("pe", lambda e, ps=ps, hl=hl: e.matmul(ps[:], tri_i[:], sighl[:, hl * 512:(hl + 1) * 512],
                                                                    start=(hl == 0), stop=(hl == 1)),
                             reads=["tri_i", "sighl"], writes=[pk])
                    S.op("act", lambda e, ps=ps: e.activation(Wi[:], ps[:], AF.Exp, scale=-C_DEC), reads=[pk], writes=["Wi"])
                    S.op("act", lambda e, ps=ps: e.activation(Wn[:], ps[:], AF.Exp, scale=C_DEC), reads=[pk], writes=["Wn"])
                    S.op("act", lambda e: e.activation(t0[:], sigw[:], AF.Exp, scale=C_DEC), reads=["sigw"], writes=["t0"])
                    S.op("dve", lambda e: e.tensor_tensor(We[:], Wi[:], t0[:], ALU.mult), reads=["Wi", "t0"], writes=["We"])
                    ps, pk = getps()
                    for hl in range(2):
                        S.op("pe", lambda e, ps=ps, hl=hl: e.matmul(ps[:], tri_r[:], sighl[:, hl * 512:(hl + 1) * 512],
                                                                    start=(hl == 0), stop=(hl == 1)),
                             reads=["tri_r", "sighl"], writes=[pk])
                    S.op("act", lambda e, ps=ps: e.activation(Wh[:], ps[:], AF.Exp, scale=-C_DEC), reads=[pk], writes=["Wh"])
                    ps, pk = getps()
                    for pr in range(4):
                        for hl in range(2):
                            S.op("pe", lambda e, pr=pr, ps=ps, hl=hl: e.matmul(
                                ps[:, pr * 2:pr * 2 + 2], sighl[:, hl * 512 + pr * 128:hl * 512 + (pr + 1) * 128],
                                seqind[:], start=(hl == 0), stop=(hl == 1)),
                                reads=["sighl", "seqind"], writes=[pk])
                    WC4 = WC[:].rearrange("p (pr par) -> p pr par", par=2)
                    for s in range(2):
                        for par in range(2):
                            S.op("act", lambda e, s=s, par=par, ps=ps: e.activation(
                                WC4[s * 64:(s + 1) * 64, :, par],
                                ps[par * 64:(par + 1) * 64, 0:8].rearrange("p (pr s) -> p pr s", s=2)[:, :, s], AF.Exp, scale=-C_DEC),
                                reads=[pk], writes=["WC"])

                    chk("F")
                    S.op("pool", lambda e: e.tensor_tensor(t1[:], k_f[:], bcn["k_k"][:], ALU.mult),
                         reads=["k_f", "bc_k_k"], writes=["t1"])
                    S.op("pool", lambda e: e.tensor_tensor(t0[:], t1[:], t1[:], ALU.mult), reads=["t1"], writes=["t0"])
                    S.op("dve", lambda e: e.tensor_reduce(s8[:], v3(t0[:], 8, 64), AX.X, ALU.add),
                         reads=["t0"], writes=["s8"])
                    S.op("dve", lambda e: e.tensor_scalar_max(s8[:], s8[:], 1e-24), reads=["s8"], writes=["s8"])
                    S.op("act", lambda e: e.activation(s8[:], s8[:], AF.Sqrt), reads=["s8"], writes=["s8"])
                    S.op("dve", lambda e: e.reciprocal(s8[:], s8[:]), reads=["s8"], writes=["s8"])
                    S.op("dve", lambda e: e.tensor_tensor(v3(kk[:], 8, 64), v3(t1[:], 8, 64), bc3(s8[:], 8, 64), ALU.mult),
                         reads=["t1", "s8"], writes=["kk"])
                    S.op("dve", lambda e: e.scalar_tensor_tensor(t0[:], a_f[:], -1.0, bcn["k_a"][:], ALU.add, ALU.mult),
                         reads=["a_f", "bc_k_a"], writes=["t0"])
                    S.op("dve", lambda e: e.scalar_tensor_tensor(k_h[:], t0[:], 1.0, k_f[:], ALU.add, ALU.mult),
                         reads=["t0", "k_f"], writes=["k_h"])
                    S.op("dve", lambda e: e.tensor_tensor(bp[:], kk[:], a_f[:], ALU.mult), reads=["kk", "a_f"], writes=["bp"])
                    S.op("dve", lambda e: e.scalar_tensor_tensor(tm4[0][:], kk[:], -1.0, We[:], ALU.mult, ALU.mult),
                         reads=["kk", "We"], writes=["tm0"])
                    S.op("pool", lambda e: e.tensor_tensor(tm4[1][:], r_f[:], Wi[:], ALU.mult), reads=["r_f", "Wi"], writes=["tm1"])
                    S.op("dve", lambda e: e.tensor_tensor(tm4[2][:], bp[:], Wn[:], ALU.mult), reads=["bp", "Wn"], writes=["tm2"])
                    S.op("pool", lambda e: e.tensor_tensor(tm4[3][:], k_h[:], Wn[:], ALU.mult), reads=["k_h", "Wn"], writes=["tm3"])
                    chk("F1")
                    for s in range(2):
                        sl_ = slice(s * 64, (s + 1) * 64)
                        S.op("dve" if s == 0 else "pool", lambda e, s=s, sl_=sl_: e.tensor_tensor(
                            Bh[sl_, :, s * 64:(s + 1) * 64], v3(bp[sl_, :], 8, 64), v3(Wh[sl_, :], 8, 64), ALU.mult),
                            reads=["bp", "Wh"], writes=["Bh"])
                        S.op("pool" if s == 0 else "dve", lambda e, s=s, sl_=sl_: e.tensor_tensor(
                            Kh[sl_, :, s * 64:(s + 1) * 64], v3(k_h[sl_, :], 8, 64), v3(Wh[sl_, :], 8, 64), ALU.mult),
                            reads=["k_h", "Wh"], writes=["Kh"])
                    chk("F2")
                    S.op("pool", lambda e: e.tensor_tensor(t0[:], r_f[:], k_h[:], ALU.mult), reads=["r_f", "k_h"], writes=["t0"])
                    S.op("pool", lambda e: e.tensor_tensor(t0[:], t0[:], bcn["r_k"][:], ALU.mult),
                         reads=["t0", "bc_r_k"], writes=["t0"])
                    S.op("dve", lambda e: e.tensor_reduce(bon8[:], v3(t0[:], 8, 64), AX.X, ALU.add),
                         reads=["t0"], writes=["bon8"])
                    chk("F3")
                    dkeys = ["arT", "arT", "bT", "kT"]
                    for qi in range(4):
                        ps, pk = getps()
                        psb = ps[:].bitcast(BF16)
                        for pr in range(4):
                            S.op("pe", lambda e, qi=qi, pr=pr, psb=psb: e.transpose(
                                psb[:, pr * 128:(pr + 1) * 128], tm4[qi][:, pr * 128:(pr + 1) * 128], ident_b[:]),
                                reads=[f"tm{qi}", "ident_b"], writes=[pk])
                        pv = psb[:, 0:512].rearrange("p (pr t) -> p pr t", pr=4)
                        if qi < 2:
                            dst = arT[:, :, qi, :]
                        elif qi == 2:
                            dst = bT[:]
                        else:
                            dst = kT[:]
                        dst5 = dst.rearrange("p (pr par) t -> p pr par t", par=2)
                        k_ = 0
                        for par in range(2 if (not _os.environ.get("MIX_NOEVAC") or str(qi) in _os.environ.get("MIX_EVACQ", "")) else 0):
                            for s in range(2):
                                d_ = dst5[s * 64:(s + 1) * 64, :, par, s * 64:(s + 1) * 64]
                                i_ = pv[par * 64:(par + 1) * 64, :, s * 64:(s + 1) * 64]
                                S.op("dve", lambda e, d_=d_, i_=i_: e.tensor_copy(d_, i_),
                                     reads=[pk], writes=[dkeys[qi]])
                                k_ += 1

                    chk("G")
                    for hp in range(4):
                        ps, pk = getps()
                        for hh in range(2):
                            h = hp * 2 + hh
                            S.op("pe", lambda e, h=h, hh=hh, ps=ps: e.matmul(
                                ps[:, hh * 256:(hh + 1) * 256], bT[:, h, :], arT[:, h, :, :].rearrange("p a t -> p (a t)"), start=True, stop=True),
                                reads=["bT", "arT"], writes=[pk])
                        S.op("dve", lambda e, hp=hp, ps=ps: e.tensor_tensor(
                            QA[:, hp * 2:hp * 2 + 2, :, :].rearrange("p h a t -> p h (a t)"),
                            v3(ps[:], 2, 256), bch(m1[:], 2, 256), ALU.mult),
                            reads=[pk, "m1"], writes=["QA"])
                        ps, pk = getps()
                        for hh in range(2):
                            h = hp * 2 + hh
                            S.op("pe", lambda e, h=h, hh=hh, ps=ps: e.matmul(
                                ps[:, hh * 256:(hh + 1) * 256], kT[:, h, :], arT[:, h, :, :].rearrange("p a t -> p (a t)"), start=True, stop=True),
                                reads=["kT", "arT"], writes=[pk])
                        S.op("dve", lambda e, hp=hp, ps=ps: e.tensor_tensor(
                            KA[:, hp * 2:hp * 2 + 2, :, :].rearrange("p h a t -> p h (a t)"),
                            v3(ps[:], 2, 256), bch(m1[:], 2, 256), ALU.mult),
                            reads=[pk, "m1"], writes=["KA"])
                    for hq in range(2):
                        ps, pk = getps()
                        for hh in range(4):
                            h = hq * 4 + hh
                            S.op("pe", lambda e, h=h, hh=hh, ps=ps: e.matmul(
                                ps[:, hh * 128:(hh + 1) * 128], arT[:, h, 0, :], bT[:, h, :], start=True, stop=True),
                                reads=["bT", "arT"], writes=[pk])
                        S.op("dve", lambda e, hq=hq, ps=ps: e.tensor_tensor(
                            Pm[0][:, hq * 4:hq * 4 + 4, :], v3(ps[:], 4, 128), bch(msl[:], 4, 128), ALU.mult),
                            reads=[pk, "msl"], writes=["Pm0"])
                    chk("H")
                    S.op("pool", lambda e: e.tensor_tensor(TTbs[0][:], QA[:, :, 0, :], bch(ident_f[:], 8, 128), ALU.add),
                         reads=["QA", "ident_f"], writes=["TTb0"])
                    for lvl in range(1, 6):
                        pi_, po_ = (lvl - 1) % 2, lvl % 2

                        def Qprev(h, lvl=lvl, pi_=pi_):
                            return QA[:, h, 0, :] if lvl == 1 else Qm[pi_][:, h, :]
                        qprev_key = "QA" if lvl == 1 else f"Qm{pi_}"
                        for hq in range(2):
                            ps, pk = getps()
                            for hh in range(4):
                                h = hq * 4 + hh
                                S.op("pe", lambda e, h=h, hh=hh, ps=ps, Qprev=Qprev, pi_=pi_: e.matmul(
                                    ps[:, hh * 128:(hh + 1) * 128], Qprev(h), Pm[pi_][:, h, :], start=True, stop=True),
                                    reads=[qprev_key, f"Pm{pi_}"], writes=[pk])
                            S.op("dve", lambda e, hq=hq, ps=ps: e.tensor_tensor(
                                Pp[:, hq * 4:hq * 4 + 4, :], v3(ps[:], 4, 128), bch(ident_f[:], 4, 128), ALU.add),
                                reads=[pk, "ident_f"], writes=["Pp"])
                            if lvl < 5:
                                S.op("dve", lambda e, hq=hq, ps=ps, po_=po_: e.tensor_copy(
                                    Pm[po_][:, hq * 4:hq * 4 + 4, :], v3(ps[:], 4, 128)),
                                    reads=[pk], writes=[f"Pm{po_}"])
                        if lvl < 5:
                            for hq in range(2):
                                ps, pk = getps()
                                for hh in range(4):
                                    h = hq * 4 + hh
                                    S.op("pe", lambda e, h=h, hh=hh, ps=ps, Qprev=Qprev, pi_=pi_: e.matmul(
                                        ps[:, hh * 128:(hh + 1) * 128], Pm[pi_][:, h, :], Qprev(h), start=True, stop=True),
                                        reads=[qprev_key, f"Pm{pi_}"], writes=[pk])
                                S.op("act", lambda e, hq=hq, ps=ps, po_=po_: e.activation(
                                    Qm[po_][:, hq * 4:hq * 4 + 4, :], v3(ps[:], 4, 128), AF.Copy),
                                    reads=[pk], writes=[f"Qm{po_}"])
                        for hq in range(2):
                            ps, pk = getps()
                            for hh in range(4):
                                h = hq * 4 + hh
                                S.op("pe", lambda e, h=h, hh=hh, ps=ps, pi_=pi_: e.matmul(
                                    ps[:, hh * 128:(hh + 1) * 128], Pp[:, h, :], TTbs[pi_][:, h, :], start=True, stop=True),
                                    reads=["Pp", f"TTb{pi_}"], writes=[pk])
                            if hq == 0:
                                S.op("act", lambda e, hq=hq, ps=ps, po_=po_: e.activation(
                                    TTbs[po_][:, hq * 4:hq * 4 + 4, :], v3(ps[:], 4, 128), AF.Copy),
                                    reads=[pk], writes=[f"TTb{po_}"])
                            else:
                                S.op("dve", lambda e, hq=hq, ps=ps, po_=po_: e.tensor_copy(
                                    TTbs[po_][:, hq * 4:hq * 4 + 4, :], v3(ps[:], 4, 128)),
                                    reads=[pk], writes=[f"TTb{po_}"])
                    TTb = TTbs[1]

                    chk("I")
                    ps, pk = getps()
                    for h in range(8):
                        S.op("pe", lambda e, h=h, ps=ps: e.matmul(ps[:, h * 64:(h + 1) * 64], arT[:, h, 0, :], Sb[:, h, :],
                                                                  start=True, stop=False), reads=["arT", "Sb"], writes=[pk])
                        S.op("pe", lambda e, h=h, ps=ps: e.matmul(ps[:, h * 64:(h + 1) * 64], KA[:, h, 0, :],
                                                                  v_b[:, h * 64:(h + 1) * 64], start=False, stop=True),
                             reads=["KA", "v_b"], writes=[pk])
                    S.op("act", lambda e, ps=ps: e.activation(Xb[:], v3(ps[:], 8, 64), AF.Copy), reads=[pk], writes=["Xb"])
                    ps, pk = getps()
                    for h in range(8):
                        S.op("pe", lambda e, h=h, ps=ps: e.matmul(ps[:, h * 64:(h + 1) * 64], TTb[:, h, :], Xb[:, h, :],
                                                                  start=True, stop=True), reads=["TTb1", "Xb"], writes=[pk])
                    S.op("dve", lambda e, ps=ps: e.tensor_copy(Ub[:], v3(ps[:], 8, 64)), reads=[pk], writes=["Ub"])
                    ps, pk = getps()
                    for h in range(8):
                        S.op("pe", lambda e, h=h, ps=ps: e.matmul(ps[:, h * 64:(h + 1) * 64], arT[:, h, 1, :], Sb[:, h, :],
                                                                  start=True, stop=False), reads=["arT", "Sb"], writes=[pk])
                        S.op("pe", lambda e, h=h, ps=ps: e.matmul(ps[:, h * 64:(h + 1) * 64], QA[:, h, 1, :], Ub[:, h, :],
                                                                  start=False, stop=False), reads=["QA", "Ub"], writes=[pk])
                        S.op("pe", lambda e, h=h, ps=ps: e.matmul(ps[:, h * 64:(h + 1) * 64], KA[:, h, 1, :],
                                                                  v_b[:, h * 64:(h + 1) * 64], start=False, stop=True),
                             reads=["KA", "v_b"], writes=[pk])
                    S.op("act", lambda e, ps=ps: e.activation(y_f[:], ps[:], AF.Copy), reads=[pk], writes=["y_f"])
                    ps, pk = getps()
                    for h in range(8):
                        S.op("pe", lambda e, h=h, ps=ps: e.matmul(ps[:, h * 64:(h + 1) * 64], Bh[:, h, :], Ub[:, h, :],
                                                                  start=True, stop=False), reads=["Bh", "Ub"], writes=[pk])
                        S.op("pe", lambda e, h=h, ps=ps: e.matmul(ps[:, h * 64:(h + 1) * 64], Kh[:, h, :],
                                                                  v_b[:, h * 64:(h + 1) * 64], start=False, stop=True),
                             reads=["Kh", "v_b"], writes=[pk])
                    S.op("dve", lambda e: e.tensor_tensor(Sf[:], Sf[:], bc3(WC[:], 8, 64), ALU.mult),
                         reads=["Sf", "WC"], writes=["Sf"])
                    S.op("dve", lambda e, ps=ps: e.tensor_tensor(Sf[:], Sf[:], v3(ps[:], 8, 64), ALU.add),
                         reads=[pk, "Sf"], writes=["Sf"])
                    S.op("pool", lambda e: e.tensor_copy(Sb[:], Sf[:]), reads=["Sf"], writes=["Sb"])

                    chk("J")
                    y3 = v3(y_f[:], 8, 64)
                    S.op("dve", lambda e: e.tensor_reduce(s8[:], y3, AX.X, ALU.add), reads=["y_f"], writes=["s8"])
                    S.op("dve", lambda e: e.tensor_scalar(s8[:], s8[:], 1.0 / 64, None, ALU.mult), reads=["s8"], writes=["s8"])
                    S.op("dve", lambda e: e.tensor_tensor(y3, y3, bc3(s8[:], 8, 64), ALU.subtract),
                         reads=["y_f", "s8"], writes=["y_f"])
                    S.op("pool", lambda e: e.tensor_tensor(t0[:], y_f[:], y_f[:], ALU.mult), reads=["y_f"], writes=["t0"])
                    S.op("dve", lambda e: e.tensor_reduce(s8[:], v3(t0[:], 8, 64), AX.X, ALU.add), reads=["t0"], writes=["s8"])
                    S.op("dve", lambda e: e.tensor_scalar(s8[:], s8[:], 1.0 / 64, 64e-5, ALU.mult, ALU.add),
                         reads=["s8"], writes=["s8"])
                    S.op("act", lambda e: e.activation(s8[:], s8[:], AF.Sqrt), reads=["s8"], writes=["s8"])
                    S.op("dve", lambda e: e.reciprocal(s8[:], s8[:]), reads=["s8"], writes=["s8"])
                    S.op("dve", lambda e: e.tensor_tensor(y3, y3, bc3(s8[:], 8, 64), ALU.mult), reads=["y_f", "s8"], writes=["y_f"])
                    S.op("pool", lambda e: e.tensor_tensor(y_f[:], y_f[:], bcn["ln_x_w"][:], ALU.mult),
                         reads=["y_f", "bc_ln_x_w"], writes=["y_f"])
                    S.op("pool", lambda e: e.tensor_tensor(y_f[:], y_f[:], bcn["ln_x_b"][:], ALU.add),
                         reads=["y_f", "bc_ln_x_b"], writes=["y_f"])
                    S.op("dve", lambda e: e.tensor_tensor(v3(t0[:], 8, 64), v3(v_b[:], 8, 64), bc3(bon8[:], 8, 64), ALU.mult),
                         reads=["v_b", "bon8"], writes=["t0"])
                    S.op("pool", lambda e: e.tensor_tensor(y_f[:], y_f[:], t0[:], ALU.add), reads=["y_f", "t0"], writes=["y_f"])
                    S.op("dve", lambda e: e.tensor_tensor(mixed[:, 0:512], y_f[:], g_f[:], ALU.mult),
                         reads=["y_f", "g_f"], writes=["mixed"])

                    chk("L")
                    ps, pk = getps()
                    psb = ps[:].bitcast(BF16)
                    for c in range(8):
                        S.op("pe", lambda e, c=c, psb=psb: e.transpose(psb[:, c * 128:(c + 1) * 128],
                                                                       mixed[:, c * 128:(c + 1) * 128], ident_b[:]),
                             reads=["mixed", "ident_b"], writes=[pk])
                    S.op("act", lambda e, psb=psb: e.activation(mT[:], psb.rearrange("p (c t) -> p c t", c=8), AF.Copy),
                         reads=[pk], writes=["mT"])
                    for nh in range(2):
                        ps, pk = getps()
                        for c in range(8):
                            S.op("pe", lambda e, c=c, nh=nh, ps=ps: e.matmul(ps[:], mT[:, c, :], wo[:, c, nh * 512:(nh + 1) * 512],
                                                                             start=(c == 0), stop=(c == 7)),
                                 reads=["mT", "wo"], writes=[pk])
                        S.op("dve", lambda e, nh=nh, ps=ps, xt=xt: e.tensor_tensor(xt[:, nh * 512:(nh + 1) * 512],
                                                                            xt[:, nh * 512:(nh + 1) * 512], ps[:], ALU.add),
                             reads=[pk, mxk], writes=[mxk])
                  except _StopTile:
                    pass
                  for s in range(2):
                        r0 = s * seq + n * 64
                        S.dma("sp", out[r0:r0 + 64, :], xt[s * 64:(s + 1) * 64, :], reads=[mxk], sem=f"ms{n % 2}")
                S.barrier()
            src_x[0] = out

        for l in range(depth):
            if do_ffn:
                ffn_phase(l, 1, False)
            if do_mix:
                mix_phase(l)
            if do_ffn:
                ffn_phase(l, 2, final_norm and l == depth - 1)
        S.emit(final_waits=[k for k in S.dma_counts if k.startswith("fs") or k.startswith("ms")])
    return nc


_CACHE = {}


def kernel(**inputs):
    x = np.ascontiguousarray(inputs["x"], dtype=np.float32)
    B, seq, d = x.shape
    depth = inputs["w_in"].shape[0]
    key = (seq, depth)
    if key not in _CACHE:
        _CACHE[key] = build_program(seq, depth)
    nc = _CACHE[key]
    consts = make_consts(seq)
    shared = {}
    for k, v in inputs.items():
        if k == "x":
            continue
        a = np.ascontiguousarray(v, dtype=np.float32)
        if k == "r_k":
            a = a.reshape(depth, 512)
        if k == "final_norm":
            a = a.reshape(1, D)
        shared[k] = a
    for k in CONST_ORDER:
        shared["c_" + k] = consts[k]
    ncores = B // 2
    in_maps = []
    for c in range(ncores):
        m = dict(shared)
        m["x"] = x[2 * c:2 * c + 2].reshape(2 * seq, d)
        in_maps.append(m)
    res = run_bass_kernel_spmd(nc, in_maps, core_ids=list(range(ncores)))
    outs = [r["out"].reshape(2, seq, d) for r in res.results]
    return np.concatenate(outs, axis=0).astype(np.float32)
```

```python
import contextlib
import numpy as np
import concourse.bass as bass
import concourse.mybir as mybir
from concourse.bass_utils import run_bass_kernel_spmd

F32 = mybir.dt.float32
BF16 = mybir.dt.bfloat16
AF = mybir.ActivationFunctionType
ALU = mybir.AluOpType
AX = mybir.AxisListType

D = 1024
DFF = 2816
NF = DFF // 128
PROJ = 3328
RW_IN = 1792
NCORES = 8
ENGS = ("pe", "act", "dve", "pool", "sp")
C_DEC = float(np.exp(-0.5))


class _StopTile(Exception):
    pass


class _Op:
    __slots__ = ("eng", "fn", "deps", "dma_deps", "idx", "needs_inc", "count",
                 "dma_sem", "epoch")


class Sched:
    def __init__(self, nc):
        self.nc = nc
        self.streams = {e: [] for e in ENGS}
        self.last_w = {}
        self.readers = {}
        self.seen = {e: {} for e in ENGS}
        self.seen_dma = {e: {} for e in ENGS}
        self.dma_counts = {}
        self.epoch = 0
        self.alias = {}

    def _new(self, eng, fn):
        o = _Op()
        o.eng = eng
        o.fn = fn
        o.deps = []
        o.dma_deps = []
        o.idx = len(self.streams[eng])
        o.needs_inc = False
        o.count = None
        o.dma_sem = None
        o.epoch = self.epoch
        return o

    def _collect(self, op, reads, writes):
        deps = []
        for k in reads:
            w = self.last_w.get(k)
            if w is not None:
                deps.append((w, "raw"))
        for k in writes:
            w = self.last_w.get(k)
            if w is not None:
                deps.append((w, "waw"))
            for r in self.readers.get(k, ()):
                deps.append((r, "war"))
        e = op.eng
        for d, kind in deps:
            if d is op:
                continue
            if d.dma_sem is not None:
                cnt = self.dma_counts[d.dma_sem]
                if self.seen_dma[e].get(d.dma_sem, 0) < cnt:
                    self.seen_dma[e][d.dma_sem] = cnt
                    op.dma_deps.append((d.dma_sem, cnt))
                continue
            if d.epoch != self.epoch:
                continue
            if d.eng == e and e == "pe":
                continue
            if self.seen[e].get(d.eng, -1) >= d.idx:
                continue
            self.seen[e][d.eng] = d.idx
            d.needs_inc = True
            op.deps.append(d)
        for k in reads:
            self.readers.setdefault(k, []).append(op)
        for k in writes:
            self.last_w[k] = op
            self.readers[k] = []

    def op(self, eng, fn, reads=(), writes=()):
        reads = [self.alias.get(k, k) for k in reads]
        writes = [self.alias.get(k, k) for k in writes]
        o = self._new(eng, fn)
        self._collect(o, reads, writes)
        self.streams[eng].append(o)
        return o

    def dma(self, queue, out, in_, reads=(), writes=(), sem="dma0"):
        def fn(eng, out=out, in_=in_):
            return eng.dma_start(out=out, in_=in_)
        sem = f"{sem}_{queue}"
        o = self.op(queue, fn, reads, writes)
        o.dma_sem = sem
        self.dma_counts[sem] = self.dma_counts.get(sem, 0) + 1
        return o

    def barrier(self):
        lasts = {}
        for e in ENGS:
            for o in reversed(self.streams[e]):
                if o.epoch != self.epoch:
                    break
                if o.dma_sem is None and o.fn is not None:
                    lasts[e] = o
                    break
        for e in ENGS:
            o = self._new(e, None)
            for e2, l in lasts.items():
                if e2 == e and e == "pe":
                    continue
                if self.seen[e].get(e2, -1) < l.idx:
                    l.needs_inc = True
                    o.deps.append(l)
            for s, c in self.dma_counts.items():
                if self.seen_dma[e].get(s, 0) < c:
                    self.seen_dma[e][s] = c
                    o.dma_deps.append((s, c))
            self.streams[e].append(o)
        self.epoch += 1
        self.seen = {e: {} for e in ENGS}

    def emit(self, final_waits=()):
        nc = self.nc
        n_epochs = self.epoch + 1
        for e in ENGS:
            c = 0
            ep = 0
            for o in self.streams[e]:
                if o.epoch != ep:
                    ep = o.epoch
                    c = 0
                if o.needs_inc:
                    c += 1
                    o.count = c
        with contextlib.ExitStack() as st:
            esem = {}
            for e in ENGS:
                used = set(o.epoch for o in self.streams[e] if o.needs_inc)
                for ep in sorted(used):
                    esem[(e, ep)] = st.enter_context(nc.semaphore(f"s_{e}_{ep}"))
            dsem = {s: st.enter_context(nc.semaphore(f"d_{s}")) for s in self.dma_counts}
            block = st.enter_context(nc.Block())

            def replay(e, eng):
                for o in self.streams[e]:
                    for d in o.deps:
                        eng.wait_ge(esem[(d.eng, d.epoch)], d.count)
                    for s, c in o.dma_deps:
                        eng.wait_ge(dsem[s], 16 * c)
                    if o.fn is None:
                        continue
                    ins = o.fn(eng)
                    if o.dma_sem is not None:
                        ins.then_inc(dsem[o.dma_sem], 16)
                    elif o.needs_inc:
                        ins.then_inc(esem[(o.eng, o.epoch)], 1)
                if e == "sp":
                    for s in final_waits:
                        eng.wait_ge(dsem[s], 16 * self.dma_counts[s])

            @block.tensor
            def _(eng):
                replay("pe", eng)

            @block.scalar
            def _(eng):
                replay("act", eng)

            @block.vector
            def _(eng):
                replay("dve", eng)

            @block.gpsimd
            def _(eng):
                replay("pool", eng)

            @block.sync
            def _(eng):
                replay("sp", eng)


def make_consts(seq):
    nt = seq // 64
    p = np.arange(128)
    s_of = p // 64
    t_of = p % 64
    same = (s_of[:, None] == s_of[None, :])
    c = {}
    c["ident"] = np.eye(128, dtype=np.float32)
    c["tri_i"] = (1.0 * (same & (t_of[:, None] <= t_of[None, :]))).astype(np.float32)
    c["tri_r"] = (1.0 * (same & (t_of[:, None] > t_of[None, :]))).astype(np.float32)
    seqind = np.zeros((128, 2), np.float32)
    seqind[p, s_of] = 1.0
    c["seqind"] = seqind
    su = (same & (t_of[:, None] < t_of[None, :])).astype(np.float32)
    ui = (same & (t_of[:, None] <= t_of[None, :])).astype(np.float32)
    sl = (same & (t_of[:, None] > t_of[None, :])).astype(np.float32)
    c["m1"] = np.concatenate([su, ui], axis=1)
    c["msl"] = sl
    H = 4
    log_g = np.log(1.0 - np.power(2.0, -5.0 - np.arange(H, dtype=np.float64)))
    j = t_of[:, None].astype(np.float64)
    i = t_of[None, :].astype(np.float64)
    dm = np.zeros((128, H, 128), np.float64)
    for h in range(H):
        dm[:, h, :] = same * np.exp(log_g[h] * (np.abs(i - j) - (i + 1.0))) * 0.125
    c["dmask"] = dm.reshape(128, H * 128).astype(np.float32)
    qw = np.exp(log_g[None, :] * (t_of[:, None] + 1.0))
    kw = np.exp(log_g[None, :] * (63.0 - t_of[:, None])) * 0.125
    cd = np.broadcast_to(np.exp(log_g * 64.0)[None, :], (128, H))
    c["qkw"] = np.concatenate([qw, kw, cd], axis=1).astype(np.float32)
    half = 32
    inv_freq = (1.0 / (np.float32(10000.0) ** np.linspace(0.0, 1.0, half, dtype=np.float32))).astype(np.float32)
    pos = np.arange(seq, dtype=np.float32)
    ang = (pos[:, None] * inv_freq[None, :]).astype(np.float32).astype(np.float64)
    cs = np.concatenate([np.cos(ang), np.sin(ang)], axis=1).astype(np.float32)
    cs = cs.reshape(nt, 64, 64).transpose(1, 0, 2)
    c["rope"] = np.ascontiguousarray(np.concatenate([cs, cs], axis=0).reshape(128, nt * 64))
    return c


CONST_ORDER = ["ident", "tri_i", "tri_r", "seqind", "m1", "msl", "dmask", "qkw", "rope"]


def build_program(seq, depth, do_ffn=True, do_mix=True, final_norm=True):
    nc = bass.Bass("TRN2", target_bir_lowering=False)
    ntok = 2 * seq
    nt = seq // 64

    def din(name, shape):
        return nc.dram_tensor(name, list(shape), F32, kind="ExternalInput").ap()

    x_in = din("x", [ntok, D])
    w = {}
    for nm, shp in [("ffn1_norm", [depth, D]), ("ffn1_w_gate", [depth, D, DFF]), ("ffn1_w_up", [depth, D, DFF]),
                    ("ffn1_w_down", [depth, DFF, D]), ("mix_norm", [depth, D]), ("w_in", [depth, D, PROJ]),
                    ("shift_mu", [depth, RW_IN]), ("w0", [depth, 512]), ("w_lora_up", [depth, 64, 512]),
                    ("a0", [depth, 512]), ("a_lora_up", [depth, 64, 512]), ("g_lora_up", [depth, 128, 512]),
                    ("k_k", [depth, 512]), ("k_a", [depth, 512]), ("r_k", [depth, 512]),
                    ("ln_x_w", [depth, 512]), ("ln_x_b", [depth, 512]), ("w_out", [depth, D, D]),
                    ("ffn2_norm", [depth, D]), ("ffn2_w_gate", [depth, D, DFF]), ("ffn2_w_up", [depth, D, DFF]),
                    ("ffn2_w_down", [depth, DFF, D]), ("final_norm", [1, D])]:
        w[nm] = din(nm, shp)
    cshape = {"ident": 128, "tri_i": 128, "tri_r": 128, "seqind": 2, "m1": 256, "msl": 128,
              "dmask": 512, "qkw": 12, "rope": nt * 64}
    cd = {k: din("c_" + k, [128, v]) for k, v in cshape.items()}
    out = nc.dram_tensor("out", [ntok, D], F32, kind="ExternalOutput").ap()

    S = Sched(nc)
    st = contextlib.ExitStack()
    with st:
        uid = [0]

        def sb(name, shape, dt=F32, stack=st):
            uid[0] += 1
            return stack.enter_context(nc.sbuf_tensor(f"{name}_{uid[0]}", list(shape), dt))

        banks = [st.enter_context(nc.psum_tensor(f"ps{i}", [128, 512], F32)) for i in range(8)]
        pctr = [0]

        def getps():
            i = pctr[0] % 8
            pctr[0] += 1
            return banks[i], f"ps{i}"

        ident_b = sb("ident_b", [128, 128], BF16)
        tri_i = sb("tri_i", [128, 128], BF16)
        tri_r = sb("tri_r", [128, 128], BF16)
        seqind = sb("seqind", [128, 2], BF16)
        m1 = sb("m1", [128, 256])
        msl = sb("msl", [128, 128])
        ident_f = sb("ident_f", [128, 128])
        dmask = sb("dmask", [128, 512])
        qkw = sb("qkw", [128, 12])
        S.dma("pool", ident_b[:], cd["ident"], writes=["ident_b"], sem="c")
        S.dma("sp", ident_f[:], cd["ident"], writes=["ident_f"], sem="c")
        S.dma("pool", tri_i[:], cd["tri_i"], writes=["tri_i"], sem="c")
        S.dma("pool", tri_r[:], cd["tri_r"], writes=["tri_r"], sem="c")
        S.dma("pool", seqind[:], cd["seqind"], writes=["seqind"], sem="c")
        S.dma("sp", m1[:], cd["m1"], writes=["m1"], sem="c")
        S.dma("sp", msl[:], cd["msl"], writes=["msl"], sem="c")
        S.dma("sp", dmask[:], cd["dmask"], writes=["dmask"], sem="c")
        S.dma("sp", qkw[:], cd["qkw"], writes=["qkw"], sem="c")

        src_x = [x_in]

        def rstd_ops(ss, n, tag):
            S.op("dve", lambda e: e.tensor_scalar(ss[:, 0:n], ss[:, 0:n], 1.0 / D, 1e-6, ALU.mult, ALU.add),
                 reads=[tag], writes=[tag])
            S.op("act", lambda e: e.activation(ss[:, 0:n], ss[:, 0:n], AF.Sqrt), reads=[tag], writes=[tag])
            S.op("dve", lambda e: e.reciprocal(ss[:, 0:n], ss[:, 0:n]), reads=[tag], writes=[tag])

        def ffn_phase(l, which, last):
            TB = 256
            nblk = ntok // TB
            with contextlib.ExitStack() as fs:
                wg = sb("wg", [128, 8, DFF], BF16, fs)
                wu = sb("wu", [128, 8, DFF], BF16, fs)
                wd = sb("wd", [128, NF, D], BF16, fs)
                gain = sb("gain", [128, D], F32, fs)
                xt = [sb(f"fx{i}", [128, 2, D], F32, fs) for i in range(2)]
                hb = sb("fh", [128, 2, D], BF16, fs)
                hT = sb("fhT", [128, 8, TB], BF16, fs)
                aT = sb("faT", [128, NF, TB], BF16, fs)
                sg = [sb(f"fsg{i}", [128, TB], F32, fs) for i in range(2)]
                junk = sb("fjunk", [128, D], BF16, fs)
                ss = [sb(f"fss{i}", [128, 4], F32, fs) for i in range(2)]
                if last:
                    fin_bc = sb("fin_bc", [128, D], F32, fs)
                    S.dma("sp", fin_bc[:], w["final_norm"][0:1, :].broadcast_to([128, D]), writes=["fin_bc"], sem="w")
                pre = "ffn1" if which == 1 else "ffn2"
                S.dma("sp", gain[:], w[pre + "_norm"][l:l + 1, :].broadcast_to([128, D]), writes=["gain"], sem="w")
                for c in range(8):
                    S.dma("pool", wg[:, c, :], w[pre + "_w_gate"][l, c * 128:(c + 1) * 128, :], writes=["wg"], sem="wgu")
                    S.dma("pool", wu[:, c, :], w[pre + "_w_up"][l, c * 128:(c + 1) * 128, :], writes=["wu"], sem="wgu")
                for f in range(NF):
                    S.dma("pool", wd[:, f, :], w[pre + "_w_down"][l, f * 128:(f + 1) * 128, :], writes=["wd"], sem="wd")

                def load(b):
                    i = b % 2
                    src = src_x[0]
                    for j in range(2):
                        r0 = b * TB + j * 128
                        S.dma("sp", xt[i][:, j, :], src[r0:r0 + 128, :], writes=[f"fx{i}"], sem=f"fl{i}")

                def norm(b):
                    i = b % 2
                    X = xt[i]
                    xk = f"fx{i}"
                    ssb = ss[i]
                    sk = f"fss{i}"
                    for j in range(2):
                        S.op("act", lambda e, j=j, X=X, ssb=ssb: e.activation(junk[:], X[:, j, :], AF.Square,
                                                                              accum_out=ssb[:, j:j + 1]),
                             reads=[xk], writes=["fjunk", sk])
                    rstd_ops(ssb, 2, sk)
                    for j in range(2):
                        S.op("dve", lambda e, j=j, X=X, ssb=ssb: e.scalar_tensor_tensor(
                            hb[:, j, :], X[:, j, :], ssb[:, j:j + 1], gain[:], ALU.mult, ALU.mult),
                            reads=[xk, sk, "gain"], writes=["fh"])

                load(0)
                norm(0)
                for b in range(nblk):
                    i = b % 2
                    X = xt[i]
                    xk = f"fx{i}"
                    if b + 1 < nblk:
                        load(b + 1)
                    ssb = ss[i]
                    sk = f"fss{i}"
                    for half in range(2):
                        ps, pk = getps()
                        psb = ps[:].bitcast(BF16)
                        for cc in range(4):
                            c = half * 4 + cc
                            for j in range(2):
                                S.op("pe", lambda e, c=c, cc=cc, j=j, psb=psb: e.transpose(
                                    psb[:, cc * 256 + j * 128: cc * 256 + (j + 1) * 128],
                                    hb[:, j, c * 128:(c + 1) * 128], ident_b[:]),
                                    reads=["fh", "ident_b"], writes=[pk])
                        eng = "act" if half == 0 else "dve"
                        if eng == "act":
                            S.op("act", lambda e, half=half, psb=psb: e.activation(
                                hT[:, half * 4:(half + 1) * 4, :], psb.rearrange("p (c t) -> p c t", c=4), AF.Copy),
                                reads=[pk], writes=["fhT"])
                        else:
                            S.op("dve", lambda e, half=half, psb=psb: e.tensor_copy(
                                hT[:, half * 4:(half + 1) * 4, :], psb.rearrange("p (c t) -> p c t", c=4)),
                                reads=[pk], writes=["fhT"])
                    for f in range(NF):
                        ps, pk = getps()
                        for c in range(8):
                            S.op("pe", lambda e, c=c, f=f, ps=ps: e.matmul(
                                ps[:, 0:TB], wg[:, c, f * 128:(f + 1) * 128], hT[:, c, :],
                                start=(c == 0), stop=(c == 7)), reads=["wg", "fhT"], writes=[pk])
                        for c in range(8):
                            S.op("pe", lambda e, c=c, f=f, ps=ps: e.matmul(
                                ps[:, TB:2 * TB], wu[:, c, f * 128:(f + 1) * 128], hT[:, c, :],
                                start=(c == 0), stop=(c == 7)), reads=["wu", "fhT"], writes=[pk])
                        sgb = sg[f % 2]
                        sgk = f"fsg{f % 2}"
                        S.op("act", lambda e, ps=ps, sgb=sgb: e.activation(sgb[:], ps[:, 0:TB], AF.Silu),
                             reads=[pk], writes=[sgk])
                        S.op("dve", lambda e, ps=ps, sgb=sgb, f=f: e.tensor_tensor(
                            aT[:, f, :], sgb[:], ps[:, TB:2 * TB], ALU.mult),
                            reads=[pk, sgk], writes=["faT"])
                    if b + 1 < nblk:
                        norm(b + 1)
                    for j in range(2):
                        for n in range(2):
                            ps, pk = getps()
                            for f in range(NF):
                                S.op("pe", lambda e, f=f, j=j, n=n, ps=ps: e.matmul(
                                    ps[:], aT[:, f, j * 128:(j + 1) * 128], wd[:, f, n * 512:(n + 1) * 512],
                                    start=(f == 0), stop=(f == NF - 1)), reads=["faT", "wd"], writes=[pk])
                            S.op("dve", lambda e, j=j, n=n, ps=ps, X=X: e.scalar_tensor_tensor(
                                X[:, j, n * 512:(n + 1) * 512], ps[:], 0.5, X[:, j, n * 512:(n + 1) * 512],
                                ALU.mult, ALU.add), reads=[pk, xk], writes=[xk])
                    if last:
                        for j in range(2):
                            S.op("act", lambda e, j=j, X=X, ssb=ssb: e.activation(
                                junk[:], X[:, j, :], AF.Square, accum_out=ssb[:, 2 + j:3 + j]),
                                reads=[xk], writes=["fjunk", sk])
                        S.op("dve", lambda e, ssb=ssb: e.tensor_scalar(ssb[:, 2:4], ssb[:, 2:4], 1.0 / D, 1e-6,
                                                                       ALU.mult, ALU.add), reads=[sk], writes=[sk])
                        S.op("act", lambda e, ssb=ssb: e.activation(ssb[:, 2:4], ssb[:, 2:4], AF.Sqrt),
                             reads=[sk], writes=[sk])
                        S.op("dve", lambda e, ssb=ssb: e.reciprocal(ssb[:, 2:4], ssb[:, 2:4]), reads=[sk], writes=[sk])
                        for j in range(2):
                            S.op("dve", lambda e, j=j, X=X, ssb=ssb: e.scalar_tensor_tensor(
                                X[:, j, :], X[:, j, :], ssb[:, 2 + j:3 + j], fin_bc[:], ALU.mult, ALU.mult),
                                reads=[xk, sk, "fin_bc"], writes=[xk])
                    for j in range(2):
                        r0 = b * TB + j * 128
                        S.dma("sp", out[r0:r0 + 128, :], X[:, j, :], reads=[xk], sem=f"fs{i}")
                S.barrier()
            src_x[0] = out

        def mix_phase(l):
            with contextlib.ExitStack() as ms:
                wm = sb("wm", [128, 8, 5120], BF16, ms)
                wo = sb("wo", [128, 8, D], BF16, ms)
                wal = sb("wal", [128, 1024], BF16, ms)
                glu = sb("glu", [128, 512], BF16, ms)
                gain = sb("mgain", [128, D], F32, ms)
                bcn = {}
                for nm in ["w0", "a0", "k_k", "k_a", "r_k", "ln_x_w", "ln_x_b"]:
                    bcn[nm] = sb("bc_" + nm, [128, 512], F32, ms)
                    S.dma("sp", bcn[nm][:], w[nm][l:l + 1, :].broadcast_to([128, 512]), writes=["bc_" + nm], sem="w")
                S.dma("sp", gain[:], w["mix_norm"][l:l + 1, :].broadcast_to([128, D]), writes=["mgain"], sem="w")
                S.op("pool", lambda e: e.memset(wal[:], 0.0), writes=["wal"])
                S.dma("pool", wal[0:64, 0:512], w["w_lora_up"][l], writes=["wal"], sem="w")
                S.dma("pool", wal[64:128, 512:1024], w["a_lora_up"][l], writes=["wal"], sem="w")
                S.dma("pool", glu[:], w["g_lora_up"][l], writes=["glu"], sem="w")
                for c in range(8):
                    S.dma("pool", wo[:, c, :], w["w_out"][l, c * 128:(c + 1) * 128, :], writes=["wo"], sem="w")
                    S.dma("pool", wm[:, c, 3584:5120], w["w_in"][l, c * 128:(c + 1) * 128, RW_IN:PROJ],
                          writes=["wm"], sem="w")
                with contextlib.ExitStack() as ps_:
                    mu = sb("mu", [128, RW_IN], F32, ps_)
                    omm = sb("omm", [128, RW_IN], F32, ps_)
                    stg = [sb(f"stg{i}", [128, RW_IN], F32, ps_) for i in range(2)]
                    S.dma("sp", mu[:], w["shift_mu"][l:l + 1, :].broadcast_to([128, RW_IN]), writes=["mu"], sem="w")
                    S.op("dve", lambda e: e.tensor_scalar(omm[:], mu[:], -1.0, 1.0, ALU.mult, ALU.add),
                         reads=["mu"], writes=["omm"])
                    for c in range(8):
                        sg_ = stg[c % 2]
                        sk_ = f"stg{c % 2}"
                        S.dma("sp", sg_[:], w["w_in"][l, c * 128:(c + 1) * 128, 0:RW_IN], writes=[sk_], sem=f"wp{c % 2}")
                        S.op("dve", lambda e, c=c, sg_=sg_: e.tensor_tensor(wm[:, c, 0:RW_IN], sg_[:], omm[:], ALU.mult),
                             reads=[sk_, "omm"], writes=["wm"])
                        S.op("pool", lambda e, c=c, sg_=sg_: e.tensor_tensor(wm[:, c, RW_IN:2 * RW_IN], sg_[:], mu[:], ALU.mult),
                             reads=[sk_, "mu"], writes=["wm"])
                    S.barrier()

                xts = [sb(f"mx{i}", [128, D], F32, ms) for i in range(2)]
                hb = sb("mh", [128, D], BF16, ms)
                junk = hb
                ss = sb("mss", [128, 2], F32, ms)
                hT = sb("mhT", [128, 8, 128], BF16, ms)
                hTp = sb("mhTp", [128, 8, 128], BF16, ms)
                carry = sb("mcarry", [128, 8, 2], BF16, ms)
                ropet = [sb(f"ropet{i}", [128, 64], F32, ms) for i in range(2)]
                G = {i: sb(f"G{i}", [128, 512], F32, ms) for i in (1, 2, 3, 4, 6, 7, 8, 9, 10)}
                G5 = sb("G5", [128, 1024], F32, ms)

                def bf3(t, h):
                    return t[:].bitcast(BF16).rearrange("p (h t) -> p h t", h=h)
                qk_f = Wi = G[1][:]
                rg_s = Wn = G[2][:]
                rot = We = G[3][:]
                o_f = Wh = G[4][:]
                Pm = [bf3(G[1], 8), bf3(G[2], 8)]
                Qm = [bf3(G[3], 8), bf3(G[4], 8)]
                ra = G5[:, 0:256]
                rb = G5[:, 256:512]
                kk = G5[:, 0:512]
                bp = G5[:, 512:1024]
                g5b = G5[:].bitcast(BF16)
                Pp = g5b[:, 1024:2048].rearrange("p (h t) -> p h t", h=8)
                sigw = G[6][:]
                TTbs = [bf3(G[6], 8), g5b[:, 0:1024].rearrange("p (h t) -> p h t", h=8)]
                r_f = y_f = G[7][:]
                k_f = G[8][:]
                mT = bf3(G[8], 8)
                g9 = G[9][:].bitcast(BF16)
                g10 = G[10][:].bitcast(BF16)
                tm4 = [g9[:, 0:512], g9[:, 512:1024], g10[:, 0:512], g10[:, 512:1024]]
                Xb = g10[:, 0:512].rearrange("p (h v) -> p h v", h=8)
                Ub = g10[:, 512:1024].rearrange("p (h v) -> p h v", h=8)
                S.alias.update({"qk_f": "G1", "Wi": "G1", "Pm0": "G1", "rg_s": "G2", "Wn": "G2", "Pm1": "G2",
                                "rot": "G3", "We": "G3", "Qm0": "G3", "o_f": "G4", "Wh": "G4", "Qm1": "G4",
                                "ra": "G5", "rb": "G5", "kk": "G5", "bp": "G5", "TTb1": "G5", "Pp": "G5", "sigw": "G6", "TTb0": "G6",
                                "r_f": "G7", "y_f": "G7", "k_f": "G8", "mT": "G8", "tm0": "G9", "tm1": "G9",
                                "tm2": "G10", "tm3": "G10", "Xb": "G10", "Ub": "G10", "mjunk": "mh"})
                v_b = sb("v_b", [128, 512], BF16, ms)
                lwT = sb("lwT", [128, 128], BF16, ms)
                lgT = sb("lgT", [128, 128], BF16, ms)
                rv_b = sb("rv_b", [128, 512], BF16, ms)
                a_f = sb("a_f", [128, 512], F32, ms)
                g_f = sb("g_f", [128, 512], F32, ms)
                WC = sb("WC", [128, 8], F32, ms)
                sighl = sb("sighl", [128, 1024], BF16, ms)
                t0 = sb("t0", [128, 512], F32, ms)
                t1 = sb("t1", [128, 512], F32, ms)
                k_h = sb("k_h", [128, 512], F32, ms)
                s8 = sb("s8", [128, 8], F32, ms)
                bon8 = sb("bon8", [128, 8], F32, ms)
                arT = sb("arT", [128, 8, 2, 128], BF16, ms)
                bT = sb("bT", [128, 8, 128], BF16, ms)
                kT = sb("kT", [128, 8, 128], BF16, ms)
                Bh = sb("Bh", [128, 8, 128], BF16, ms)
                Kh = sb("Kh", [128, 8, 128], BF16, ms)
                QA = sb("QA", [128, 8, 2, 128], BF16, ms)
                KA = sb("KA", [128, 8, 2, 128], BF16, ms)
                Sf = sb("Sf", [128, 8, 64], F32, ms)
                Sb = sb("Sb", [128, 8, 64], BF16, ms)
                mixed = sb("mixed", [128, D], BF16, ms)
                qt_b = sb("qt_b", [128, 256], BF16, ms)
                kp_b = sb("kp_b", [128, 256], BF16, ms)
                rKh = sb("rKh", [128, 4, 128], BF16, ms)
                rqT = sb("rqT", [128, 4, 128], BF16, ms)
                rkT = sb("rkT", [128, 4, 128], BF16, ms)
                Sc = sb("Sc", [128, 4, 128], BF16, ms)
                RSf = sb("RSf", [128, 4, 128], F32, ms)
                RSb = sb("RSb", [128, 4, 128], BF16, ms)
                s4 = sb("s4", [128, 4], F32, ms)

                for tname, tt in [("arT", arT), ("bT", bT), ("kT", kT), ("Bh", Bh), ("Kh", Kh), ("rKh", rKh),
                                  ("rqT", rqT), ("rkT", rkT), ("Sb", Sb), ("RSb", RSb)]:
                    S.op("pool", lambda e, tt=tt: e.memset(tt[:], 0.0), writes=[tname])
                S.op("dve", lambda e: e.memset(Sf[:], 0.0), writes=["Sf"])
                S.op("dve", lambda e: e.memset(RSf[:], 0.0), writes=["RSf"])
                S.op("dve", lambda e: e.memset(carry[:], 0.0), writes=["mcarry"])

                def bc3(ap2, n_in, n_out):
                    return ap2.unsqueeze(2).to_broadcast([ap2.shape[0], n_in, n_out])

                def bch(ap2, nh, ncol):
                    return ap2.unsqueeze(1).to_broadcast([ap2.shape[0], nh, ncol])

                def v3(ap2, a, b_):
                    return ap2.rearrange("p (a b) -> p a b", a=a)

                src = src_x[0]
                import os as _os
                _stop = _os.environ.get("MIX_STOP", "")

                def chk(tag):
                    if tag == _stop:
                        raise _StopTile()

                def front(n):
                    xt = xts[n % 2]
                    mxk = f"mx{n % 2}"
                    for s in range(2):
                        r0 = s * seq + n * 64
                        S.dma("sp", xt[s * 64:(s + 1) * 64, :], src[r0:r0 + 64, :], writes=[mxk], sem=f"ml{n % 2}")
                    S.op("act", lambda e, xt=xt: e.activation(junk[:], xt[:], AF.Square, accum_out=ss[:, 0:1]),
                         reads=[mxk], writes=["mjunk", "mss"])
                    rstd_ops(ss, 1, "mss")
                    S.op("dve", lambda e, xt=xt: e.scalar_tensor_tensor(hb[:], xt[:], ss[:, 0:1], gain[:], ALU.mult, ALU.mult),
                         reads=[mxk, "mss", "mgain"], writes=["mh"])
                    ps, pk = getps()
                    psb = ps[:].bitcast(BF16)
                    for c in range(8):
                        S.op("pe", lambda e, c=c, psb=psb: e.transpose(psb[:, c * 128:(c + 1) * 128],
                                                                       hb[:, c * 128:(c + 1) * 128], ident_b[:]),
                             reads=["mh", "ident_b"], writes=[pk])
                    S.op("act", lambda e, psb=psb: e.activation(
                        hT[:], psb.rearrange("p (c t) -> p c t", c=8), AF.Copy),
                        reads=[pk], writes=["mhT"])
                    hT4 = hT[:].rearrange("p c (s t) -> p c s t", s=2)
                    hTp4 = hTp[:].rearrange("p c (s t) -> p c s t", s=2)
                    S.op("pool", lambda e: e.tensor_copy(hTp4[:, :, :, 1:64], hT4[:, :, :, 0:63]),
                         reads=["mhT"], writes=["mhTp"])
                    S.op("pool", lambda e: e.tensor_copy(hTp4[:, :, :, 0], carry[:]),
                         reads=["mcarry"], writes=["mhTp"])
                    S.op("pool", lambda e: e.tensor_copy(carry[:], hT4[:, :, :, 63]),
                         reads=["mhT"], writes=["mcarry"])


                front(0)
                for n in range(nt):
                  try:
                    xt = xts[n % 2]
                    mxk = f"mx{n % 2}"
                    def cur(c):
                        return hT[:, c, :]

                    def prev(c):
                        return hTp[:, c, :]

                    chk("D")
                    def proj_rw(ps_ap, col0, ncol, pk):
                        for c in range(8):
                            S.op("pe", lambda e, c=c: e.matmul(ps_ap, cur(c), wm[:, c, col0:col0 + ncol],
                                                               start=(c == 0), stop=False),
                                 reads=["mhT", "wm"], writes=[pk])
                        for c in range(8):
                            S.op("pe", lambda e, c=c: e.matmul(ps_ap, prev(c), wm[:, c, RW_IN + col0:RW_IN + col0 + ncol],
                                                               start=False, stop=(c == 7)),
                                 reads=["mhTp", "wm"], writes=[pk])

                    ps, pk = getps()
                    proj_rw(ps[:], 0, 512, pk)
                    S.op("act", lambda e, ps=ps: e.activation(r_f[:], ps[:], AF.Copy), reads=[pk], writes=["r_f"])
                    ps, pk = getps()
                    proj_rw(ps[:], 512, 512, pk)
                    S.op("dve", lambda e, ps=ps: e.tensor_copy(k_f[:], ps[:]), reads=[pk], writes=["k_f"])
                    ps, pk = getps()
                    proj_rw(ps[:], 1024, 512, pk)
                    S.op("dve", lambda e, ps=ps: e.tensor_copy(v_b[:], ps[:]), reads=[pk], writes=["v_b"])
                    ps, pk = getps()
                    for gi in range(2):
                        col0 = 1536 + gi * 128
                        for c in range(8):
                            S.op("pe", lambda e, c=c, gi=gi, col0=col0, ps=ps: e.matmul(
                                ps[:, gi * 128:(gi + 1) * 128], wm[:, c, col0:col0 + 128], cur(c),
                                start=(c == 0), stop=False), reads=["mhT", "wm"], writes=[pk])
                        for c in range(8):
                            S.op("pe", lambda e, c=c, gi=gi, col0=col0, ps=ps: e.matmul(
                                ps[:, gi * 128:(gi + 1) * 128], wm[:, c, RW_IN + col0:RW_IN + col0 + 128], prev(c),
                                start=False, stop=(c == 7)), reads=["mhTp", "wm"], writes=[pk])
                    S.op("act", lambda e, ps=ps: e.activation(lwT[0:64, :], ps[0:64, 0:128], AF.Tanh), reads=[pk], writes=["lwT"])
                    S.op("act", lambda e, ps=ps: e.activation(lwT[64:128, :], ps[64:128, 0:128], AF.Copy), reads=[pk], writes=["lwT"])
                    S.op("act", lambda e, ps=ps: e.activation(lgT[:], ps[:, 128:256], AF.Sigmoid), reads=[pk], writes=["lgT"])
                    ps, pk = getps()
                    for c in range(8):
                        S.op("pe", lambda e, c=c, ps=ps: e.matmul(ps[:], cur(c), wm[:, c, 3584:4096],
                                                                  start=(c == 0), stop=(c == 7)),
                             reads=["mhT", "wm"], writes=[pk])
                    S.op("dve", lambda e, ps=ps: e.tensor_copy(qk_f[:], ps[:]), reads=[pk], writes=["qk_f"])
                    ps, pk = getps()
                    for c in range(8):
                        S.op("pe", lambda e, c=c, ps=ps: e.matmul(ps[:], cur(c), wm[:, c, 4096:4608],
                                                                  start=(c == 0), stop=(c == 7)),
                             reads=["mhT", "wm"], writes=[pk])
                    S.op("act", lambda e, ps=ps: e.activation(rv_b[:], ps[:], AF.Copy), reads=[pk], writes=["rv_b"])
                    ps, pk = getps()
                    for c in range(8):
                        S.op("pe", lambda e, c=c, ps=ps: e.matmul(ps[:], cur(c), wm[:, c, 4608:5120],
                                                                  start=(c == 0), stop=(c == 7)),
                             reads=["mhT", "wm"], writes=[pk])
                    S.op("act", lambda e, ps=ps: e.activation(rg_s[:], ps[:], AF.Silu), reads=[pk], writes=["rg_s"])

                    chk("K")
                    if n + 1 < nt:
                        front(n + 1)
                    cs_ = ropet[n % 2]
                    ropek = f"ropet{n % 2}"
                    S.dma("sp", cs_[:], cd["rope"][:, n * 64:(n + 1) * 64], writes=[ropek], sem=f"rp{n % 2}")
                    cosb = cs_[:, 0:32].unsqueeze(1).to_broadcast([128, 8, 32])
                    sinb = cs_[:, 32:64].unsqueeze(1).to_broadcast([128, 8, 32])
                    qk4 = qk_f[:].rearrange("p (g a d) -> p g a d", g=8, a=2)
                    rot4 = rot[:].rearrange("p (g a d) -> p g a d", g=8, a=2)
                    ra3 = v3(ra[:], 8, 32)
                    rb3 = v3(rb[:], 8, 32)
                    S.op("dve", lambda e, cosb=cosb, sinb=sinb: e.tensor_tensor(ra3, qk4[:, :, 0, :], cosb, ALU.mult), reads=["qk_f", ropek], writes=["ra"])
                    S.op("pool", lambda e, cosb=cosb, sinb=sinb: e.tensor_tensor(rb3, qk4[:, :, 1, :], sinb, ALU.mult), reads=["qk_f", ropek], writes=["rb"])
                    S.op("dve", lambda e: e.tensor_tensor(rot4[:, :, 0, :], ra3, rb3, ALU.subtract), reads=["ra", "rb"], writes=["rot"])
                    S.op("dve", lambda e, cosb=cosb, sinb=sinb: e.tensor_tensor(ra3, qk4[:, :, 0, :], sinb, ALU.mult), reads=["qk_f", ropek], writes=["ra"])
                    S.op("pool", lambda e, cosb=cosb, sinb=sinb: e.tensor_tensor(rb3, qk4[:, :, 1, :], cosb, ALU.mult), reads=["qk_f", ropek], writes=["rb"])
                    S.op("dve", lambda e: e.tensor_tensor(rot4[:, :, 1, :], ra3, rb3, ALU.add), reads=["ra", "rb"], writes=["rot"])
                    S.op("dve", lambda e: e.tensor_tensor(v3(qt_b[:], 4, 64), v3(rot[:, 0:256], 4, 64), bc3(qkw[:, 0:4], 4, 64), ALU.mult),
                         reads=["rot", "qkw"], writes=["qt_b"])
                    S.op("pool", lambda e: e.tensor_copy(kp_b[:], rot[:, 256:512]), reads=["rot"], writes=["kp_b"])
                    for s in range(2):
                        sl_ = slice(s * 64, (s + 1) * 64)
                        S.op("dve" if s == 0 else "pool", lambda e, s=s, sl_=sl_: e.tensor_tensor(
                            rKh[sl_, :, s * 64:(s + 1) * 64], v3(rot[sl_, 256:512], 4, 64), bc3(qkw[sl_, 4:8], 4, 64), ALU.mult),
                            reads=["rot", "qkw"], writes=["rKh"])
                    chk("K1")
                    for gi, (srcb, skey, dst, dk) in enumerate([(qt_b, "qt_b", rqT, "rqT"), (kp_b, "kp_b", rkT, "rkT")]):
                        ps, pk = getps()
                        psb = ps[:].bitcast(BF16)
                        for pr in range(2):
                            S.op("pe", lambda e, pr=pr, psb=psb, srcb=srcb: e.transpose(
                                psb[:, pr * 128:(pr + 1) * 128], srcb[:, pr * 128:(pr + 1) * 128], ident_b[:]),
                                reads=[skey, "ident_b"], writes=[pk])
                        pv = psb[:, 0:256].rearrange("p (g t) -> p g t", g=2)
                        dst5 = dst[:].rearrange("p (pr par) t -> p pr par t", par=2)
                        for par in range(2):
                            for s in range(2):
                                d_ = dst5[s * 64:(s + 1) * 64, :, par, s * 64:(s + 1) * 64]
                                i_ = pv[par * 64:(par + 1) * 64, :, s * 64:(s + 1) * 64]
                                S.op("dve", lambda e, d_=d_, i_=i_: e.tensor_copy(d_, i_), reads=[pk], writes=[dk])
                    chk("K2")
                    ps, pk = getps()
                    for h in range(4):
                        S.op("pe", lambda e, h=h, ps=ps: e.matmul(ps[:, h * 128:(h + 1) * 128], rkT[:, h, :], rqT[:, h, :],
                                                                  start=True, stop=True), reads=["rkT", "rqT"], writes=[pk])
                    S.op("dve", lambda e, ps=ps: e.tensor_tensor(Sc[:], v3(ps[:], 4, 128), v3(dmask[:], 4, 128), ALU.mult),
                         reads=[pk, "dmask"], writes=["Sc"])
                    ps, pk = getps()
                    for h in range(4):
                        S.op("pe", lambda e, h=h, ps=ps: e.matmul(ps[:, h * 128:(h + 1) * 128], Sc[:, h, :],
                                                                  rv_b[:, h * 128:(h + 1) * 128], start=True, stop=False),
                             reads=["Sc", "rv_b"], writes=[pk])
                        S.op("pe", lambda e, h=h, ps=ps: e.matmul(ps[:, h * 128:(h + 1) * 128], rqT[:, h, :], RSb[:, h, :],
                                                                  start=False, stop=True), reads=["rqT", "RSb"], writes=[pk])
                    S.op("act", lambda e, ps=ps: e.activation(o_f[:], ps[:], AF.Copy), reads=[pk], writes=["o_f"])
                    ps, pk = getps()
                    for h in range(4):
                        S.op("pe", lambda e, h=h, ps=ps: e.matmul(ps[:, h * 128:(h + 1) * 128], rKh[:, h, :],
                                                                  rv_b[:, h * 128:(h + 1) * 128], start=True, stop=True),
                             reads=["rKh", "rv_b"], writes=[pk])
                    S.op("dve", lambda e: e.tensor_tensor(RSf[:], RSf[:], bc3(qkw[:, 8:12], 4, 128), ALU.mult),
                         reads=["RSf", "qkw"], writes=["RSf"])
                    S.op("dve", lambda e, ps=ps: e.tensor_tensor(RSf[:], RSf[:], v3(ps[:], 4, 128), ALU.add),
                         reads=[pk, "RSf"], writes=["RSf"])
                    S.op("pool", lambda e: e.tensor_copy(RSb[:], RSf[:]), reads=["RSf"], writes=["RSb"])
                    S.op("pool", lambda e: e.tensor_tensor(t1[:], o_f[:], o_f[:], ALU.mult), reads=["o_f"], writes=["t1"])
                    S.op("dve", lambda e: e.tensor_reduce(s4[:], v3(t1[:], 4, 128), AX.X, ALU.add), reads=["t1"], writes=["s4"])
                    S.op("dve", lambda e: e.tensor_scalar(s4[:], s4[:], 1.0 / 128, 1e-6, ALU.mult, ALU.add), reads=["s4"], writes=["s4"])
                    S.op("act", lambda e: e.activation(s4[:], s4[:], AF.Sqrt), reads=["s4"], writes=["s4"])
                    S.op("dve", lambda e: e.reciprocal(s4[:], s4[:]), reads=["s4"], writes=["s4"])
                    S.op("dve", lambda e: e.tensor_tensor(v3(o_f[:], 4, 128), v3(o_f[:], 4, 128), bc3(s4[:], 4, 128), ALU.mult),
                         reads=["o_f", "s4"], writes=["o_f"])
                    S.op("pool", lambda e: e.tensor_tensor(mixed[:, 512:1024], o_f[:], rg_s[:], ALU.mult),
                         reads=["o_f", "rg_s"], writes=["mixed"])

                    chk("E")
                    ps, pk = getps()
                    S.op("pe", lambda e, ps=ps: e.matmul(ps[:], lwT[:], wal[:, 0:512], start=True, stop=True),
                         reads=["lwT", "wal"], writes=[pk])
                    S.op("dve", lambda e, ps=ps: e.tensor_tensor(t0[:], ps[:], bcn["w0"][:], ALU.add),
                         reads=[pk, "bc_w0"], writes=["t0"])
                    S.op("act", lambda e: e.activation(sigw[:], t0[:], AF.Sigmoid), reads=["t0"], writes=["sigw"])
                    ps, pk = getps()
                    S.op("pe", lambda e, ps=ps: e.matmul(ps[:], lwT[:], wal[:, 512:1024], start=True, stop=True),
                         reads=["lwT", "wal"], writes=[pk])
                    S.op("dve", lambda e, ps=ps: e.tensor_tensor(t1[:], ps[:], bcn["a0"][:], ALU.add),
                         reads=[pk, "bc_a0"], writes=["t1"])
                    S.op("act", lambda e: e.activation(a_f[:], t1[:], AF.Sigmoid), reads=["t1"], writes=["a_f"])
                    ps, pk = getps()
                    S.op("pe", lambda e, ps=ps: e.matmul(ps[:], lgT[:], glu[:], start=True, stop=True),
                         reads=["lgT", "glu"], writes=[pk])
                    S.op("act", lambda e, ps=ps: e.activation(g_f[:], ps[:], AF.Copy), reads=[pk], writes=["g_f"])
                    S.op("pool", lambda e: e.tensor_copy(sighl[:, 0:512], sigw[:]), reads=["sigw"], writes=["sighl"])
                    S.op("dve", lambda e: e.tensor_tensor(sighl[:, 512:1024], sigw[:], sighl[:, 0:512], ALU.subtract),
                         reads=["sigw", "sighl"], writes=["sighl"])
                    ps, pk = getps()
                    for hl in range(2):
                        S.op("pe", lambda e, ps=ps, hl=hl: e.matmul(ps[:], tri_i[:], sighl[:, hl * 512:(hl + 1) * 512],
                                                                    start=(hl == 0), stop=(hl == 1)),
                             reads=["tri_i", "sighl"], writes=[pk])
                    S.op("act", lambda e, ps=ps: e.activation(Wi[:], ps[:], AF.Exp, scale=-C_DEC), reads=[pk], writes=["Wi"])
                    S.op("act", lambda e, ps=ps: e.activation(Wn[:], ps[:], AF.Exp, scale=C_DEC), reads=[pk], writes=["Wn"])
                    S.op("act", lambda e: e.activation(t0[:], sigw[:], AF.Exp, scale=C_DEC), reads=["sigw"], writes=["t0"])
                    S.op("dve", lambda e: e.tensor_tensor(We[:], Wi[:], t0[:], ALU.mult), reads=["Wi", "t0"], writes=["We"])
                    ps, pk = getps()
                    for hl in range(2):
                        S.op("pe", lambda e, ps=ps, hl=hl: e.matmul(ps[:], tri_r[:], sighl[:, hl * 512:(hl + 1) * 512],
                                                                    start=(hl == 0), stop=(hl == 1)),
                             reads=["tri_r", "sighl"], writes=[pk])
                    S.op("act", lambda e, ps=ps: e.activation(Wh[:], ps[:], AF.Exp, scale=-C_DEC), reads=[pk], writes=["Wh"])
                    ps, pk = getps()
                    for pr in range(4):
                        for hl in range(2):
                            S.op("pe", lambda e, pr=pr, ps=ps, hl=hl: e.matmul(
                                ps[:, pr * 2:pr * 2 + 2], sighl[:, hl * 512 + pr * 128:hl * 512 + (pr + 1) * 128],
                                seqind[:], start=(hl == 0), stop=(hl == 1)),
                                reads=["sighl", "seqind"], writes=[pk])
                    WC4 = WC[:].rearrange("p (pr par) -> p pr par", par=2)
                    for s in range(2):
                        for par in range(2):
                            S.op("act", lambda e, s=s, par=par, ps=ps: e.activation(
                                WC4[s * 64:(s + 1) * 64, :, par],
                                ps[par * 64:(par + 1) * 64, 0:8].rearrange("p (pr s) -> p pr s", s=2)[:, :, s], AF.Exp, scale=-C_DEC),
                                reads=[pk], writes=["WC"])

                    chk("F")
                    S.op("pool", lambda e: e.tensor_tensor(t1[:], k_f[:], bcn["k_k"][:], ALU.mult),
                         reads=["k_f", "bc_k_k"], writes=["t1"])
                    S.op("pool", lambda e: e.tensor_tensor(t0[:], t1[:], t1[:], ALU.mult), reads=["t1"], writes=["t0"])
                    S.op("dve", lambda e: e.tensor_reduce(s8[:], v3(t0[:], 8, 64), AX.X, ALU.add),
                         reads=["t0"], writes=["s8"])
                    S.op("dve", lambda e: e.tensor_scalar_max(s8[:], s8[:], 1e-24), reads=["s8"], writes=["s8"])
                    S.op("act", lambda e: e.activation(s8[:], s8[:], AF.Sqrt), reads=["s8"], writes=["s8"])
                    S.op("dve", lambda e: e.reciprocal(s8[:], s8[:]), reads=["s8"], writes=["s8"])
                    S.op("dve", lambda e: e.tensor_tensor(v3(kk[:], 8, 64), v3(t1[:], 8, 64), bc3(s8[:], 8, 64), ALU.mult),
                         reads=["t1", "s8"], writes=["kk"])
                    S.op("dve", lambda e: e.scalar_tensor_tensor(t0[:], a_f[:], -1.0, bcn["k_a"][:], ALU.add, ALU.mult),
                         reads=["a_f", "bc_k_a"], writes=["t0"])
                    S.op("dve", lambda e: e.scalar_tensor_tensor(k_h[:], t0[:], 1.0, k_f[:], ALU.add, ALU.mult),
                         reads=["t0", "k_f"], writes=["k_h"])
                    S.op("dve", lambda e: e.tensor_tensor(bp[:], kk[:], a_f[:], ALU.mult), reads=["kk", "a_f"], writes=["bp"])
                    S.op("dve", lambda e: e.scalar_tensor_tensor(tm4[0][:], kk[:], -1.0, We[:], ALU.mult, ALU.mult),
                         reads=["kk", "We"], writes=["tm0"])
                    S.op("pool", lambda e: e.tensor_tensor(tm4[1][:], r_f[:], Wi[:], ALU.mult), reads=["r_f", "Wi"], writes=["tm1"])
                    S.op("dve", lambda e: e.tensor_tensor(tm4[2][:], bp[:], Wn[:], ALU.mult), reads=["bp", "Wn"], writes=["tm2"])
                    S.op("pool", lambda e: e.tensor_tensor(tm4[3][:], k_h[:], Wn[:], ALU.mult), reads=["k_h", "Wn"], writes=["tm3"])
                    chk("F1")
                    for s in range(2):
                        sl_ = slice(s * 64, (s + 1) * 64)
                        S.op("dve" if s == 0 else "pool", lambda e, s=s, sl_=sl_: e.tensor_tensor(
                            Bh[sl_, :, s * 64:(s + 1) * 64], v3(bp[sl_, :], 8, 64), v3(Wh[sl_, :], 8, 64), ALU.mult),
                            reads=["bp", "Wh"], writes=["Bh"])
                        S.op("pool" if s == 0 else "dve", lambda e, s=s, sl_=sl_: e.tensor_tensor(
                            Kh[sl_, :, s * 64:(s + 1) * 64], v3(k_h[sl_, :], 8, 64), v3(Wh[sl_, :], 8, 64), ALU.mult),
                            reads=["k_h", "Wh"], writes=["Kh"])
                    chk("F2")
                    S.op("pool", lambda e: e.tensor_tensor(t0[:], r_f[:], k_h[:], ALU.mult), reads=["r_f", "k_h"], writes=["t0"])
                    S.op("pool", lambda e: e.tensor_tensor(t0[:], t0[:], bcn["r_k"][:], ALU.mult),
                         reads=["t0", "bc_r_k"], writes=["t0"])
                    S.op("dve", lambda e: e.tensor_reduce(bon8[:], v3(t0[:], 8, 64), AX.X, ALU.add),
                         reads=["t0"], writes=["bon8"])
                    chk("F3")
                    dkeys = ["arT", "arT", "bT", "kT"]
                    for qi in range(4):
                        ps, pk = getps()
                        psb = ps[:].bitcast(BF16)
                        for pr in range(4):
                            S.op("pe", lambda e, qi=qi, pr=pr, psb=psb: e.transpose(
                                psb[:, pr * 128:(pr + 1) * 128], tm4[qi][:, pr * 128:(pr + 1) * 128], ident_b[:]),
                                reads=[f"tm{qi}", "ident_b"], writes=[pk])
                        pv = psb[:, 0:512].rearrange("p (pr t) -> p pr t", pr=4)
                        if qi < 2:
                            dst = arT[:, :, qi, :]
                        elif qi == 2:
                            dst = bT[:]
                        else:
                            dst = kT[:]
                        dst5 = dst.rearrange("p (pr par) t -> p pr par t", par=2)
                        k_ = 0
                        for par in range(2 if (not _os.environ.get("MIX_NOEVAC") or str(qi) in _os.environ.get("MIX_EVACQ", "")) else 0):
                            for s in range(2):
                                d_ = dst5[s * 64:(s + 1) * 64, :, par, s * 64:(s + 1) * 64]
                                i_ = pv[par * 64:(par + 1) * 64, :, s * 64:(s + 1) * 64]
                                S.op("dve", lambda e, d_=d_, i_=i_: e.tensor_copy(d_, i_),
                                     reads=[pk], writes=[dkeys[qi]])
                                k_ += 1

                    chk("G")
                    for hp in range(4):
                        ps, pk = getps()
                        for hh in range(2):
                            h = hp * 2 + hh
                            S.op("pe", lambda e, h=h, hh=hh, ps=ps: e.matmul(
                                ps[:, hh * 256:(hh + 1) * 256], bT[:, h, :], arT[:, h, :, :].rearrange("p a t -> p (a t)"), start=True, stop=True),
                                reads=["bT", "arT"], writes=[pk])
                        S.op("dve", lambda e, hp=hp, ps=ps: e.tensor_tensor(
                            QA[:, hp * 2:hp * 2 + 2, :, :].rearrange("p h a t -> p h (a t)"),
                            v3(ps[:], 2, 256), bch(m1[:], 2, 256), ALU.mult),
                            reads=[pk, "m1"], writes=["QA"])
                        ps, pk = getps()
                        for hh in range(2):
                            h = hp * 2 + hh
                            S.op("pe", lambda e, h=h, hh=hh, ps=ps: e.matmul(
                                ps[:, hh * 256:(hh + 1) * 256], kT[:, h, :], arT[:, h, :, :].rearrange("p a t -> p (a t)"), start=True, stop=True),
                                reads=["kT", "arT"], writes=[pk])
                        S.op("dve", lambda e, hp=hp, ps=ps: e.tensor_tensor(
                            KA[:, hp * 2:hp * 2 + 2, :, :].rearrange("p h a t -> p h (a t)"),
                            v3(ps[:], 2, 256), bch(m1[:], 2, 256), ALU.mult),
                            reads=[pk, "m1"], writes=["KA"])
                    for hq in range(2):
                        ps, pk = getps()
                        for hh in range(4):
                            h = hq * 4 + hh
                            S.op("pe", lambda e, h=h, hh=hh, ps=ps: e.matmul(
                                ps[:, hh * 128:(hh + 1) * 128], arT[:, h, 0, :], bT[:, h, :], start=True, stop=True),
                                reads=["bT", "arT"], writes=[pk])
                        S.op("dve", lambda e, hq=hq, ps=ps: e.tensor_tensor(
                            Pm[0][:, hq * 4:hq * 4 + 4, :], v3(ps[:], 4, 128), bch(msl[:], 4, 128), ALU.mult),
                            reads=[pk, "msl"], writes=["Pm0"])
                    chk("H")
                    S.op("pool", lambda e: e.tensor_tensor(TTbs[0][:], QA[:, :, 0, :], bch(ident_f[:], 8, 128), ALU.add),
                         reads=["QA", "ident_f"], writes=["TTb0"])
                    for lvl in range(1, 6):
                        pi_, po_ = (lvl - 1) % 2, lvl % 2

                        def Qprev(h, lvl=lvl, pi_=pi_):
                            return QA[:, h, 0, :] if lvl == 1 else Qm[pi_][:, h, :]
                        qprev_key = "QA" if lvl == 1 else f"Qm{pi_}"
                        for hq in range(2):
                            ps, pk = getps()
                            for hh in range(4):
                                h = hq * 4 + hh
                                S.op("pe", lambda e, h=h, hh=hh, ps=ps, Qprev=Qprev, pi_=pi_: e.matmul(
                                    ps[:, hh * 128:(hh + 1) * 128], Qprev(h), Pm[pi_][:, h, :], start=True, stop=True),
                                    reads=[qprev_key, f"Pm{pi_}"], writes=[pk])
                            S.op("act", lambda e, hq=hq, ps=ps, po_=po_: e.activation(
                                Pm[po_][:, hq * 4:hq * 4 + 4, :], v3(ps[:], 4, 128), AF.Copy),
                                reads=[pk], writes=[f"Pm{po_}"])
                        if lvl < 5:
                            for hq in range(2):
                                ps, pk = getps()
                                for hh in range(4):
                                    h = hq * 4 + hh
                                    S.op("pe", lambda e, h=h, hh=hh, ps=ps, Qprev=Qprev, pi_=pi_: e.matmul(
                                        ps[:, hh * 128:(hh + 1) * 128], Pm[pi_][:, h, :], Qprev(h), start=True, stop=True),
                                        reads=[qprev_key, f"Pm{pi_}"], writes=[pk])
                                S.op("dve", lambda e, hq=hq, ps=ps, po_=po_: e.tensor_copy(
                                    Qm[po_][:, hq * 4:hq * 4 + 4, :], v3(ps[:], 4, 128)),
                                    reads=[pk], writes=[f"Qm{po_}"])
                        for hq in range(2):
                            ps, pk = getps()
                            for hh in range(4):
                                h = hq * 4 + hh
                                S.op("pe", lambda e, h=h, hh=hh, ps=ps, pi_=pi_, po_=po_: e.matmul(
                                    ps[:, hh * 128:(hh + 1) * 128], Pm[po_][:, h, :], TTbs[pi_][:, h, :], start=True, stop=False),
                                    reads=[f"Pm{po_}", f"TTb{pi_}"], writes=[pk])
                                S.op("pe", lambda e, h=h, hh=hh, ps=ps, pi_=pi_: e.matmul(
                                    ps[:, hh * 128:(hh + 1) * 128], ident_b[:], TTbs[pi_][:, h, :], start=False, stop=True),
                                    reads=["ident_b", f"TTb{pi_}"], writes=[pk])
                            if hq == 0:
                                S.op("act", lambda e, hq=hq, ps=ps, po_=po_: e.activation(
                                    TTbs[po_][:, hq * 4:hq * 4 + 4, :], v3(ps[:], 4, 128), AF.Copy),
                                    reads=[pk], writes=[f"TTb{po_}"])
                            else:
                                S.op("dve", lambda e, hq=hq, ps=ps, po_=po_: e.tensor_copy(
                                    TTbs[po_][:, hq * 4:hq * 4 + 4, :], v3(ps[:], 4, 128)),
                                    reads=[pk], writes=[f"TTb{po_}"])
                    TTb = TTbs[1]

                    chk("I")
                    ps, pk = getps()
                    for h in range(8):
                        S.op("pe", lambda e, h=h, ps=ps: e.matmul(ps[:, h * 64:(h + 1) * 64], arT[:, h, 0, :], Sb[:, h, :],
                                                                  start=True, stop=False), reads=["arT", "Sb"], writes=[pk])
                        S.op("pe", lambda e, h=h, ps=ps: e.matmul(ps[:, h * 64:(h + 1) * 64], KA[:, h, 0, :],
                                                                  v_b[:, h * 64:(h + 1) * 64], start=False, stop=True),
                             reads=["KA", "v_b"], writes=[pk])
                    S.op("act", lambda e, ps=ps: e.activation(Xb[:], v3(ps[:], 8, 64), AF.Copy), reads=[pk], writes=["Xb"])
                    ps, pk = getps()
                    for h in range(8):
                        S.op("pe", lambda e, h=h, ps=ps: e.matmul(ps[:, h * 64:(h + 1) * 64], TTb[:, h, :], Xb[:, h, :],
                                                                  start=True, stop=True), reads=["TTb1", "Xb"], writes=[pk])
                    S.op("dve", lambda e, ps=ps: e.tensor_copy(Ub[:], v3(ps[:], 8, 64)), reads=[pk], writes=["Ub"])
                    ps, pk = getps()
                    for h in range(8):
                        S.op("pe", lambda e, h=h, ps=ps: e.matmul(ps[:, h * 64:(h + 1) * 64], arT[:, h, 1, :], Sb[:, h, :],
                                                                  start=True, stop=False), reads=["arT", "Sb"], writes=[pk])
                        S.op("pe", lambda e, h=h, ps=ps: e.matmul(ps[:, h * 64:(h + 1) * 64], QA[:, h, 1, :], Ub[:, h, :],
                                                                  start=False, stop=False), reads=["QA", "Ub"], writes=[pk])
                        S.op("pe", lambda e, h=h, ps=ps: e.matmul(ps[:, h * 64:(h + 1) * 64], KA[:, h, 1, :],
                                                                  v_b[:, h * 64:(h + 1) * 64], start=False, stop=True),
                             reads=["KA", "v_b"], writes=[pk])
                    S.op("act", lambda e, ps=ps: e.activation(y_f[:], ps[:], AF.Copy), reads=[pk], writes=["y_f"])
                    ps, pk = getps()
                    for h in range(8):
                        S.op("pe", lambda e, h=h, ps=ps: e.matmul(ps[:, h * 64:(h + 1) * 64], Bh[:, h, :], Ub[:, h, :],
                                                                  start=True, stop=False), reads=["Bh", "Ub"], writes=[pk])
                        S.op("pe", lambda e, h=h, ps=ps: e.matmul(ps[:, h * 64:(h + 1) * 64], Kh[:, h, :],
                                                                  v_b[:, h * 64:(h + 1) * 64], start=False, stop=True),
                             reads=["Kh", "v_b"], writes=[pk])
                    S.op("dve", lambda e: e.tensor_tensor(Sf[:], Sf[:], bc3(WC[:], 8, 64), ALU.mult),
                         reads=["Sf", "WC"], writes=["Sf"])
                    S.op("dve", lambda e, ps=ps: e.tensor_tensor(Sf[:], Sf[:], v3(ps[:], 8, 64), ALU.add),
                         reads=[pk, "Sf"], writes=["Sf"])
                    S.op("pool", lambda e: e.tensor_copy(Sb[:], Sf[:]), reads=["Sf"], writes=["Sb"])

                    chk("J")
                    y3 = v3(y_f[:], 8, 64)
                    S.op("dve", lambda e: e.tensor_reduce(s8[:], y3, AX.X, ALU.add), reads=["y_f"], writes=["s8"])
                    S.op("dve", lambda e: e.tensor_scalar(s8[:], s8[:], 1.0 / 64, None, ALU.mult), reads=["s8"], writes=["s8"])
                    S.op("dve", lambda e: e.tensor_tensor(y3, y3, bc3(s8[:], 8, 64), ALU.subtract),
                         reads=["y_f", "s8"], writes=["y_f"])
                    S.op("pool", lambda e: e.tensor_tensor(t0[:], y_f[:], y_f[:], ALU.mult), reads=["y_f"], writes=["t0"])
                    S.op("dve", lambda e: e.tensor_reduce(s8[:], v3(t0[:], 8, 64), AX.X, ALU.add), reads=["t0"], writes=["s8"])
                    S.op("dve", lambda e: e.tensor_scalar(s8[:], s8[:], 1.0 / 64, 64e-5, ALU.mult, ALU.add),
                         reads=["s8"], writes=["s8"])
                    S.op("act", lambda e: e.activation(s8[:], s8[:], AF.Sqrt), reads=["s8"], writes=["s8"])
                    S.op("dve", lambda e: e.reciprocal(s8[:], s8[:]), reads=["s8"], writes=["s8"])
                    S.op("dve", lambda e: e.tensor_tensor(y3, y3, bc3(s8[:], 8, 64), ALU.mult), reads=["y_f", "s8"], writes=["y_f"])
                    S.op("pool", lambda e: e.tensor_tensor(y_f[:], y_f[:], bcn["ln_x_w"][:], ALU.mult),
                         reads=["y_f", "bc_ln_x_w"], writes=["y_f"])
                    S.op("pool", lambda e: e.tensor_tensor(y_f[:], y_f[:], bcn["ln_x_b"][:], ALU.add),
                         reads=["y_f", "bc_ln_x_b"], writes=["y_f"])
                    S.op("dve", lambda e: e.tensor_tensor(v3(t0[:], 8, 64), v3(v_b[:], 8, 64), bc3(bon8[:], 8, 64), ALU.mult),
                         reads=["v_b", "bon8"], writes=["t0"])
                    S.op("pool", lambda e: e.tensor_tensor(y_f[:], y_f[:], t0[:], ALU.add), reads=["y_f", "t0"], writes=["y_f"])
                    S.op("dve", lambda e: e.tensor_tensor(mixed[:, 0:512], y_f[:], g_f[:], ALU.mult),
                         reads=["y_f", "g_f"], writes=["mixed"])

                    chk("L")
                    ps, pk = getps()
                    psb = ps[:].bitcast(BF16)
                    for c in range(8):
                        S.op("pe", lambda e, c=c, psb=psb: e.transpose(psb[:, c * 128:(c + 1) * 128],
                                                                       mixed[:, c * 128:(c + 1) * 128], ident_b[:]),
                             reads=["mixed", "ident_b"], writes=[pk])
                    S.op("act", lambda e, psb=psb: e.activation(mT[:], psb.rearrange("p (c t) -> p c t", c=8), AF.Copy),
                         reads=[pk], writes=["mT"])
                    for nh in range(2):
                        ps, pk = getps()
                        for c in range(8):
                            S.op("pe", lambda e, c=c, nh=nh, ps=ps: e.matmul(ps[:], mT[:, c, :], wo[:, c, nh * 512:(nh + 1) * 512],
                                                                             start=(c == 0), stop=(c == 7)),
                                 reads=["mT", "wo"], writes=[pk])
                        S.op("dve", lambda e, nh=nh, ps=ps, xt=xt: e.tensor_tensor(xt[:, nh * 512:(nh + 1) * 512],
                                                                            xt[:, nh * 512:(nh + 1) * 512], ps[:], ALU.add),
                             reads=[pk, mxk], writes=[mxk])
                  except _StopTile:
                    pass
                  for s in range(2):
                        r0 = s * seq + n * 64
                        S.dma("sp", out[r0:r0 + 64, :], xt[s * 64:(s + 1) * 64, :], reads=[mxk], sem=f"ms{n % 2}")
                S.barrier()
            src_x[0] = out

        for l in range(depth):
            if do_ffn:
                ffn_phase(l, 1, False)
            if do_mix:
                mix_phase(l)
            if do_ffn:
                ffn_phase(l, 2, final_norm and l == depth - 1)
        S.emit(final_waits=[k for k in S.dma_counts if k.startswith("fs") or k.startswith("ms")])
    return nc


_CACHE = {}


def kernel(**inputs):
    x = np.ascontiguousarray(inputs["x"], dtype=np.float32)
    B, seq, d = x.shape
    depth = inputs["w_in"].shape[0]
    key = (seq, depth)
    if key not in _CACHE:
        _CACHE[key] = build_program(seq, depth)
    nc = _CACHE[key]
    consts = make_consts(seq)
    shared = {}
    for k, v in inputs.items():
        if k == "x":
            continue
        a = np.ascontiguousarray(v, dtype=np.float32)
        if k == "r_k":
            a = a.reshape(depth, 512)
        if k == "final_norm":
            a = a.reshape(1, D)
        shared[k] = a
    for k in CONST_ORDER:
        shared["c_" + k] = consts[k]
    ncores = B // 2
    in_maps = []
    for c in range(ncores):
        m = dict(shared)
        m["x"] = x[2 * c:2 * c + 2].reshape(2 * seq, d)
        in_maps.append(m)
    res = run_bass_kernel_spmd(nc, in_maps, core_ids=list(range(ncores)))
    outs = [r["out"].reshape(2, seq, d) for r in res.results]
    return np.concatenate(outs, axis=0).astype(np.float32)
```
